# Optimizing a Trainium2 kernel written in Bass

```python
import math
import jax, jax.numpy as jnp
from jax import lax
import numpy as np

D_MODEL = 4096
BATCH = 4
SEQ = 4096
DEPTH = 1

HEAD_DIM = 64
D_MIX = D_MODEL
D_RWKV = D_MIX // 2
D_ATTN = D_MIX - D_RWKV
N_RWKV_HEADS = D_RWKV // HEAD_DIM
N_Q_HEADS = D_ATTN // HEAD_DIM
N_KV_HEADS = 8
GQA_GROUP = N_Q_HEADS // N_KV_HEADS
D_KV = N_KV_HEADS * HEAD_DIM
WINDOW = 128
BLOCK = 128


def _lora_dim(c, factor, power):
    return max(32, int(round(factor * c ** power / 32)) * 32)


D_DECAY_LORA = _lora_dim(D_RWKV, 1.8, 0.5)
D_AAA_LORA = _lora_dim(D_RWKV, 1.8, 0.5)
D_GATE_LORA = _lora_dim(D_RWKV, 0.6, 0.8)
N_RWKV_COLS = 3 * D_RWKV + D_DECAY_LORA + D_AAA_LORA + D_GATE_LORA
RWKV_SPLITS = [D_RWKV, 2 * D_RWKV, 3 * D_RWKV, 3 * D_RWKV + D_DECAY_LORA,
               3 * D_RWKV + D_DECAY_LORA + D_AAA_LORA]
N_ATTN_COLS = D_ATTN + 2 * D_KV
N_IN_COLS = N_RWKV_COLS + N_ATTN_COLS
RWKV_GN_EPS = 64e-5
NORM_EPS = 1e-6

N_GROUPS = 8
EXPERTS_PER_GROUP = 8
N_EXPERTS = N_GROUPS * EXPERTS_PER_GROUP
TOP_K_IN_GROUP = 2
D_EXPERT = D_MODEL // 8
MOE_BLOCK = 128

kernel_name = 'hymba_rwkv7_swa_sink_alibi_hmoe_adaln_block'


def rmsnorm(x, g):
    xf = x.astype(jnp.float32)
    y = xf * lax.rsqrt(jnp.mean(xf * xf, axis=-1, keepdims=True) + NORM_EPS)
    return (y * g).astype(x.dtype)


def modulate(u, shift, scale):
    return u * (1.0 + scale[:, None, :]) + shift[:, None, :]


def token_shift(p):
    return jnp.pad(p, ((0, 0), (1, 0), (0, 0)))[:, :-1]


def rwkv7_time_mix(p, mu, w0, w_up, a0, a_up, g_up, k_k, k_a, r_k, lnx_w, lnx_b):
    B, S, _ = p.shape
    H, N = N_RWKV_HEADS, HEAD_DIM
    f32 = jnp.float32
    p = p + (token_shift(p) - p) * mu
    r, k, v, xw, xa, xg = jnp.split(p, RWKV_SPLITS, axis=-1)
    w = -jax.nn.softplus(-(w0 + jnp.tanh(xw) @ w_up)) - 0.5
    decay = jnp.exp(-jnp.exp(w.astype(f32)))
    a = jax.nn.sigmoid(a0 + xa @ a_up)
    g = jax.nn.sigmoid(xg) @ g_up

    def heads(t):
        return t.astype(f32).reshape(B, S, H, N)

    kk = heads(k * k_k)
    kk = kk / jnp.maximum(jnp.sqrt(jnp.sum(kk * kk, axis=-1, keepdims=True)), 1e-12)
    k = k * (1.0 + (a - 1.0) * k_a)
    r_h, k_h, v_h, a_h, w_h = heads(r), heads(k), heads(v), heads(a), heads(decay)

    def step(state, inp):
        r_t, w_t, k_t, v_t, kk_t, b_t = inp
        sa = jnp.einsum('bhij,bhj->bhi', state, -kk_t)
        state = (state * w_t[:, :, None, :] + sa[..., :, None] * b_t[..., None, :]
                 + v_t[..., :, None] * k_t[..., None, :])
        y_t = jnp.einsum('bhij,bhj->bhi', state, r_t)
        return state, y_t

    def tm(t):
        return jnp.moveaxis(t, 1, 0)

    state0 = jnp.zeros((B, H, N, N), f32)
    _, y = lax.scan(step, state0, (tm(r_h), tm(w_h), tm(k_h), tm(v_h), tm(kk), tm(kk * a_h)))
    y = jnp.moveaxis(y, 0, 1)
    mean = jnp.mean(y, axis=-1, keepdims=True)
    var = jnp.mean(jnp.square(y - mean), axis=-1, keepdims=True)
    y = ((y - mean) * lax.rsqrt(var + RWKV_GN_EPS)).reshape(B, S, D_RWKV) * lnx_w + lnx_b
    bonus = (jnp.sum(r_h * k_h * r_k, axis=-1, keepdims=True) * v_h).reshape(B, S, D_RWKV)
    return ((y + bonus) * g).astype(p.dtype)


def sliding_window_attention(q, k, v, sinks, out_g):
    B, S, _ = q.shape
    nb = S // BLOCK
    f32 = jnp.float32
    qb = q.reshape(B, nb, BLOCK, N_KV_HEADS, GQA_GROUP, HEAD_DIM)

    def band(t):
        t = t.reshape(B, S, N_KV_HEADS, HEAD_DIM)
        tp = jnp.pad(t, ((0, 0), (BLOCK, 0), (0, 0), (0, 0))).reshape(B, nb + 1, BLOCK, N_KV_HEADS, HEAD_DIM)
        return jnp.concatenate([tp[:, :-1], tp[:, 1:]], axis=2)

    kb, vb = band(k), band(v)
    qi = jnp.arange(BLOCK)[:, None]
    kj = jnp.arange(2 * BLOCK)[None, :]
    dist = qi + BLOCK - kj
    in_window = (dist >= 0) & (dist < WINDOW)
    slopes = jnp.exp2(-8.0 * jnp.arange(1, N_Q_HEADS + 1, dtype=f32) / N_Q_HEADS)
    slopes = slopes.reshape(N_KV_HEADS, GQA_GROUP)
    alibi = -slopes[:, :, None, None] * dist.astype(f32)
    sink = sinks.astype(f32).reshape(N_KV_HEADS, GQA_GROUP)[None, :, :, None]
    scale = HEAD_DIM ** -0.5

    def one_block(args):
        n, qn, kn, vn = args
        s = jnp.einsum('bqhgd,bkhd->bhgqk', qn, kn, preferred_element_type=f32) * scale + alibi
        valid = in_window & (n * BLOCK - BLOCK + kj >= 0)
        s = jnp.where(valid, s, -jnp.inf)
        m = jnp.maximum(jnp.max(s, axis=-1), sink)
        pr = jnp.exp(s - m[..., None])
        denom = jnp.sum(pr, axis=-1) + jnp.exp(sink - m)
        pr = pr / denom[..., None]
        return jnp.einsum('bhgqk,bkhd->bqhgd', pr.astype(vn.dtype), vn)

    o = lax.map(one_block, (jnp.arange(nb), jnp.moveaxis(qb, 1, 0),
                            jnp.moveaxis(kb, 1, 0), jnp.moveaxis(vb, 1, 0)))
    o = jnp.moveaxis(o, 0, 1).reshape(B, S, N_Q_HEADS, HEAD_DIM).astype(f32)
    o = o * lax.rsqrt(jnp.mean(o * o, axis=-1, keepdims=True) + NORM_EPS)
    return (o.reshape(B, S, D_ATTN) * out_g).astype(q.dtype)


def hierarchical_moe(u, router_group, router_group_bias, router_expert, router_expert_bias,
                     we_gate, we_up, we_down):
    B, S, D = u.shape
    T = B * S
    f32 = jnp.float32
    xf = u.reshape(T, D)
    pg = jax.nn.softmax((xf @ router_group).astype(f32) + router_group_bias, axis=-1)
    g_idx = jnp.argmax(pg, axis=-1)
    g_w = jnp.take_along_axis(pg, g_idx[:, None], axis=-1)[:, 0]
    le = ((xf @ router_expert).astype(f32) + router_expert_bias).reshape(T, N_GROUPS, EXPERTS_PER_GROUP)
    le = jnp.take_along_axis(le, g_idx[:, None, None], axis=1)[:, 0]
    pe = jax.nn.softmax(le, axis=-1)
    top_w, top_i = lax.top_k(pe, TOP_K_IN_GROUP)
    top_w = top_w / jnp.sum(top_w, axis=-1, keepdims=True)
    expert_id = g_idx[:, None] * EXPERTS_PER_GROUP + top_i
    weight = g_w[:, None] * top_w

    M = T * TOP_K_IN_GROUP
    e_flat = expert_id.reshape(M).astype(jnp.int32)
    w_flat = weight.reshape(M)
    tok_flat = jnp.repeat(jnp.arange(T, dtype=jnp.int32), TOP_K_IN_GROUP)
    order = jnp.argsort(e_flat)
    e_sorted = e_flat[order]
    counts = jnp.bincount(e_flat, length=N_EXPERTS)
    starts = jnp.cumsum(counts) - counts
    padded = ((counts + MOE_BLOCK - 1) // MOE_BLOCK) * MOE_BLOCK
    pends = jnp.cumsum(padded)
    pstarts = pends - padded
    dest = pstarts[e_sorted] + (jnp.arange(M) - starts[e_sorted])
    NB = -(-M // MOE_BLOCK) + N_EXPERTS
    P = NB * MOE_BLOCK
    row_tok = jnp.full((P,), T, jnp.int32).at[dest].set(tok_flat[order])
    row_w = jnp.zeros((P,), f32).at[dest].set(w_flat[order])
    block_expert = jnp.minimum(jnp.searchsorted(pends, jnp.arange(NB) * MOE_BLOCK, side='right'),
                               N_EXPERTS - 1)
    x_pad = jnp.concatenate([xf, jnp.zeros((1, D), xf.dtype)], axis=0)

    def body(acc, inp):
        tok, w, e = inp
        xb = x_pad[tok]
        h = jax.nn.silu(xb @ we_gate[e]) * (xb @ we_up[e])
        yb = ((h @ we_down[e]) * w[:, None]).astype(acc.dtype)
        return acc.at[tok].add(yb), None

    acc0 = jnp.zeros((T + 1, D), u.dtype)
    acc, _ = lax.scan(body, acc0, (row_tok.reshape(NB, MOE_BLOCK), row_w.reshape(NB, MOE_BLOCK), block_expert))
    return acc[:T].reshape(B, S, D)


def setup_inputs(seed: int = 0) -> dict:
    key = jax.random.key(seed)
    ks = jax.random.split(key, 32)
    L, D = DEPTH, D_MODEL
    f32 = jnp.float32

    def nrm(k, shape, scale):
        return jax.random.normal(k, shape, f32) * scale

    chan = jnp.arange(D_RWKV, dtype=f32) / (D_RWKV - 1)
    w0 = -6.0 + 5.0 * chan ** 0.7
    return {
        'x': nrm(ks[0], (BATCH, SEQ, D), 1.0),
        'c': nrm(ks[1], (BATCH, D), 1.0),
        'w_cond': nrm(ks[2], (L, D, 6 * D), D ** -0.5),
        'b_cond': nrm(ks[3], (L, 6 * D), 0.01),
        'norm1_g': 1.0 + nrm(ks[4], (L, D), 0.01),
        'w_in': nrm(ks[5], (L, D, N_IN_COLS), D ** -0.5),
        'rwkv_mu': jax.random.uniform(ks[6], (L, N_RWKV_COLS), f32),
        'rwkv_w0': w0[None, :] + nrm(ks[7], (L, D_RWKV), 0.1),
        'rwkv_w_up': nrm(ks[8], (L, D_DECAY_LORA, D_RWKV), 0.1),
        'rwkv_a0': nrm(ks[9], (L, D_RWKV), 0.1),
        'rwkv_a_up': nrm(ks[10], (L, D_AAA_LORA, D_RWKV), D_AAA_LORA ** -0.5),
        'rwkv_g_up': nrm(ks[11], (L, D_GATE_LORA, D_RWKV), D_GATE_LORA ** -0.5),
        'rwkv_k_k': 0.85 + nrm(ks[12], (L, D_RWKV), 0.05),
        'rwkv_k_a': 1.0 + nrm(ks[13], (L, D_RWKV), 0.05),
        'rwkv_r_k': nrm(ks[14], (L, N_RWKV_HEADS, HEAD_DIM), 0.1),
        'rwkv_lnx_w': 1.0 + nrm(ks[15], (L, D_RWKV), 0.01),
        'rwkv_lnx_b': nrm(ks[16], (L, D_RWKV), 0.01),
        'attn_sinks': nrm(ks[17], (L, N_Q_HEADS), 0.5),
        'attn_out_g': 1.0 + nrm(ks[18], (L, D_ATTN), 0.01),
        'w_out': nrm(ks[19], (L, D_MIX, D), D_MIX ** -0.5),
        'norm2_g': 1.0 + nrm(ks[20], (L, D), 0.01),
        'router_group': nrm(ks[21], (L, D, N_GROUPS), D ** -0.5),
        'router_group_bias': nrm(ks[22], (L, N_GROUPS), 0.01),
        'router_expert': nrm(ks[23], (L, D, N_EXPERTS), D ** -0.5),
        'router_expert_bias': nrm(ks[24], (L, N_EXPERTS), 0.01),
        'expert_w_gate': nrm(ks[25], (L, N_EXPERTS, D, D_EXPERT), D ** -0.5),
        'expert_w_up': nrm(ks[26], (L, N_EXPERTS, D, D_EXPERT), D ** -0.5),
        'expert_w_down': nrm(ks[27], (L, N_EXPERTS, D_EXPERT, D), D_EXPERT ** -0.5),
        'norm_f_g': 1.0 + nrm(ks[28], (D,), 0.01),
    }


def reference(x, c, w_cond, b_cond, norm1_g, w_in, rwkv_mu, rwkv_w0, rwkv_w_up, rwkv_a0,
              rwkv_a_up, rwkv_g_up, rwkv_k_k, rwkv_k_a, rwkv_r_k, rwkv_lnx_w, rwkv_lnx_b,
              attn_sinks, attn_out_g, w_out, norm2_g, router_group, router_group_bias,
              router_expert, router_expert_bias, expert_w_gate, expert_w_up, expert_w_down,
              norm_f_g):
    for l in range(DEPTH):
        mod = jax.nn.silu(c) @ w_cond[l] + b_cond[l]
        sh1, sc1, g1, sh2, sc2, g2 = jnp.split(mod, 6, axis=-1)
        u = modulate(rmsnorm(x, norm1_g[l]), sh1, sc1)
        proj = u @ w_in[l]
        p_rwkv = proj[..., :N_RWKV_COLS]
        q = proj[..., N_RWKV_COLS:N_RWKV_COLS + D_ATTN]
        k = proj[..., N_RWKV_COLS + D_ATTN:N_RWKV_COLS + D_ATTN + D_KV]
        v = proj[..., N_RWKV_COLS + D_ATTN + D_KV:]
        y_rwkv = rwkv7_time_mix(p_rwkv, rwkv_mu[l], rwkv_w0[l], rwkv_w_up[l], rwkv_a0[l],
                                rwkv_a_up[l], rwkv_g_up[l], rwkv_k_k[l], rwkv_k_a[l],
                                rwkv_r_k[l], rwkv_lnx_w[l], rwkv_lnx_b[l])
        y_attn = sliding_window_attention(q, k, v, attn_sinks[l], attn_out_g[l])
        y = jnp.concatenate([y_rwkv, y_attn], axis=-1) @ w_out[l]
        x = x + g1[:, None, :] * y
        u = modulate(rmsnorm(x, norm2_g[l]), sh2, sc2)
        y = hierarchical_moe(u, router_group[l], router_group_bias[l], router_expert[l],
                             router_expert_bias[l], expert_w_gate[l], expert_w_up[l], expert_w_down[l])
        x = x + g2[:, None, :] * y
    return rmsnorm(x, norm_f_g)
```

```python
import contextlib
import numpy as np
import ml_dtypes
import concourse.bass as bass
import concourse.mybir as mybir
from concourse.bass_utils import run_bass_kernel_spmd

F32 = mybir.dt.float32
BF16 = mybir.dt.bfloat16
I32 = mybir.dt.int32
AF = mybir.ActivationFunctionType
ALU = mybir.AluOpType
AX = mybir.AxisListType

ENGS = ("pe", "act", "dve", "pool", "sp")
HANDLES = {"pe": "tensor", "act": "scalar", "dve": "vector", "pool": "gpsimd", "sp": "sync"}

D = 4096
T = 4096
NRW = 6592
NIN = 9664
DR = 2048
EPS = 1e-6
GN_EPS = 64e-5
NB = 128
NEG = -30000.0


class Buf:
    __slots__ = ("lw", "rd")

    def __init__(self):
        self.lw = None
        self.rd = []


class Tl:
    __slots__ = ("t", "b")

    def __init__(self, t):
        self.t = t
        self.b = Buf()


class Sched:
    NDMA = {"sp": 16, "pool": 48}

    def __init__(self, nc, stack):
        self.nc = nc
        self.ops = {e: [] for e in ENGS}
        self.cnt = {e: 0 for e in ENGS}
        self.esem = {e: stack.enter_context(nc.semaphore("es_" + e)) for e in ENGS if e != "sp"}
        self.dsem = {q: [stack.enter_context(nc.semaphore("ds_%s%d" % (q, i))) for i in range(self.NDMA[q])]
                     for q in ("sp", "pool")}
        self.dcnt = {q: [0] * self.NDMA[q] for q in self.dsem}
        self.dnext = {q: 0 for q in self.dsem}
        self.seen = {e: {} for e in ENGS}
        self.psb = [Tl(stack.enter_context(nc.psum_tensor("psb%d" % i, [128, 512], F32))) for i in range(8)]
        self.psn = 0
        self.held = set()
        self.ntile = 0

    def tile(self, st, shape, dtype=F32):
        self.ntile += 1
        return Tl(st.enter_context(self.nc.sbuf_tensor("t%d" % self.ntile, list(shape), dtype)))

    def ps(self, hold=False):
        while True:
            p = self.psb[self.psn % 8]
            self.psn += 1
            if id(p) not in self.held:
                break
        if hold:
            self.held.add(id(p))
        return p

    def ps_release(self, p):
        self.held.discard(id(p))

    def _need(self, eng, waits, ev):
        if ev is None:
            return
        sem, val, src = ev
        if eng == "pe" and src == "pe":
            return
        key = id(sem)
        if self.seen[eng].get(key, 0) >= val:
            return
        cur = waits.get(key)
        if cur is None or cur[1] < val:
            waits[key] = (sem, val)

    def _deps(self, eng, reads, writes):
        waits = {}
        for b in reads:
            self._need(eng, waits, b.b.lw)
        for b in writes:
            self._need(eng, waits, b.b.lw)
            for r in b.b.rd:
                self._need(eng, waits, r)
        for key, (sem, val) in waits.items():
            self.seen[eng][key] = val
        return list(waits.values())

    def _commit(self, ev, reads, writes):
        for b in reads:
            b.b.rd.append(ev)
        for b in writes:
            b.b.lw = ev
            b.b.rd = []

    def _emit(self, eng, fn, waits, inc):
        eh = getattr(self.nc, HANDLES[eng])
        for sem, val in waits:
            eh.wait_ge(sem, val)
        if fn is not None:
            fn(eh).then_inc(inc[0], inc[1])

    def op(self, eng, fn, reads=(), writes=()):
        waits = self._deps(eng, reads, writes)
        self.cnt[eng] += 1
        ev = (self.esem[eng], self.cnt[eng], eng)
        self._emit(eng, fn, waits, (self.esem[eng], 1))
        self._commit(ev, reads, writes)

    def dma(self, q, fn, reads=(), writes=()):
        waits = self._deps(q, reads, writes)
        k = self.dnext[q]
        self.dnext[q] = (k + 1) % self.NDMA[q]
        sem = self.dsem[q][k]
        prev = self.dcnt[q][k]
        if prev > 0 and self.seen[q].get(id(sem), 0) < prev:
            self.seen[q][id(sem)] = prev
            waits.append((sem, prev))
        self.dcnt[q][k] = prev + 16
        ev = (sem, prev + 16, "dma")
        self._emit(q, fn, waits, (sem, 16))
        self._commit(ev, reads, writes)

    def barrier(self):
        evs = [(self.esem[e], self.cnt[e], "bar") for e in self.esem if self.cnt[e] > 0]
        for q in self.dsem:
            for k in range(self.NDMA[q]):
                if self.dcnt[q][k] > 0:
                    evs.append((self.dsem[q][k], self.dcnt[q][k], "bar"))
        for e in ENGS:
            waits = {}
            for ev in evs:
                self._need(e, waits, ev)
            for key, (sem, val) in waits.items():
                self.seen[e][key] = val
            if waits:
                self._emit(e, None, list(waits.values()), None)

    def flush(self):
        self.barrier()


C_ID = 0
C_ONES = 128
C_TRI = 256
C_ML = 384
C_MU = 448
C_MUI = 512
C_SU = 576
C_SUI = 640
C_K128 = 704
C_PID = 768
C_P128 = 769
C_IOTA64 = 770
C_RM = 834
C_END = C_RM + 2048


def make_consts():
    c = np.zeros((128, C_END), np.float32)
    c[:, C_ID:C_ID + 128] = np.eye(128)
    c[:, C_ONES:C_ONES + 128] = 1.0
    i = np.arange(128)
    c[:, C_TRI:C_TRI + 128] = (i[:, None] < i[None, :])
    j = np.arange(64)
    c[:64, C_ML:C_ML + 64] = (j[None, :] < j[:, None])
    c[:64, C_MU:C_MU + 64] = (j[:, None] < j[None, :])
    c[:64, C_MUI:C_MUI + 64] = (j[:, None] <= j[None, :])
    c[:64, C_SU:C_SU + 64] = (j[:, None] < j[None, :])
    c[:64, C_SUI:C_SUI + 64] = (j[:, None] <= j[None, :])
    c[:, C_K128:C_K128 + 64] = 128.0 * j[None, :]
    c[:, C_PID] = i
    c[:, C_P128] = 128.0 * i
    c[:, C_IOTA64:C_IOTA64 + 64] = j[None, :]
    rm = np.ones(2048, np.float32)
    rm[::64] = 0.0
    c[:, C_RM:C_RM + 2048] = rm[None, :]
    return c


def make_attn_bias():
    qi = np.arange(128)[:, None]
    kj = np.arange(256)[None, :]
    dist = (qi + 128 - kj).astype(np.float32)
    inw = (dist >= 0) & (dist < 128)
    slopes = np.exp2(-8.0 * np.arange(1, 33, dtype=np.float32) / 32).astype(np.float32).reshape(8, 4)
    ab = np.zeros((128, 2, 8, 4, 256), np.float32)
    for h in range(8):
        for g in range(4):
            a = np.where(inw, -slopes[h, g] * dist, NEG).astype(np.float32)
            ab[:, 0, h, g, :] = a
            a0 = a.copy()
            a0[:, :128] = NEG
            ab[:, 1, h, g, :] = a0
    return ab.reshape(128, 2 * 8 * 4 * 256)


def build(last_phase=99, debug=False):
    nc = bass.Bass("TRN2", target_bir_lowering=False)
    io = {}

    def din(name, shape, dt=F32):
        io[name] = nc.dram_tensor(name, list(shape), dt, kind="ExternalInput").ap()

    din("x", [T, D]); din("c", [1, D]); din("w_cond", [D, 6 * D]); din("b_cond", [1, 6 * D])
    din("norm1_g", [1, D]); din("w_in", [D, NIN]); din("rwkv_mu", [1, NRW]); din("rwkv_w0", [1, DR])
    din("rwkv_w_up", [96, DR]); din("rwkv_a0", [1, DR]); din("rwkv_a_up", [96, DR]); din("rwkv_g_up", [256, DR])
    din("rwkv_k_k", [1, DR]); din("rwkv_k_a", [1, DR]); din("rwkv_r_k", [1, DR]); din("rwkv_lnx_w", [1, DR])
    din("rwkv_lnx_b", [1, DR]); din("attn_sinks", [1, 32]); din("attn_out_g", [1, DR]); din("w_out", [D, D])
    din("norm2_g", [1, D]); din("router", [D, 72]); din("router_bias", [1, 72])
    if last_phase >= 5:
        din("ew_gate", [64 * D, 512]); din("ew_up", [64 * D, 512]); din("ew_down", [64 * 512, D])
    din("norm_f_g", [1, D]); din("consts", [128, C_END]); din("attn_bias", [128, 2 * 8 * 1024])
    out = nc.dram_tensor("out", [T, D], F32, kind="ExternalOutput").ap()

    def scratch(name, shape, dt=F32):
        kind = "ExternalOutput" if debug else "Internal"
        io[name] = nc.dram_tensor(name, list(shape), dt, kind=kind).ap()

    scratch("MODROW", [1, 6 * D]); scratch("PT", [NIN, T]); scratch("YT", [D, T]); scratch("X1", [T, D])
    scratch("U2", [T + 1, D], BF16); scratch("ROWTOK", [NB * 128, 16]); scratch("ROWW", [NB * 128, 16])
    scratch("YACC", [T + 1, D])

    with contextlib.ExitStack() as gs:
        S = Sched(nc, gs)
        CONST = S.tile(gs, [128, C_END])
        S.dma("sp", lambda e: e.dma_start(out=CONST.t[:], in_=io["consts"]), writes=[CONST])
        ident = CONST.t[:, C_ID:C_ID + 128]
        A1 = S.tile(gs, [128, 32]); SH1 = S.tile(gs, [128, 32]); A2 = S.tile(gs, [128, 32]); SH2 = S.tile(gs, [128, 32])

        def col_from_row(ph, src_ap, dst_fn):
            r = S.tile(ph, [32, 128])
            S.dma("sp", lambda e: e.dma_start(out=r.t[:], in_=src_ap.rearrange("o (k p) -> (o k) p", p=128)), writes=[r])
            ps = S.ps()
            S.op("pe", lambda e: e.transpose(ps.t[:, 0:32], r.t[:], ident[0:32, 0:32]), reads=[r, CONST], writes=[ps])
            dst_fn(ps)

        with contextlib.ExitStack() as ph:
            scT = S.tile(ph, [128, 32])
            col_from_row(ph, io["c"], lambda ps: S.op(
                "act", lambda e: e.activation(out=scT.t[:], in_=ps.t[:, 0:32], func=AF.Silu), reads=[ps], writes=[scT]))
            bc = S.tile(ph, [1, 6 * D])
            S.dma("sp", lambda e: e.dma_start(out=bc.t[:], in_=io["b_cond"]), writes=[bc])
            wts = [S.tile(ph, [128, 2048]) for _ in range(3)]
            mrs = [S.tile(ph, [1, 2048]) for _ in range(2)]
            n = 0
            for ng in range(12):
                pbs = [S.ps() for _ in range(4)]
                for kc in range(32):
                    wt = wts[n % 3]
                    n += 1
                    S.dma("sp", lambda e, wt=wt, kc=kc, ng=ng: e.dma_start(
                        out=wt.t[:], in_=io["w_cond"][kc * 128:(kc + 1) * 128, ng * 2048:(ng + 1) * 2048]), writes=[wt])
                    for j in range(4):
                        S.op("pe", lambda e, wt=wt, kc=kc, j=j, pb=pbs[j]: e.matmul(
                            pb.t[0:1, :], lhsT=scT.t[:, kc:kc + 1], rhs=wt.t[:, j * 512:(j + 1) * 512],
                            start=(kc == 0), stop=(kc == 31)), reads=[wt, scT], writes=[pbs[j]])
                mr = mrs[ng % 2]
                for j in range(4):
                    S.op("dve", lambda e, mr=mr, j=j, pb=pbs[j], ng=ng: e.tensor_tensor(
                        out=mr.t[0:1, j * 512:(j + 1) * 512], in0=pb.t[0:1, :],
                        in1=bc.t[0:1, ng * 2048 + j * 512: ng * 2048 + (j + 1) * 512], op=ALU.add),
                        reads=[pbs[j], bc], writes=[mr])
                S.dma("sp", lambda e, mr=mr, ng=ng: e.dma_start(
                    out=io["MODROW"][0:1, ng * 2048:(ng + 1) * 2048], in_=mr.t[:]), reads=[mr], writes=[])
            S.flush()
            for (Ax, SHx, gname, ish, isc) in ((A1, SH1, "norm1_g", 0, 1), (A2, SH2, "norm2_g", 3, 4)):
                gcol = S.tile(ph, [128, 32])
                col_from_row(ph, io[gname], lambda ps, gcol=gcol: S.op(
                    "dve", lambda e: e.tensor_copy(out=gcol.t[:], in_=ps.t[:, 0:32]), reads=[ps], writes=[gcol]))
                col_from_row(ph, io["MODROW"][0:1, isc * D:(isc + 1) * D], lambda ps, gcol=gcol, Ax=Ax: S.op(
                    "dve", lambda e: e.scalar_tensor_tensor(out=Ax.t[:], in0=ps.t[:, 0:32], scalar=1.0, in1=gcol.t[:],
                                                            op0=ALU.add, op1=ALU.mult), reads=[ps, gcol], writes=[Ax]))
                col_from_row(ph, io["MODROW"][0:1, ish * D:(ish + 1) * D], lambda ps, SHx=SHx: S.op(
                    "dve", lambda e: e.tensor_copy(out=SHx.t[:], in_=ps.t[:, 0:32]), reads=[ps], writes=[SHx]))
            S.flush()
        if last_phase <= 0:
            return nc

        def norm_tile(xt, scr, ssq, rstd):
            S.op("act", lambda e: e.activation(out=scr.t[:], in_=xt.t[:], func=AF.Square, accum_out=ssq.t[:, 0:1]),
                 reads=[xt], writes=[scr, ssq])
            S.op("dve", lambda e: e.tensor_scalar(out=rstd.t[:, 0:1], in0=ssq.t[:, 0:1], scalar1=1.0 / D, scalar2=EPS,
                                                  op0=ALU.mult, op1=ALU.add), reads=[ssq], writes=[rstd])
            S.op("act", lambda e: e.activation(out=rstd.t[:, 0:1], in_=rstd.t[:, 0:1], func=AF.Sqrt), reads=[rstd], writes=[rstd])
            S.op("dve", lambda e: e.reciprocal(out=rstd.t[:, 0:1], in_=rstd.t[:, 0:1]), reads=[rstd], writes=[rstd])
            S.op("act", lambda e: e.activation(out=xt.t[:], in_=xt.t[:], func=AF.Copy, scale=rstd.t[:, 0:1]),
                 reads=[xt, rstd], writes=[xt])

        TQ = 1024
        with contextlib.ExitStack() as ph:
            uT = S.tile(ph, [128, 32, TQ], BF16)
            xts = [S.tile(ph, [128, D]) for _ in range(2)]
            scr = S.tile(ph, [128, D], BF16)
            ssq = S.tile(ph, [128, 1]); rstd = S.tile(ph, [128, 1])
            wbs = [S.tile(ph, [128, 32, 256], BF16) for _ in range(2)]
            ots = [S.tile(ph, [128, TQ]) for _ in range(2)]
            nw = 0
            no = 0
            for tq in range(T // TQ):
                for tt in range(TQ // 128):
                    xt = xts[tt % 2]
                    r0 = tq * TQ + tt * 128
                    S.dma("sp", lambda e, xt=xt, r0=r0: e.dma_start(out=xt.t[:], in_=io["x"][r0:r0 + 128, :]), writes=[xt])
                    norm_tile(xt, scr, ssq, rstd)
                    for kg in range(8):
                        ps = S.ps()
                        for k4 in range(4):
                            kc = kg * 4 + k4
                            S.op("pe", lambda e, ps=ps, xt=xt, kc=kc, k4=k4: e.transpose(
                                ps.t[:, k4 * 128:(k4 + 1) * 128], xt.t[:, kc * 128:(kc + 1) * 128], ident),
                                reads=[xt, CONST], writes=[ps])
                        for k4 in range(4):
                            kc = kg * 4 + k4
                            if k4 % 2 == 0:
                                S.op("dve", lambda e, ps=ps, kc=kc, k4=k4, tt=tt: e.tensor_scalar(
                                    out=uT.t[:, kc, tt * 128:(tt + 1) * 128], in0=ps.t[:, k4 * 128:(k4 + 1) * 128],
                                    scalar1=A1.t[:, kc:kc + 1], scalar2=SH1.t[:, kc:kc + 1], op0=ALU.mult, op1=ALU.add),
                                    reads=[ps, A1, SH1], writes=[uT])
                            else:
                                S.op("act", lambda e, ps=ps, kc=kc, k4=k4, tt=tt: e.activation(
                                    out=uT.t[:, kc, tt * 128:(tt + 1) * 128], in_=ps.t[:, k4 * 128:(k4 + 1) * 128],
                                    func=AF.Identity, scale=A1.t[:, kc:kc + 1], bias=SH1.t[:, kc:kc + 1]),
                                    reads=[ps, A1, SH1], writes=[uT])
                for cg in range(38):
                    c0 = cg * 256
                    ncol = min(256, NIN - c0)
                    wb = wbs[nw % 2]
                    nw += 1
                    S.dma("pool", lambda e, wb=wb, c0=c0, ncol=ncol: e.dma_start(
                        out=wb.t[:, :, 0:ncol], in_=io["w_in"][:, c0:c0 + ncol].rearrange("(k p) j -> p k j", p=128)),
                        writes=[wb])
                    for sc in range((ncol + 127) // 128):
                        m = min(128, ncol - sc * 128)
                        ot = ots[no % 2]
                        no += 1
                        for tg in range(TQ // 512):
                            ps = S.ps()
                            for kc in range(32):
                                S.op("pe", lambda e, ps=ps, wb=wb, kc=kc, sc=sc, m=m, tg=tg: e.matmul(
                                    ps.t[0:m, :], lhsT=wb.t[:, kc, sc * 128:sc * 128 + m], rhs=uT.t[:, kc, tg * 512:(tg + 1) * 512],
                                    start=(kc == 0), stop=(kc == 31)), reads=[wb, uT], writes=[ps])
                            if tg % 2 == 0:
                                S.op("dve", lambda e, ps=ps, ot=ot, m=m, tg=tg: e.tensor_copy(
                                    out=ot.t[0:m, tg * 512:(tg + 1) * 512], in_=ps.t[0:m, :]), reads=[ps], writes=[ot])
                            else:
                                S.op("act", lambda e, ps=ps, ot=ot, m=m, tg=tg: e.activation(
                                    out=ot.t[0:m, tg * 512:(tg + 1) * 512], in_=ps.t[0:m, :], func=AF.Copy), reads=[ps], writes=[ot])
                        S.dma("sp", lambda e, ot=ot, m=m, c0=c0, sc=sc, tq=tq: e.dma_start(
                            out=io["PT"][c0 + sc * 128:c0 + sc * 128 + m, tq * TQ:(tq + 1) * TQ], in_=ot.t[0:m, :]),
                            reads=[ot], writes=[])
            S.flush()
        if last_phase <= 1:
            return nc
        return build_rest(nc, S, gs, io, out, CONST, ident, A1, SH1, A2, SH2, norm_tile, last_phase)


def build_rest(nc, S, gs, io, out, CONST, ident, A1, SH1, A2, SH2, norm_tile, last_phase):
    io["_norm_tile"] = norm_tile
    phase_rwkv(nc, S, io, CONST, ident)
    if last_phase <= 2:
        return nc
    phase_attn(nc, S, io, CONST, ident)
    if last_phase <= 3:
        return nc
    phase_wout(nc, S, io, CONST, ident, A2, SH2, norm_tile, gs)
    if last_phase <= 4:
        return nc
    phase_moe(nc, S, io, CONST, ident)
    if last_phase <= 5:
        return nc
    phase_final(nc, S, io, out)
    return nc


def cview(CONST, c0, n, parts=64):
    return CONST.t[0:parts, c0:c0 + n]


def bc3(ap2, nh, last=True):
    p, n = ap2.shape
    if last:
        return ap2.unsqueeze(1).broadcast_to([p, nh, n])
    return ap2.unsqueeze(2).broadcast_to([p, n, nh])


def phase_rwkv(nc, S, io, CONST, ident):
    NH, NT, C = 8, 128, 64
    NCH = NT // C
    id64 = CONST.t[0:64, C_ID:C_ID + 64]
    ones64 = CONST.t[0:64, C_ONES:C_ONES + 64]
    ML = bc3(cview(CONST, C_ML, 64), NH)
    MU = bc3(cview(CONST, C_MU, 64), NH)
    MUI = bc3(cview(CONST, C_MUI, 64), NH)
    ID4 = bc3(id64, NH)
    RM = CONST.t[0:64, C_RM:C_RM + NH * NT]
    PT = io["PT"]
    mu = io["rwkv_mu"]
    with contextlib.ExitStack() as ph:
        def tl(shape, dt=F32):
            return S.tile(ph, shape, dt)

        def col_from(src_ap, rows, cols, dst):
            r = tl([rows, cols])
            S.dma("sp", lambda e: e.dma_start(out=r.t[:], in_=src_ap), writes=[r])
            ps = S.ps()
            S.op("pe", lambda e: e.transpose(ps.t[0:cols, 0:rows], r.t[:], ident[0:rows, 0:rows]), reads=[r, CONST], writes=[ps])
            S.op("dve", lambda e: e.tensor_copy(out=dst.t[:], in_=ps.t[0:cols, 0:rows]), reads=[ps], writes=[dst])

        def headcol(src_row):
            d = tl([64, 32])
            col_from(src_row.rearrange("o (h j) -> (o h) j", j=64), 32, 64, d)
            return d

        MUR = headcol(mu[0:1, 0:2048]); MUK = headcol(mu[0:1, 2048:4096]); MUV = headcol(mu[0:1, 4096:6144])
        W0 = headcol(io["rwkv_w0"]); A0 = headcol(io["rwkv_a0"]); KK_ = headcol(io["rwkv_k_k"]); KA = headcol(io["rwkv_k_a"])
        RK_ = headcol(io["rwkv_r_k"]); LNW = headcol(io["rwkv_lnx_w"]); LNB = headcol(io["rwkv_lnx_b"])
        NW0 = tl([64, 32])
        S.op("dve", lambda e: e.tensor_scalar(out=NW0.t[:], in0=W0.t[:], scalar1=-1.0, scalar2=None, op0=ALU.mult), reads=[W0], writes=[NW0])
        MUW = tl([96, 1]); col_from(mu[0:1, 6144:6240], 1, 96, MUW)
        MUA = tl([96, 1]); col_from(mu[0:1, 6240:6336], 1, 96, MUA)
        MUG = tl([128, 2]); col_from(mu[0:1, 6336:6592].rearrange("o (k p) -> (o k) p", p=128), 2, 128, MUG)
        WUP = tl([96, DR]); AUP = tl([96, DR]); GUP = tl([128, 2, DR])
        S.dma("sp", lambda e: e.dma_start(out=WUP.t[:], in_=io["rwkv_w_up"]), writes=[WUP])
        S.dma("sp", lambda e: e.dma_start(out=AUP.t[:], in_=io["rwkv_a_up"]), writes=[AUP])
        S.dma("sp", lambda e: e.dma_start(out=GUP.t[:], in_=io["rwkv_g_up"].rearrange("(k p) c -> p k c", p=128)), writes=[GUP])
        ST = tl([64, 32, 64])
        S.op("pool", lambda e: e.memset(ST.t[:], 0.0), writes=[ST])

        TW = tl([96, NT]); XA = tl([96, NT]); SG = tl([128, 2, NT])
        lc = tl([128, 2, NT]); lp = tl([128, 2, NT])
        big = lambda: tl([64, NH, NT])
        R = big(); K = big(); V = big(); PV = big(); E = big(); A = big(); G_ = big(); KKt = big(); NR = big()
        K2 = big(); B = big(); L = big(); GAM = big(); GI = big(); BON = big(); YN = big()
        GP = PV; KKD = KKt; BI = B; KI = K2; RD = R
        VT = tl([64, NCH, NH, 64]); BIT = tl([64, NCH, NH, 64]); KIT = tl([64, NCH, NH, 64])
        sm = lambda: tl([64, NH, 64])
        M = [[sm(), sm()] for _ in range(NCH)]; MT = [[sm(), sm()] for _ in range(NCH)]
        TT = [sm() for _ in range(NCH)]; BTm = [sm() for _ in range(NCH)]; PTm = [sm() for _ in range(NCH)]; QTm = [sm() for _ in range(NCH)]
        YS = [sm() for _ in range(NCH)]; XS = sm(); US = sm(); SQ = sm(); TMP = sm()
        s1 = tl([64, NH]); s2 = tl([64, NH]); mean = tl([64, NH]); rstd = tl([64, NH])

        def load_mix(dst, cur, prev, rows, t0, parts, nh, mucol):
            S.dma("sp", lambda e: e.dma_start(out=cur.t[0:parts, 0:nh, :], in_=rows(t0, t0 + NT)), writes=[cur])
            if t0 == 0:
                S.op("pool", lambda e: e.memset(prev.t[0:parts, 0:nh, 0:1], 0.0), writes=[prev])
                S.dma("sp", lambda e: e.dma_start(out=prev.t[0:parts, 0:nh, 1:NT], in_=rows(0, NT - 1)), writes=[prev])
            else:
                S.dma("sp", lambda e: e.dma_start(out=prev.t[0:parts, 0:nh, :], in_=rows(t0 - 1, t0 + NT - 1)), writes=[prev])
            S.op("pool", lambda e: e.tensor_tensor(out=prev.t[0:parts, 0:nh, :], in0=prev.t[0:parts, 0:nh, :],
                                                   in1=cur.t[0:parts, 0:nh, :], op=ALU.subtract), reads=[cur, prev], writes=[prev])
            for h in range(nh):
                S.op("dve", lambda e, h=h: e.scalar_tensor_tensor(
                    out=dst.t[0:parts, h, :], in0=prev.t[0:parts, h, :], scalar=mucol(h), in1=cur.t[0:parts, h, :],
                    op0=ALU.mult, op1=ALU.add), reads=[prev, cur], writes=[dst])

        for tq in range(T // NT):
            t0 = tq * NT
            load_mix(lp, lc, lp, lambda a, b: PT[6144:6240, a:b].unsqueeze(1), t0, 96, 1, lambda h: MUW.t[:, 0:1])
            S.op("act", lambda e: e.activation(out=TW.t[:], in_=lp.t[0:96, 0, :], func=AF.Tanh), reads=[lp], writes=[TW])
            load_mix(lp, lc, lp, lambda a, b: PT[6240:6336, a:b].unsqueeze(1), t0, 96, 1, lambda h: MUA.t[:, 0:1])
            S.op("act", lambda e: e.activation(out=XA.t[:], in_=lp.t[0:96, 0, :], func=AF.Copy), reads=[lp], writes=[XA])
            load_mix(lp, lc, lp, lambda a, b: PT[6336:6592, a:b].rearrange("(k p) t -> p k t", p=128), t0, 128, 2,
                     lambda h: MUG.t[:, h:h + 1])
            S.op("act", lambda e: e.activation(out=SG.t[:], in_=lp.t[:], func=AF.Sigmoid), reads=[lp], writes=[SG])
            for hg in range(32 // NH):
                H0 = hg * NH
                c0 = H0 * 64
                hv = lambda base: (lambda a, b: PT[base + c0:base + c0 + NH * 64, a:b].rearrange("(h j) t -> j h t", j=64))
                load_mix(R, GAM, PV, hv(0), t0, 64, NH, lambda h: MUR.t[:, H0 + h:H0 + h + 1])
                load_mix(K, GI, PV, hv(2048), t0, 64, NH, lambda h: MUK.t[:, H0 + h:H0 + h + 1])
                load_mix(V, NR, PV, hv(4096), t0, 64, NH, lambda h: MUV.t[:, H0 + h:H0 + h + 1])
                for h in range(NH):
                    H = H0 + h
                    cs = slice(H * 64, H * 64 + 64)
                    p1 = S.ps()
                    S.op("pe", lambda e, p1=p1, cs=cs: e.matmul(p1.t[0:64, 0:NT], lhsT=WUP.t[:, cs], rhs=TW.t[:], start=True, stop=True),
                         reads=[WUP, TW], writes=[p1])
                    S.op("act", lambda e, p1=p1, h=h, H=H: e.activation(out=E.t[:, h, :], in_=p1.t[0:64, 0:NT], func=AF.Exp,
                                                                       scale=-1.0, bias=NW0.t[:, H:H + 1]), reads=[p1, NW0], writes=[E])
                    S.op("pool", lambda e, h=h, H=H: e.tensor_scalar(out=KKt.t[:, h, :], in0=K.t[:, h, :], scalar1=KK_.t[:, H:H + 1],
                                                                    scalar2=None, op0=ALU.mult), reads=[K, KK_], writes=[KKt])
                for h in range(NH):
                    H = H0 + h
                    cs = slice(H * 64, H * 64 + 64)
                    p3 = S.ps()
                    for kc in range(2):
                        S.op("pe", lambda e, p3=p3, cs=cs, kc=kc: e.matmul(p3.t[0:64, 0:NT], lhsT=GUP.t[:, kc, cs], rhs=SG.t[:, kc, :],
                                                                         start=(kc == 0), stop=(kc == 1)), reads=[GUP, SG], writes=[p3])
                    S.op("dve", lambda e, p3=p3, h=h: e.tensor_copy(out=G_.t[:, h, :], in_=p3.t[0:64, 0:NT]), reads=[p3], writes=[G_])
                S.op("act", lambda e: e.activation(out=E.t[:], in_=E.t[:], func=AF.Ln, bias=1.0), reads=[E], writes=[E])
                S.op("act", lambda e: e.activation(out=E.t[:], in_=E.t[:], func=AF.Exp, scale=-1.0, bias=-0.5), reads=[E], writes=[E])
                for h in range(NH):
                    H = H0 + h
                    cs = slice(H * 64, H * 64 + 64)
                    p2 = S.ps()
                    S.op("pe", lambda e, p2=p2, cs=cs: e.matmul(p2.t[0:64, 0:NT], lhsT=AUP.t[:, cs], rhs=XA.t[:], start=True, stop=True),
                         reads=[AUP, XA], writes=[p2])
                    S.op("act", lambda e, p2=p2, h=h, H=H: e.activation(out=A.t[:, h, :], in_=p2.t[0:64, 0:NT], func=AF.Sigmoid,
                                                                       bias=A0.t[:, H:H + 1]), reads=[p2, A0], writes=[A])
                S.op("pool", lambda e: e.tensor_tensor(out=NR.t[:], in0=KKt.t[:], in1=KKt.t[:], op=ALU.mult), reads=[KKt], writes=[NR])
                for h in range(NH):
                    p1 = S.ps()
                    S.op("pe", lambda e, p1=p1, h=h: e.matmul(p1.t[0:64, 0:NT], lhsT=ones64, rhs=NR.t[:, h, :], start=True, stop=True),
                         reads=[NR, CONST], writes=[p1])
                    S.op("dve", lambda e, p1=p1, h=h: e.tensor_scalar(out=L.t[:, h, :], in0=p1.t[0:64, 0:NT], scalar1=1e-19, scalar2=None, op0=ALU.max),
                         reads=[p1], writes=[L])
                S.op("act", lambda e: e.activation(out=L.t[:], in_=L.t[:], func=AF.Ln), reads=[L], writes=[L])
                S.op("act", lambda e: e.activation(out=NR.t[:], in_=L.t[:], func=AF.Exp, scale=-0.5), reads=[L], writes=[NR])
                S.op("dve", lambda e: e.tensor_tensor(out=KKt.t[:], in0=KKt.t[:], in1=NR.t[:], op=ALU.mult), reads=[KKt, NR], writes=[KKt])
                for h in range(NH):
                    H = H0 + h
                    S.op("dve", lambda e, h=h, H=H: e.tensor_scalar(out=K2.t[:, h, :], in0=A.t[:, h, :], scalar1=-1.0, scalar2=KA.t[:, H:H + 1],
                                                                   op0=ALU.add, op1=ALU.mult), reads=[A, KA], writes=[K2])
                S.op("dve", lambda e: e.scalar_tensor_tensor(out=K2.t[:], in0=K2.t[:], scalar=1.0, in1=K.t[:], op0=ALU.add, op1=ALU.mult),
                     reads=[K2, K], writes=[K2])
                S.op("pool", lambda e: e.tensor_tensor(out=B.t[:], in0=KKt.t[:], in1=A.t[:], op=ALU.mult), reads=[KKt, A], writes=[B])
                S.op("pool", lambda e: e.tensor_tensor(out=BON.t[:], in0=R.t[:], in1=K2.t[:], op=ALU.mult), reads=[R, K2], writes=[BON])
                for h in range(NH):
                    H = H0 + h
                    S.op("dve", lambda e, h=h, H=H: e.tensor_scalar(out=BON.t[:, h, :], in0=BON.t[:, h, :], scalar1=RK_.t[:, H:H + 1],
                                                                   scalar2=None, op0=ALU.mult), reads=[BON, RK_], writes=[BON])
                    p1 = S.ps()
                    S.op("pe", lambda e, p1=p1, h=h: e.matmul(p1.t[0:64, 0:NT], lhsT=ones64, rhs=BON.t[:, h, :], start=True, stop=True),
                         reads=[BON, CONST], writes=[p1])
                    S.op("dve", lambda e, p1=p1, h=h: e.tensor_tensor(out=BON.t[:, h, :], in0=p1.t[0:64, 0:NT], in1=V.t[:, h, :], op=ALU.mult),
                         reads=[p1, V, BON], writes=[BON])
                S.op("dve", lambda e: e.tensor_tensor_scan(out=L.t[:].rearrange("p h t -> p (h t)"), data0=RM,
                                                           data1=E.t[:].rearrange("p h t -> p (h t)"), initial=0.0,
                                                           op0=ALU.mult, op1=ALU.subtract), reads=[E, CONST, L], writes=[L])
                S.op("act", lambda e: e.activation(out=GAM.t[:], in_=L.t[:], func=AF.Exp), reads=[L], writes=[GAM])
                S.op("act", lambda e: e.activation(out=GI.t[:], in_=L.t[:], func=AF.Exp, scale=-1.0), reads=[L], writes=[GI])
                S.op("pool", lambda e: e.tensor_tensor(out=GP.t[:], in0=L.t[:], in1=E.t[:], op=ALU.add), reads=[L, E], writes=[GP])
                S.op("act", lambda e: e.activation(out=GP.t[:], in_=GP.t[:], func=AF.Exp), reads=[GP], writes=[GP])
                S.op("dve", lambda e: e.tensor_tensor(out=KKD.t[:], in0=KKt.t[:], in1=GP.t[:], op=ALU.mult), reads=[KKt, GP], writes=[KKD])
                S.op("pool", lambda e: e.tensor_tensor(out=BI.t[:], in0=B.t[:], in1=GI.t[:], op=ALU.mult), reads=[B, GI], writes=[BI])
                S.op("dve", lambda e: e.tensor_tensor(out=KI.t[:], in0=K2.t[:], in1=GI.t[:], op=ALU.mult), reads=[K2, GI], writes=[KI])
                S.op("pool", lambda e: e.tensor_tensor(out=RD.t[:], in0=R.t[:], in1=GAM.t[:], op=ALU.mult), reads=[R, GAM], writes=[RD])
                for (src, dst) in ((V, VT), (BI, BIT), (KI, KIT)):
                    for c in range(NCH):
                        pt = S.ps()
                        for h in range(NH):
                            S.op("pe", lambda e, pt=pt, src=src, c=c, h=h: e.transpose(
                                pt.t[0:64, h * 64:(h + 1) * 64], src.t[:, h, c * C:(c + 1) * C], id64), reads=[src, CONST], writes=[pt])
                        S.op("act" if c % 2 else "dve", (lambda e, pt=pt, dst=dst, c=c: e.activation(
                            out=dst.t[:, c, :, :], in_=pt.t[0:64, 0:NH * 64].rearrange("p (h i) -> p h i", h=NH), func=AF.Copy)) if c % 2 else
                            (lambda e, pt=pt, dst=dst, c=c: e.tensor_copy(
                                out=dst.t[:, c, :, :], in_=pt.t[0:64, 0:NH * 64].rearrange("p (h i) -> p h i", h=NH))),
                            reads=[pt], writes=[dst])

                def mm4(lhs_fn, rhs_fn, reads):
                    p = S.ps()
                    for h in range(NH):
                        l_ = lhs_fn(h)
                        r_ = rhs_fn(h)
                        S.op("pe", lambda e, p=p, h=h, l_=l_, r_=r_: e.matmul(p.t[0:64, h * 64:(h + 1) * 64], lhsT=l_, rhs=r_, start=True, stop=True),
                             reads=reads, writes=[p])
                    return p

                def pv(p):
                    return p.t[0:64, 0:NH * 64].rearrange("p (h i) -> p h i", h=NH)

                tcs = [slice(c * C, (c + 1) * C) for c in range(NCH)]
                for c in range(NCH):
                    tc = tcs[c]
                    p = mm4(lambda h: KKD.t[:, h, tc], lambda h: BI.t[:, h, tc], [KKD, BI])
                    S.op("dve", lambda e, p=p, c=c: e.tensor_tensor(out=M[c][0].t[:], in0=pv(p), in1=ML, op=ALU.mult), reads=[p, CONST], writes=[M[c][0]])
                    p = mm4(lambda h: BI.t[:, h, tc], lambda h: KKD.t[:, h, tc], [KKD, BI])
                    S.op("dve", lambda e, p=p, c=c: e.tensor_tensor(out=MT[c][0].t[:], in0=pv(p), in1=MU, op=ALU.mult), reads=[p, CONST], writes=[MT[c][0]])
                    S.op("pool", lambda e, c=c: e.tensor_tensor(out=TT[c].t[:], in0=ID4, in1=MT[c][0].t[:], op=ALU.subtract), reads=[MT[c][0], CONST], writes=[TT[c]])
                    p = mm4(lambda h: KI.t[:, h, tc], lambda h: KKD.t[:, h, tc], [KKD, KI])
                    S.op("dve", lambda e, p=p, c=c: e.tensor_tensor(out=BTm[c].t[:], in0=pv(p), in1=MU, op=ALU.mult), reads=[p, CONST], writes=[BTm[c]])
                    p = mm4(lambda h: BI.t[:, h, tc], lambda h: RD.t[:, h, tc], [RD, BI])
                    S.op("dve", lambda e, p=p, c=c: e.tensor_tensor(out=PTm[c].t[:], in0=pv(p), in1=MUI, op=ALU.mult), reads=[p, CONST], writes=[PTm[c]])
                    p = mm4(lambda h: KI.t[:, h, tc], lambda h: RD.t[:, h, tc], [RD, KI])
                    S.op("dve", lambda e, p=p, c=c: e.tensor_tensor(out=QTm[c].t[:], in0=pv(p), in1=MUI, op=ALU.mult), reads=[p, CONST], writes=[QTm[c]])
                cur = 0
                for lvl in range(5):
                    nxt = 1 - cur
                    for c in range(NCH):
                        Mc, MTc, Mn = M[c][cur], MT[c][cur], M[c][nxt]
                        p = mm4(lambda h, MTc=MTc: MTc.t[:, h, :], lambda h, Mc=Mc: Mc.t[:, h, :], [Mc, MTc])
                        S.op("act", lambda e, p=p, Mn=Mn: e.activation(out=Mn.t[:], in_=pv(p), func=AF.Copy), reads=[p], writes=[Mn])
                    if lvl < 4:
                        for c in range(NCH):
                            Mc, MTc, MTn = M[c][cur], MT[c][cur], MT[c][nxt]
                            p = mm4(lambda h, Mc=Mc: Mc.t[:, h, :], lambda h, MTc=MTc: MTc.t[:, h, :], [Mc, MTc])
                            if c % 2 == 0:
                                S.op("act", lambda e, p=p, MTn=MTn: e.activation(out=MTn.t[:], in_=pv(p), func=AF.Copy), reads=[p], writes=[MTn])
                            else:
                                S.op("dve", lambda e, p=p, MTn=MTn: e.tensor_copy(out=MTn.t[:], in_=pv(p)), reads=[p], writes=[MTn])
                    for c in range(NCH):
                        Mn = M[c][nxt]
                        TTc = TT[c]
                        p = mm4(lambda h, Mn=Mn: Mn.t[:, h, :], lambda h, TTc=TTc: TTc.t[:, h, :], [Mn, TTc])
                        S.op("dve", lambda e, p=p, TTc=TTc: e.tensor_tensor(out=TTc.t[:], in0=pv(p), in1=TTc.t[:], op=ALU.add), reads=[p, TTc], writes=[TTc])
                    cur = nxt

                def gn_out(c):
                    tc = tcs[c]
                    Y_ = YS[c]
                    S.op("dve", lambda e: e.tensor_reduce(out=s1.t[:], in_=Y_.t[:], axis=AX.X, op=ALU.add), reads=[Y_], writes=[s1])
                    S.op("pool", lambda e: e.tensor_tensor(out=SQ.t[:], in0=Y_.t[:], in1=Y_.t[:], op=ALU.mult), reads=[Y_], writes=[SQ])
                    S.op("dve", lambda e: e.tensor_reduce(out=s2.t[:], in_=SQ.t[:], axis=AX.X, op=ALU.add), reads=[SQ], writes=[s2])
                    S.op("dve", lambda e: e.tensor_scalar(out=mean.t[:], in0=s1.t[:], scalar1=1.0 / 64, scalar2=None, op0=ALU.mult), reads=[s1], writes=[mean])
                    S.op("dve", lambda e: e.tensor_tensor(out=s1.t[:], in0=mean.t[:], in1=mean.t[:], op=ALU.mult), reads=[mean], writes=[s1])
                    S.op("dve", lambda e: e.scalar_tensor_tensor(out=rstd.t[:], in0=s2.t[:], scalar=1.0 / 64, in1=s1.t[:], op0=ALU.mult, op1=ALU.subtract),
                         reads=[s2, s1], writes=[rstd])
                    S.op("dve", lambda e: e.tensor_scalar(out=rstd.t[:], in0=rstd.t[:], scalar1=GN_EPS, scalar2=None, op0=ALU.add), reads=[rstd], writes=[rstd])
                    S.op("act", lambda e: e.activation(out=rstd.t[:], in_=rstd.t[:], func=AF.Ln), reads=[rstd], writes=[rstd])
                    S.op("act", lambda e: e.activation(out=rstd.t[:], in_=rstd.t[:], func=AF.Exp, scale=-0.5), reads=[rstd], writes=[rstd])
                    S.op("pool", lambda e: e.tensor_tensor(out=Y_.t[:], in0=Y_.t[:], in1=bc3(mean.t[:, :], 64, last=False), op=ALU.subtract),
                         reads=[Y_, mean], writes=[Y_])
                    S.op("pool", lambda e: e.tensor_tensor(out=Y_.t[:], in0=Y_.t[:], in1=bc3(rstd.t[:, :], 64, last=False), op=ALU.mult),
                         reads=[Y_, rstd], writes=[Y_])
                    pt = S.ps()
                    for h in range(NH):
                        S.op("pe", lambda e, pt=pt, h=h: e.transpose(pt.t[0:64, h * 64:(h + 1) * 64], Y_.t[:, h, :], id64), reads=[Y_, CONST], writes=[pt])
                    for h in range(NH):
                        H = H0 + h
                        S.op("act", lambda e, pt=pt, h=h, H=H: e.activation(out=YN.t[:, h, tc], in_=pt.t[0:64, h * 64:(h + 1) * 64], func=AF.Identity,
                                                                           scale=LNW.t[:, H:H + 1], bias=LNB.t[:, H:H + 1]), reads=[pt, LNW, LNB], writes=[YN])

                for c in range(NCH):
                    tc = tcs[c]
                    p = S.ps()
                    for h in range(NH):
                        S.op("pe", lambda e, p=p, h=h: e.matmul(p.t[0:64, h * 64:(h + 1) * 64], lhsT=KKD.t[:, h, tc], rhs=ST.t[:, H0 + h, :],
                                                              start=True, stop=False), reads=[KKD, ST], writes=[p])
                        S.op("pe", lambda e, p=p, h=h: e.matmul(p.t[0:64, h * 64:(h + 1) * 64], lhsT=BTm[c].t[:, h, :], rhs=VT.t[:, c, h, :],
                                                              start=False, stop=True), reads=[BTm[c], VT], writes=[p])
                    S.op("act", lambda e, p=p: e.activation(out=XS.t[:], in_=pv(p), func=AF.Copy), reads=[p], writes=[XS])
                    TTc = TT[c]
                    p = mm4(lambda h: TTc.t[:, h, :], lambda h: XS.t[:, h, :], [TTc, XS])
                    S.op("dve", lambda e, p=p: e.tensor_scalar(out=US.t[:], in0=pv(p), scalar1=-1.0, scalar2=None, op0=ALU.mult), reads=[p], writes=[US])
                    p = S.ps()
                    for h in range(NH):
                        o = p.t[0:64, h * 64:(h + 1) * 64]
                        S.op("pe", lambda e, o=o, h=h: e.matmul(o, lhsT=BIT.t[:, c, h, :], rhs=US.t[:, h, :], start=True, stop=False),
                             reads=[BIT, US], writes=[p])
                        S.op("pe", lambda e, o=o, h=h: e.matmul(o, lhsT=KIT.t[:, c, h, :], rhs=VT.t[:, c, h, :], start=False, stop=True),
                             reads=[KIT, VT], writes=[p])
                    py = S.ps()
                    for h in range(NH):
                        o = py.t[0:64, h * 64:(h + 1) * 64]
                        S.op("pe", lambda e, o=o, h=h: e.matmul(o, lhsT=RD.t[:, h, tc], rhs=ST.t[:, H0 + h, :], start=True, stop=False),
                             reads=[RD, ST], writes=[py])
                        S.op("pe", lambda e, o=o, h=h: e.matmul(o, lhsT=PTm[c].t[:, h, :], rhs=US.t[:, h, :], start=False, stop=False),
                             reads=[PTm[c], US], writes=[py])
                        S.op("pe", lambda e, o=o, h=h: e.matmul(o, lhsT=QTm[c].t[:, h, :], rhs=VT.t[:, c, h, :], start=False, stop=True),
                             reads=[QTm[c], VT], writes=[py])
                    S.op("dve", lambda e, p=p: e.tensor_tensor(out=TMP.t[:], in0=pv(p), in1=ST.t[:, H0:H0 + NH, :], op=ALU.add),
                         reads=[p, ST], writes=[TMP])
                    S.op("dve", lambda e, c=c: e.tensor_tensor(out=ST.t[:, H0:H0 + NH, :], in0=TMP.t[:],
                                                               in1=GAM.t[:, :, c * C + C - 1:c * C + C].broadcast_to([64, NH, 64]), op=ALU.mult),
                         reads=[TMP, GAM], writes=[ST])
                    S.op("act", lambda e, py=py, c=c: e.activation(out=YS[c].t[:], in_=pv(py), func=AF.Copy), reads=[py], writes=[YS[c]])
                    if c > 0:
                        gn_out(c - 1)
                gn_out(NCH - 1)
                S.op("dve", lambda e: e.tensor_tensor(out=YN.t[:], in0=YN.t[:], in1=BON.t[:], op=ALU.add), reads=[YN, BON], writes=[YN])
                S.op("dve", lambda e: e.tensor_tensor(out=YN.t[:], in0=YN.t[:], in1=G_.t[:], op=ALU.mult), reads=[YN, G_], writes=[YN])
                S.dma("sp", lambda e, c0=c0, t0=t0: e.dma_start(
                    out=io["YT"][c0:c0 + NH * 64, t0:t0 + NT].rearrange("(h i) t -> i h t", i=64), in_=YN.t[:]), reads=[YN], writes=[])
        S.flush()


def phase_attn(nc, S, io, CONST, ident):
    TQ = 1024
    NBQ = TQ // 128
    PT = io["PT"]
    QB = NRW
    KB = NRW + 2048
    VB = NRW + 2048 + 512
    with contextlib.ExitStack() as ph:
        def tl(shape, dt=F32):
            return S.tile(ph, shape, dt)

        OG = tl([64, 32])
        r_ = tl([32, 64])
        S.dma("sp", lambda e: e.dma_start(out=r_.t[:], in_=io["attn_out_g"].rearrange("o (h j) -> (o h) j", j=64)), writes=[r_])
        ps = S.ps()
        S.op("pe", lambda e: e.transpose(ps.t[0:64, 0:32], r_.t[:], ident[0:32, 0:32]), reads=[r_, CONST], writes=[ps])
        S.op("dve", lambda e: e.tensor_copy(out=OG.t[:], in_=ps.t[0:64, 0:32]), reads=[ps], writes=[OG])
        SINK = tl([128, 32])
        S.dma("sp", lambda e: e.dma_start(out=SINK.t[:], in_=io["attn_sinks"][0].partition_broadcast(128)), writes=[SINK])

        QT = tl([64, 4, TQ]); YA = tl([64, 4, TQ]); KT = tl([64, TQ + 128]); VT_ = tl([64, TQ + 128])
        AB = tl([128, 4, 256]); AB0 = tl([128, 4, 256])
        SC = tl([128, 4, 256]); PTS = tl([128, 4, 2, 128]); VTM = tl([128, NBQ + 1, 64])
        O = tl([128, 4, 64]); SQ = tl([128, 4, 64])
        mx = tl([128, 4]); nmx = tl([128, 4]); rs = tl([128, 4]); es = tl([128, 4]); ss = tl([128, 4])
        abv = io["attn_bias"].rearrange("p (v h c) -> p v h c", v=2, h=8)
        for h in range(8):
            S.dma("sp", lambda e, h=h: e.dma_start(out=AB.t[:], in_=abv[:, 0, h, :].rearrange("p (g k) -> p g k", g=4)), writes=[AB])
            S.dma("sp", lambda e, h=h: e.dma_start(out=AB0.t[:], in_=abv[:, 1, h, :].rearrange("p (g k) -> p g k", g=4)), writes=[AB0])
            for tq in range(T // TQ):
                t0 = tq * TQ
                S.dma("sp", lambda e, h=h, t0=t0: e.dma_start(
                    out=QT.t[:], in_=PT[QB + h * 256:QB + (h + 1) * 256, t0:t0 + TQ].rearrange("(g d) t -> d g t", d=64)), writes=[QT])
                for (dst, base) in ((KT, KB), (VT_, VB)):
                    if tq == 0:
                        S.op("pool", lambda e, dst=dst: e.memset(dst.t[:, 0:128], 0.0), writes=[dst])
                        S.dma("sp", lambda e, dst=dst, base=base, h=h: e.dma_start(
                            out=dst.t[:, 128:128 + TQ], in_=PT[base + h * 64:base + (h + 1) * 64, 0:TQ]), writes=[dst])
                    else:
                        S.dma("sp", lambda e, dst=dst, base=base, h=h, t0=t0: e.dma_start(
                            out=dst.t[:], in_=PT[base + h * 64:base + (h + 1) * 64, t0 - 128:t0 + TQ]), writes=[dst])
                for half in range(2):
                    blks = list(range(half * 8, min(NBQ + 1, half * 8 + 8)))
                    pv_ = S.ps()
                    for i, bk in enumerate(blks):
                        S.op("pe", lambda e, pv_=pv_, i=i, bk=bk: e.transpose(pv_.t[:, i * 64:(i + 1) * 64], VT_.t[:, bk * 128:(bk + 1) * 128],
                                                                             ident[0:64, 0:64]), reads=[VT_, CONST], writes=[pv_])
                    nb_ = len(blks)
                    S.op("dve", lambda e, pv_=pv_, b0=blks[0], nb_=nb_: e.tensor_copy(
                        out=VTM.t[:, b0:b0 + nb_, :], in_=pv_.t[:, 0:nb_ * 64].rearrange("p (b d) -> p b d", d=64)), reads=[pv_], writes=[VTM])
                for n in range(NBQ):
                    qs = slice(n * 128, (n + 1) * 128)
                    ABn = AB0 if (tq == 0 and n == 0) else AB
                    pss = [S.ps(), S.ps()]
                    for g in range(4):
                        S.op("pe", lambda e, g=g, p=pss[g // 2]: e.matmul(p.t[:, (g % 2) * 256:(g % 2 + 1) * 256], lhsT=QT.t[:, g, qs],
                                                                        rhs=KT.t[:, n * 128:n * 128 + 256], start=True, stop=True),
                             reads=[QT, KT], writes=[pss[g // 2]])
                    for b2 in range(2):
                        S.op("dve", lambda e, b2=b2, p=pss[b2], ABn=ABn: e.scalar_tensor_tensor(
                            out=SC.t[:, 2 * b2:2 * b2 + 2, :], in0=p.t[:, 0:512].rearrange("p (g k) -> p g k", g=2), scalar=0.125,
                            in1=ABn.t[:, 2 * b2:2 * b2 + 2, :], op0=ALU.mult, op1=ALU.add), reads=[pss[b2], ABn], writes=[SC])
                    S.op("dve", lambda e: e.tensor_reduce(out=mx.t[:], in_=SC.t[:], axis=AX.X, op=ALU.max), reads=[SC], writes=[mx])
                    S.op("dve", lambda e, h=h: e.tensor_tensor(out=mx.t[:], in0=mx.t[:], in1=SINK.t[:, 4 * h:4 * h + 4], op=ALU.max),
                         reads=[mx, SINK], writes=[mx])
                    S.op("dve", lambda e: e.tensor_scalar(out=nmx.t[:], in0=mx.t[:], scalar1=-1.0, scalar2=None, op0=ALU.mult), reads=[mx], writes=[nmx])
                    for g in range(4):
                        S.op("act", lambda e, g=g: e.activation(out=SC.t[:, g, :], in_=SC.t[:, g, :], func=AF.Exp, bias=nmx.t[:, g:g + 1],
                                                                accum_out=rs.t[:, g:g + 1]), reads=[SC, nmx], writes=[SC, rs])
                    S.op("dve", lambda e, h=h: e.tensor_tensor(out=es.t[:], in0=SINK.t[:, 4 * h:4 * h + 4], in1=mx.t[:], op=ALU.subtract),
                         reads=[SINK, mx], writes=[es])
                    S.op("act", lambda e: e.activation(out=es.t[:], in_=es.t[:], func=AF.Exp), reads=[es], writes=[es])
                    S.op("dve", lambda e: e.tensor_tensor(out=rs.t[:], in0=rs.t[:], in1=es.t[:], op=ALU.add), reads=[rs, es], writes=[rs])
                    S.op("dve", lambda e: e.reciprocal(out=rs.t[:], in_=rs.t[:]), reads=[rs], writes=[rs])
                    for b2 in range(2):
                        pt = S.ps()
                        for gi in range(2):
                            g = 2 * b2 + gi
                            for kh in range(2):
                                S.op("pe", lambda e, pt=pt, g=g, gi=gi, kh=kh: e.transpose(
                                    pt.t[:, (gi * 2 + kh) * 128:(gi * 2 + kh + 1) * 128], SC.t[:, g, kh * 128:(kh + 1) * 128], ident),
                                    reads=[SC, CONST], writes=[pt])
                        if b2 == 0:
                            S.op("act", lambda e, pt=pt, b2=b2: e.activation(out=PTS.t[:, 2 * b2:2 * b2 + 2, :, :],
                                                                             in_=pt.t[:, 0:512].rearrange("p (g k q) -> p g k q", g=2, k=2), func=AF.Copy),
                                 reads=[pt], writes=[PTS])
                        else:
                            S.op("dve", lambda e, pt=pt, b2=b2: e.tensor_copy(out=PTS.t[:, 2 * b2:2 * b2 + 2, :, :],
                                                                              in_=pt.t[:, 0:512].rearrange("p (g k q) -> p g k q", g=2, k=2)),
                                 reads=[pt], writes=[PTS])
                    po = S.ps()
                    for g in range(4):
                        for kh in range(2):
                            S.op("pe", lambda e, g=g, kh=kh, po=po: e.matmul(po.t[:, g * 64:(g + 1) * 64], lhsT=PTS.t[:, g, kh, :], rhs=VTM.t[:, n + kh, :],
                                                                           start=(kh == 0), stop=(kh == 1)), reads=[PTS, VTM], writes=[po])
                    S.op("dve", lambda e, po=po: e.tensor_tensor(out=O.t[:], in0=po.t[:, 0:256].rearrange("p (g d) -> p g d", g=4),
                                                                 in1=rs.t[:, :].unsqueeze(2).broadcast_to([128, 4, 64]), op=ALU.mult),
                         reads=[po, rs], writes=[O])
                    S.op("pool", lambda e: e.tensor_tensor(out=SQ.t[:], in0=O.t[:], in1=O.t[:], op=ALU.mult), reads=[O], writes=[SQ])
                    S.op("dve", lambda e: e.tensor_reduce(out=ss.t[:], in_=SQ.t[:], axis=AX.X, op=ALU.add), reads=[SQ], writes=[ss])
                    S.op("dve", lambda e: e.tensor_scalar(out=ss.t[:], in0=ss.t[:], scalar1=1.0 / 64, scalar2=EPS, op0=ALU.mult, op1=ALU.add),
                         reads=[ss], writes=[ss])
                    S.op("act", lambda e: e.activation(out=ss.t[:], in_=ss.t[:], func=AF.Sqrt), reads=[ss], writes=[ss])
                    S.op("dve", lambda e: e.reciprocal(out=ss.t[:], in_=ss.t[:]), reads=[ss], writes=[ss])
                    S.op("dve", lambda e: e.tensor_tensor(out=O.t[:], in0=O.t[:], in1=ss.t[:, :].unsqueeze(2).broadcast_to([128, 4, 64]), op=ALU.mult),
                         reads=[O, ss], writes=[O])
                    pT = S.ps()
                    for g in range(4):
                        S.op("pe", lambda e, g=g, pT=pT: e.transpose(pT.t[0:64, g * 128:(g + 1) * 128], O.t[:, g, :], ident), reads=[O, CONST], writes=[pT])
                    for g in range(4):
                        S.op("act", lambda e, g=g, pT=pT, h=h: e.activation(out=YA.t[:, g, qs], in_=pT.t[0:64, g * 128:(g + 1) * 128], func=AF.Copy,
                                                                           scale=OG.t[:, 4 * h + g:4 * h + g + 1]), reads=[pT, OG], writes=[YA])
                S.dma("sp", lambda e, h=h, t0=t0: e.dma_start(
                    out=io["YT"][2048 + h * 256:2048 + (h + 1) * 256, t0:t0 + TQ].rearrange("(g d) t -> d g t", d=64), in_=YA.t[:]),
                    reads=[YA], writes=[])
        S.flush()


def phase_wout(nc, S, io, CONST, ident, A2, SH2, norm_tile, gs=None):
    TQ = 1024
    X1 = io["X1"]
    MOD = io["MODROW"]
    with contextlib.ExitStack() as ph:
        def tl(shape, dt=F32):
            return S.tile(ph, shape, dt)
        G1 = tl([128, D])
        S.dma("sp", lambda e: e.dma_start(out=G1.t[:], in_=MOD[0, 2 * D:3 * D].partition_broadcast(128)), writes=[G1])
        yT = tl([128, 32, TQ], BF16)
        wos = [tl([128, 32, 512], BF16) for _ in range(2)]
        xps = [tl([128, 512]) for _ in range(3)]
        tmps = [tl([128, 512]) for _ in range(2)]
        nw = 0
        nx = 0
        for tq in range(T // TQ):
            t0 = tq * TQ
            for kh in range(4):
                S.dma("pool", lambda e, t0=t0, kh=kh: e.dma_start(
                    out=yT.t[:, kh * 8:(kh + 1) * 8, :],
                    in_=io["YT"][kh * 1024:(kh + 1) * 1024, t0:t0 + TQ].rearrange("(k p) t -> p k t", p=128)), writes=[yT])
            for cg in range(8):
                wo = wos[nw % 2]
                nw += 1
                for kh in range(4):
                    S.dma("pool", lambda e, wo=wo, cg=cg, kh=kh: e.dma_start(
                        out=wo.t[:, kh * 8:(kh + 1) * 8, :],
                        in_=io["w_out"][kh * 1024:(kh + 1) * 1024, cg * 512:(cg + 1) * 512].rearrange("(k p) j -> p k j", p=128)), writes=[wo])
                for tt in range(TQ // 128):
                    r0 = t0 + tt * 128
                    xp = xps[nx % 3]
                    tmp = tmps[nx % 2]
                    nx += 1
                    S.dma("sp", lambda e, xp=xp, r0=r0, cg=cg: e.dma_start(out=xp.t[:], in_=io["x"][r0:r0 + 128, cg * 512:(cg + 1) * 512]), writes=[xp])
                    ps = S.ps()
                    for kc in range(32):
                        S.op("pe", lambda e, ps=ps, wo=wo, kc=kc, tt=tt: e.matmul(
                            ps.t[:, :], lhsT=yT.t[:, kc, tt * 128:(tt + 1) * 128], rhs=wo.t[:, kc, :], start=(kc == 0), stop=(kc == 31)),
                            reads=[yT, wo], writes=[ps])
                    S.op("dve", lambda e, ps=ps, tmp=tmp, cg=cg: e.tensor_tensor(out=tmp.t[:], in0=ps.t[:, :], in1=G1.t[:, cg * 512:(cg + 1) * 512], op=ALU.mult),
                         reads=[ps, G1], writes=[tmp])
                    S.op("pool", lambda e, tmp=tmp, xp=xp: e.tensor_tensor(out=xp.t[:], in0=tmp.t[:], in1=xp.t[:], op=ALU.add), reads=[tmp, xp], writes=[xp])
                    S.dma("sp", lambda e, xp=xp, r0=r0, cg=cg: e.dma_start(out=X1[r0:r0 + 128, cg * 512:(cg + 1) * 512], in_=xp.t[:]), reads=[xp], writes=[])
        S.flush()

    R_ = {}
    io["_route"] = R_
    ms = contextlib.ExitStack()
    io["_moe_stack"] = ms
    io["_IDXG"] = S.tile(ms, [128, NB], I32)
    io["_IDXD"] = S.tile(ms, [128, NB], I32)
    io["_CBF"] = S.tile(ms, [128, 128], BF16)
    gs = contextlib.ExitStack()
    io["_route_stack"] = gs
    R_["OH1"] = S.tile(gs, [128, 32, 64]); R_["OH2"] = S.tile(gs, [128, 32, 64]); R_["RANK"] = S.tile(gs, [128, 32, 64])
    R_["W1"] = S.tile(gs, [128, 32]); R_["W2"] = S.tile(gs, [128, 32]); R_["SELSUM"] = S.tile(gs, [128, 64])
    OH1, OH2, RANK, W1, W2, SELSUM = R_["OH1"], R_["OH2"], R_["RANK"], R_["W1"], R_["W2"], R_["SELSUM"]
    TRI = CONST.t[:, C_TRI:C_TRI + 128]
    ONES = CONST.t[:, C_ONES:C_ONES + 128]
    with contextlib.ExitStack() as ph:
        def tl(shape, dt=F32):
            return S.tile(ph, shape, dt)
        A2R = tl([128, D]); SH2R = tl([128, D])
        xts = [tl([128, D]) for _ in range(2)]
        scr = tl([128, D], BF16)
        ssq = tl([128, 1]); rstd = tl([128, 1])
        S.dma("sp", lambda e: e.dma_start(out=xts[0].t[:], in_=io["norm2_g"][0].partition_broadcast(128)), writes=[xts[0]])
        S.dma("sp", lambda e: e.dma_start(out=A2R.t[:], in_=MOD[0, 4 * D:5 * D].partition_broadcast(128)), writes=[A2R])
        S.dma("sp", lambda e: e.dma_start(out=SH2R.t[:], in_=MOD[0, 3 * D:4 * D].partition_broadcast(128)), writes=[SH2R])
        S.op("dve", lambda e: e.scalar_tensor_tensor(out=A2R.t[:], in0=A2R.t[:], scalar=1.0, in1=xts[0].t[:], op0=ALU.add, op1=ALU.mult),
             reads=[A2R, xts[0]], writes=[A2R])
        RT = tl([128, 32, 72])
        S.dma("sp", lambda e: e.dma_start(out=RT.t[:], in_=io["router"].rearrange("(k p) j -> p k j", p=128)), writes=[RT])
        RB = tl([128, 72])
        S.dma("sp", lambda e: e.dma_start(out=RB.t[:], in_=io["router_bias"][0].partition_broadcast(128)), writes=[RB])
        uTs = [tl([128, 4, 128]) for _ in range(2)]
        LG = tl([128, 72]); T88 = tl([128, 8, 8]); SEL = tl([128, 64])
        mg = tl([128, 1]); nmg = tl([128, 1]); sg = tl([128, 1]); eg = tl([128, 8]); ohg = tl([128, 8]); le = tl([128, 8]); le2 = tl([128, 8])
        m1 = tl([128, 1]); m2 = tl([128, 1]); oh1 = tl([128, 8]); oh2 = tl([128, 8]); dd = tl([128, 1]); w1 = tl([128, 1])
        S.op("pool", lambda e: e.memset(SELSUM.t[:], 0.0), writes=[SELSUM])
        zr = tl([1, D], BF16)
        S.op("pool", lambda e: e.memset(zr.t[:], 0.0), writes=[zr])
        S.dma("sp", lambda e: e.dma_start(out=io["U2"][T:T + 1, :], in_=zr.t[:]), reads=[zr], writes=[])
        nu = 0
        for tt in range(T // 128):
            xt = xts[tt % 2]
            r0 = tt * 128
            S.dma("sp", lambda e, xt=xt, r0=r0: e.dma_start(out=xt.t[:], in_=X1[r0:r0 + 128, :]), writes=[xt])
            norm_tile(xt, scr, ssq, rstd)
            S.op("dve", lambda e, xt=xt: e.tensor_tensor(out=xt.t[:], in0=xt.t[:], in1=A2R.t[:], op=ALU.mult), reads=[xt, A2R], writes=[xt])
            S.op("pool", lambda e, xt=xt: e.tensor_tensor(out=xt.t[:], in0=xt.t[:], in1=SH2R.t[:], op=ALU.add), reads=[xt, SH2R], writes=[xt])
            for hf in range(2):
                S.dma("pool", lambda e, xt=xt, r0=r0, hf=hf: e.dma_start(out=io["U2"][r0:r0 + 128, hf * 2048:(hf + 1) * 2048],
                                                                        in_=xt.t[:, hf * 2048:(hf + 1) * 2048]), reads=[xt], writes=[])
            pl = S.ps(hold=True)
            for kg in range(8):
                pt = S.ps()
                for k4 in range(4):
                    kc = kg * 4 + k4
                    S.op("pe", lambda e, pt=pt, xt=xt, kc=kc, k4=k4: e.transpose(pt.t[:, k4 * 128:(k4 + 1) * 128], xt.t[:, kc * 128:(kc + 1) * 128], ident),
                         reads=[xt, CONST], writes=[pt])
                uT = uTs[nu % 2]
                nu += 1
                if kg % 2 == 0:
                    S.op("dve", lambda e, pt=pt, uT=uT: e.tensor_copy(out=uT.t[:], in_=pt.t[:, :].rearrange("p (k t) -> p k t", k=4)), reads=[pt], writes=[uT])
                else:
                    S.op("act", lambda e, pt=pt, uT=uT: e.activation(out=uT.t[:], in_=pt.t[:, :].rearrange("p (k t) -> p k t", k=4), func=AF.Copy),
                         reads=[pt], writes=[uT])
                for k4 in range(4):
                    kc = kg * 4 + k4
                    S.op("pe", lambda e, pl=pl, uT=uT, kc=kc, k4=k4: e.matmul(pl.t[:, 0:72], lhsT=uT.t[:, k4, :], rhs=RT.t[:, kc, :],
                                                                             start=(kc == 0), stop=(kc == 31)), reads=[uT, RT], writes=[pl])
            S.ps_release(pl)
            S.op("dve", lambda e, pl=pl: e.tensor_tensor(out=LG.t[:], in0=pl.t[:, 0:72], in1=RB.t[:], op=ALU.add), reads=[pl, RB], writes=[LG])
            S.op("dve", lambda e: e.tensor_reduce(out=mg.t[:], in_=LG.t[:, 0:8], axis=AX.X, op=ALU.max), reads=[LG], writes=[mg])
            S.op("dve", lambda e: e.tensor_scalar(out=nmg.t[:], in0=mg.t[:], scalar1=-1.0, scalar2=None, op0=ALU.mult), reads=[mg], writes=[nmg])
            S.op("act", lambda e: e.activation(out=eg.t[:], in_=LG.t[:, 0:8], func=AF.Exp, bias=nmg.t[:, 0:1], accum_out=sg.t[:, 0:1]),
                 reads=[LG, nmg], writes=[eg, sg])
            S.op("dve", lambda e: e.reciprocal(out=sg.t[:], in_=sg.t[:]), reads=[sg], writes=[sg])
            S.op("dve", lambda e: e.tensor_scalar(out=ohg.t[:], in0=LG.t[:, 0:8], scalar1=mg.t[:, 0:1], scalar2=None, op0=ALU.is_equal),
                 reads=[LG, mg], writes=[ohg])
            S.op("dve", lambda e: e.tensor_tensor(out=T88.t[:], in0=LG.t[:, 8:72].rearrange("p (g x) -> p g x", g=8),
                                                  in1=ohg.t[:, :].unsqueeze(2).broadcast_to([128, 8, 8]), op=ALU.mult), reads=[LG, ohg], writes=[T88])
            S.op("dve", lambda e: e.tensor_reduce(out=le.t[:], in_=T88.t[:].rearrange("p g x -> p x g"), axis=AX.X, op=ALU.add), reads=[T88], writes=[le])
            S.op("dve", lambda e: e.tensor_reduce(out=m1.t[:], in_=le.t[:], axis=AX.X, op=ALU.max), reads=[le], writes=[m1])
            S.op("dve", lambda e: e.tensor_scalar(out=oh1.t[:], in0=le.t[:], scalar1=m1.t[:, 0:1], scalar2=None, op0=ALU.is_equal), reads=[le, m1], writes=[oh1])
            S.op("dve", lambda e: e.scalar_tensor_tensor(out=le2.t[:], in0=oh1.t[:], scalar=-1e30, in1=le.t[:], op0=ALU.mult, op1=ALU.add),
                 reads=[oh1, le], writes=[le2])
            S.op("dve", lambda e: e.tensor_reduce(out=m2.t[:], in_=le2.t[:], axis=AX.X, op=ALU.max), reads=[le2], writes=[m2])
            S.op("dve", lambda e: e.tensor_scalar(out=oh2.t[:], in0=le2.t[:], scalar1=m2.t[:, 0:1], scalar2=None, op0=ALU.is_equal), reads=[le2, m2], writes=[oh2])
            S.op("dve", lambda e: e.tensor_tensor(out=dd.t[:], in0=m2.t[:], in1=m1.t[:], op=ALU.subtract), reads=[m1, m2], writes=[dd])
            S.op("act", lambda e: e.activation(out=dd.t[:], in_=dd.t[:], func=AF.Exp), reads=[dd], writes=[dd])
            S.op("dve", lambda e: e.tensor_scalar(out=w1.t[:], in0=dd.t[:], scalar1=1.0, scalar2=None, op0=ALU.add), reads=[dd], writes=[w1])
            S.op("dve", lambda e: e.reciprocal(out=w1.t[:], in_=w1.t[:]), reads=[w1], writes=[w1])
            S.op("dve", lambda e: e.tensor_tensor(out=dd.t[:], in0=dd.t[:], in1=w1.t[:], op=ALU.mult), reads=[dd, w1], writes=[dd])
            S.op("dve", lambda e, tt=tt: e.tensor_tensor(out=W1.t[:, tt:tt + 1], in0=w1.t[:], in1=sg.t[:], op=ALU.mult), reads=[w1, sg], writes=[W1])
            S.op("dve", lambda e, tt=tt: e.tensor_tensor(out=W2.t[:, tt:tt + 1], in0=dd.t[:], in1=sg.t[:], op=ALU.mult), reads=[dd, sg], writes=[W2])
            for (oh, OHA) in ((oh1, OH1), (oh2, OH2)):
                S.op("dve", lambda e, oh=oh, OHA=OHA, tt=tt: e.tensor_tensor(
                    out=OHA.t[:, tt, :].rearrange("p (g x) -> p g x", g=8), in0=ohg.t[:, :].unsqueeze(2).broadcast_to([128, 8, 8]),
                    in1=oh.t[:, :].unsqueeze(1).broadcast_to([128, 8, 8]), op=ALU.mult), reads=[ohg, oh], writes=[OHA])
            S.op("dve", lambda e, tt=tt: e.tensor_tensor(out=SEL.t[:], in0=OH1.t[:, tt, :], in1=OH2.t[:, tt, :], op=ALU.add), reads=[OH1, OH2], writes=[SEL])
            pr = S.ps()
            S.op("pe", lambda e, pr=pr: e.matmul(pr.t[:, 0:64], lhsT=TRI, rhs=SEL.t[:], start=True, stop=False), reads=[SEL, CONST], writes=[pr])
            S.op("pe", lambda e, pr=pr: e.matmul(pr.t[:, 0:64], lhsT=ONES, rhs=SELSUM.t[:], start=False, stop=True), reads=[SELSUM, CONST], writes=[pr])
            S.op("act", lambda e, pr=pr, tt=tt: e.activation(out=RANK.t[:, tt, :], in_=pr.t[:, 0:64], func=AF.Copy), reads=[pr], writes=[RANK])
            S.op("pool", lambda e: e.tensor_tensor(out=SELSUM.t[:], in0=SELSUM.t[:], in1=SEL.t[:], op=ALU.add), reads=[SELSUM, SEL], writes=[SELSUM])
        S.flush()


def phase_moe(nc, S, io, CONST, ident):
    R_ = io["_route"]
    OH1, OH2, RANK, W1, W2, SELSUM = R_["OH1"], R_["OH2"], R_["RANK"], R_["W1"], R_["W2"], R_["SELSUM"]
    ONES = CONST.t[:, C_ONES:C_ONES + 128]
    K128 = CONST.t[:, C_K128:C_K128 + 64]
    PID = CONST.t[:, C_PID:C_PID + 1]
    P128 = CONST.t[:, C_P128:C_P128 + 1]
    ROWTOK = io["ROWTOK"]
    ROWW = io["ROWW"]
    YACC = io["YACC"]
    U2 = io["U2"]
    IOA = bass.IndirectOffsetOnAxis
    with contextlib.ExitStack() as ph:
        def tl(shape, dt=F32):
            return S.tile(ph, shape, dt)
        IDXG = io["_IDXG"]
        CONST_BF = io["_CBF"]
        S.op("dve", lambda e: e.tensor_copy(out=CONST_BF.t[:], in_=ident), reads=[CONST], writes=[CONST_BF])
        IDXD = io["_IDXD"]
        with contextlib.ExitStack() as p2:
            def t2(shape, dt=F32):
                return S.tile(p2, shape, dt)
            cnt = t2([64, 1]); nblk = t2([64, 1]); cmp = t2([64, 64]); pbc = t2([64, 128])
            PST = t2([128, 64]); PEND = t2([128, 64])
            pc = S.ps()
            S.op("pe", lambda e: e.matmul(pc.t[0:64, 0:128], lhsT=SELSUM.t[:], rhs=ONES, start=True, stop=True), reads=[SELSUM, CONST], writes=[pc])
            S.op("dve", lambda e: e.tensor_copy(out=cnt.t[:], in_=pc.t[0:64, 0:1]), reads=[pc], writes=[cnt])
            S.op("dve", lambda e: e.tensor_scalar(out=cmp.t[:], in0=K128[0:64, :], scalar1=cnt.t[:, 0:1], scalar2=None, op0=ALU.is_lt),
                 reads=[cnt, CONST], writes=[cmp])
            S.op("dve", lambda e: e.tensor_reduce(out=nblk.t[:], in_=cmp.t[:], axis=AX.X, op=ALU.add), reads=[cmp], writes=[nblk])
            S.op("dve", lambda e: e.tensor_scalar(out=nblk.t[:], in0=nblk.t[:], scalar1=128.0, scalar2=None, op0=ALU.mult), reads=[nblk], writes=[nblk])
            S.op("dve", lambda e: e.tensor_scalar(out=pbc.t[:], in0=ONES[0:64, :], scalar1=nblk.t[:, 0:1], scalar2=None, op0=ALU.mult),
                 reads=[nblk, CONST], writes=[pbc])
            pp = S.ps()
            S.op("pe", lambda e: e.matmul(pp.t[:, 0:64], lhsT=pbc.t[:], rhs=CONST.t[0:64, C_SU:C_SU + 64], start=True, stop=True), reads=[pbc, CONST], writes=[pp])
            S.op("dve", lambda e: e.tensor_copy(out=PST.t[:], in_=pp.t[:, 0:64]), reads=[pp], writes=[PST])
            pe_ = S.ps()
            S.op("pe", lambda e: e.matmul(pe_.t[:, 0:64], lhsT=pbc.t[:], rhs=CONST.t[0:64, C_SUI:C_SUI + 64], start=True, stop=True), reads=[pbc, CONST], writes=[pe_])
            S.op("dve", lambda e: e.tensor_copy(out=PEND.t[:], in_=pe_.t[:, 0:64]), reads=[pe_], writes=[PEND])
            TMPA = t2([128, 32, 64]); TMPB = t2([128, 32, 64])
            DEST = [t2([128, 32]), t2([128, 32])]
            DESTI = [t2([128, 32], I32), t2([128, 32], I32)]
            S.op("dve", lambda e: e.tensor_tensor(out=TMPA.t[:], in0=RANK.t[:], in1=PST.t[:, :].unsqueeze(1).broadcast_to([128, 32, 64]), op=ALU.add),
                 reads=[RANK, PST], writes=[TMPA])
            for k, OH in enumerate((OH1, OH2)):
                S.op("dve", lambda e, OH=OH: e.tensor_tensor(out=TMPB.t[:], in0=TMPA.t[:], in1=OH.t[:], op=ALU.mult), reads=[TMPA, OH], writes=[TMPB])
                S.op("dve", lambda e, k=k: e.tensor_reduce(out=DEST[k].t[:], in_=TMPB.t[:], axis=AX.X, op=ALU.add), reads=[TMPB], writes=[DEST[k]])
                S.op("dve", lambda e, k=k: e.tensor_copy(out=DESTI[k].t[:], in_=DEST[k].t[:]), reads=[DEST[k]], writes=[DESTI[k]])
            TOK = t2([128, 32])
            S.op("dve", lambda e: e.tensor_scalar(out=TOK.t[:], in0=K128[:, 0:32], scalar1=PID, scalar2=None, op0=ALU.add), reads=[CONST], writes=[TOK])
            TOK16 = t2([128, 32, 16])
            S.op("dve", lambda e: e.tensor_copy(out=TOK16.t[:], in_=TOK.t[:, :].unsqueeze(2).broadcast_to([128, 32, 16])), reads=[TOK], writes=[TOK16])
            W16 = [t2([128, 32, 16]), t2([128, 32, 16])]
            for k, W in enumerate((W1, W2)):
                S.op("dve", lambda e, k=k, W=W: e.tensor_copy(out=W16[k].t[:], in_=W.t[:, :].unsqueeze(2).broadcast_to([128, 32, 16])), reads=[W], writes=[W16[k]])
            FILL = t2([128, 128, 16])
            S.op("pool", lambda e: e.memset(FILL.t[:], float(T)), writes=[FILL])
            S.dma("sp", lambda e: e.dma_start(out=ROWTOK.rearrange("(p r) c -> p r c", p=128), in_=FILL.t[:]), reads=[FILL], writes=[])
            FILL0 = t2([128, 128, 16])
            S.op("pool", lambda e: e.memset(FILL0.t[:], 0.0), writes=[FILL0])
            S.dma("sp", lambda e: e.dma_start(out=ROWW.rearrange("(p r) c -> p r c", p=128), in_=FILL0.t[:]), reads=[FILL0], writes=[])
            S.flush()
            for tt in range(32):
                for k in range(2):
                    S.dma("pool", lambda e, tt=tt, k=k: e.indirect_dma_start(
                        out=ROWTOK, out_offset=IOA(ap=DESTI[k].t[:, tt:tt + 1], axis=0), in_=TOK16.t[:, tt, :], in_offset=None),
                        reads=[DESTI[k], TOK16], writes=[])
                    S.dma("pool", lambda e, tt=tt, k=k: e.indirect_dma_start(
                        out=ROWW, out_offset=IOA(ap=DESTI[k].t[:, tt:tt + 1], axis=0), in_=W16[k].t[:, tt, :], in_offset=None),
                        reads=[DESTI[k], W16[k]], writes=[])
            BE = t2([128, 1]); cmp2 = t2([128, 64]); DG = t2([128, 128]); BEROW = t2([128, 128]); BASE = t2([128, 128])
            S.op("dve", lambda e: e.tensor_scalar(out=cmp2.t[:], in0=PEND.t[:], scalar1=P128, scalar2=None, op0=ALU.is_le), reads=[PEND, CONST], writes=[cmp2])
            S.op("dve", lambda e: e.tensor_reduce(out=BE.t[:], in_=cmp2.t[:], axis=AX.X, op=ALU.add), reads=[cmp2], writes=[BE])
            S.op("dve", lambda e: e.tensor_scalar(out=BE.t[:], in0=BE.t[:], scalar1=63.0, scalar2=None, op0=ALU.min), reads=[BE], writes=[BE])
            EMP = t2([128, 1])
            S.op("dve", lambda e: e.tensor_tensor(out=EMP.t[:], in0=P128, in1=PEND.t[:, 63:64], op=ALU.is_ge), reads=[PEND, CONST], writes=[EMP])
            S.op("dve", lambda e: e.scalar_tensor_tensor(out=BE.t[:], in0=EMP.t[:], scalar=1000.0, in1=BE.t[:], op0=ALU.mult, op1=ALU.add),
                 reads=[EMP, BE], writes=[BE])
            S.op("dve", lambda e: e.tensor_scalar(out=DG.t[:], in0=ident, scalar1=BE.t[:, 0:1], scalar2=None, op0=ALU.mult), reads=[BE, CONST], writes=[DG])
            pb = S.ps()
            S.op("pe", lambda e: e.matmul(pb.t[:, 0:128], lhsT=ONES, rhs=DG.t[:], start=True, stop=True), reads=[DG, CONST], writes=[pb])
            S.op("dve", lambda e: e.tensor_copy(out=BEROW.t[:], in_=pb.t[:, 0:128]), reads=[pb], writes=[BEROW])
            S.op("dve", lambda e: e.tensor_scalar(out=BASE.t[:], in0=BEROW.t[:], scalar1=4096.0, scalar2=PID, op0=ALU.mult, op1=ALU.add),
                 reads=[BEROW, CONST], writes=[BASE])
            S.op("dve", lambda e: e.tensor_copy(out=IDXG.t[:], in_=BASE.t[:]), reads=[BASE], writes=[IDXG])
            S.op("dve", lambda e: e.tensor_scalar(out=BASE.t[:], in0=BEROW.t[:], scalar1=512.0, scalar2=PID, op0=ALU.mult, op1=ALU.add),
                 reads=[BEROW, CONST], writes=[BASE])
            S.op("dve", lambda e: e.tensor_scalar(out=BASE.t[:], in0=BASE.t[:], scalar1=2.0, scalar2=None, op0=ALU.mult), reads=[BASE], writes=[BASE])
            S.op("dve", lambda e: e.tensor_copy(out=IDXD.t[:], in_=BASE.t[:]), reads=[BASE], writes=[IDXD])
            S.flush()
        io["_route_stack"].close()

        WG = [tl([128, 8, 512], BF16) for _ in range(3)]
        WU = [tl([128, 8, 512], BF16) for _ in range(3)]
        WDS = [[tl([128, D], BF16) for _ in range(4)] for _ in range(2)]
        xbs = [tl([128, D], BF16) for _ in range(2)]
        xbT = tl([128, 32, 128], BF16)
        hs = tl([128, 512]); hb = tl([128, 512], BF16); hT = tl([128, 4, 128], BF16)
        yb = tl([128, D])
        TIF = [tl([128, 1]) for _ in range(3)]; RW = [tl([128, 1]) for _ in range(3)]
        TI = [tl([128, 1], I32) for _ in range(3)]; TI2 = [tl([128, 2], I32) for _ in range(3)]; TF2 = [tl([128, 2]) for _ in range(3)]
        yacc_dep = tl([1, 1])
        S.op("pool", lambda e: e.memset(yb.t[:], 0.0), writes=[yb])
        for tt in range(T // 128):
            S.dma("sp", lambda e, tt=tt: e.dma_start(out=YACC[tt * 128:(tt + 1) * 128, :], in_=yb.t[:]), reads=[yb], writes=[yacc_dep])
        S.dma("sp", lambda e: e.dma_start(out=YACC[T:T + 1, :], in_=yb.t[0:1, :]), reads=[yb], writes=[yacc_dep])
        S.flush()
        REG_G = nc.gpsimd.alloc_register("bc_gate")
        nc.gpsimd.reg_mov(REG_G, 64 * D)
        REG_D = nc.gpsimd.alloc_register("bc_down")
        nc.gpsimd.reg_mov(REG_D, 70000)
        EWG = io["ew_gate"]; EWU = io["ew_up"]
        EWD = io["ew_down"].rearrange("r (two c) -> (r two) c", two=2)
        YACC2 = YACC.rearrange("r (two c) -> (r two) c", two=2)
        nw = 0

        def emit_idx(i):
            b = i % 3
            S.dma("sp", lambda e: e.dma_start(out=TIF[b].t[:], in_=ROWTOK[i * 128:(i + 1) * 128, 0:1], allow_slow_non_contiguous=True), writes=[TIF[b]])
            S.dma("sp", lambda e: e.dma_start(out=RW[b].t[:], in_=ROWW[i * 128:(i + 1) * 128, 0:1], allow_slow_non_contiguous=True), writes=[RW[b]])
            S.op("dve", lambda e: e.tensor_copy(out=TI[b].t[:], in_=TIF[b].t[:]), reads=[TIF[b]], writes=[TI[b]])
            S.op("dve", lambda e: e.tensor_scalar(out=TF2[b].t[:, 0:1], in0=TIF[b].t[:], scalar1=2.0, scalar2=None, op0=ALU.mult), reads=[TIF[b]], writes=[TF2[b]])
            S.op("dve", lambda e: e.tensor_scalar(out=TF2[b].t[:, 1:2], in0=TIF[b].t[:], scalar1=2.0, scalar2=1.0, op0=ALU.mult, op1=ALU.add),
                 reads=[TIF[b]], writes=[TF2[b]])
            S.op("dve", lambda e: e.tensor_copy(out=TI2[b].t[:], in_=TF2[b].t[:]), reads=[TF2[b]], writes=[TI2[b]])
            xb = xbs[i % 2]
            S.dma("pool", lambda e: e.indirect_dma_start(out=xb.t[:], out_offset=None, in_=U2, in_offset=IOA(ap=TI[b].t[:, 0:1], axis=0)),
                  reads=[TI[b]], writes=[xb])

        def emit_wd(i):
            wd = WDS[i % 2]
            for fc in range(4):
                for hf in range(2):
                    S.dma("pool", lambda e, fc=fc, hf=hf: e.indirect_dma_start(
                        out=wd[fc].t[:, hf * 2048:(hf + 1) * 2048], out_offset=None, in_=EWD, in_offset=IOA(ap=IDXD.t[:, i:i + 1], axis=0),
                        element_offset=(fc * 256 + hf) * 2048, bounds_check=REG_D, oob_is_err=False), reads=[IDXD], writes=[wd[fc]])

        def emit_scatter(i):
            b = i % 3
            for hf in range(2):
                S.dma("pool", lambda e, hf=hf: e.indirect_dma_start(
                    out=YACC2, out_offset=IOA(ap=TI2[b].t[:, hf:hf + 1], axis=0), in_=yb.t[:, hf * 2048:(hf + 1) * 2048], in_offset=None,
                    compute_op=ALU.add), reads=[yb, TI2[b]], writes=[yacc_dep])

        emit_idx(0)
        emit_wd(0)
        for i in range(NB):
            b = i % 3
            if i + 1 < NB:
                emit_idx(i + 1)
                emit_wd(i + 1)
            xb = xbs[i % 2]
            wd = WDS[i % 2]
            for kg in range(8):
                pt = S.ps()
                ptb = pt.t[:, :].bitcast(BF16)
                for k4 in range(4):
                    kc = kg * 4 + k4
                    S.op("pe", lambda e, ptb=ptb, xb=xb, kc=kc, k4=k4: e.transpose(ptb[:, k4 * 128:(k4 + 1) * 128], xb.t[:, kc * 128:(kc + 1) * 128],
                                                                                  CONST_BF.t[:, :]), reads=[xb, CONST_BF], writes=[pt])
                if kg % 2 == 0:
                    S.op("dve", lambda e, ptb=ptb, kg=kg: e.tensor_copy(out=xbT.t[:, kg * 4:(kg + 1) * 4, :],
                                                                        in_=ptb[:, 0:512].rearrange("p (k t) -> p k t", k=4)), reads=[pt], writes=[xbT])
                else:
                    S.op("act", lambda e, ptb=ptb, kg=kg: e.activation(out=xbT.t[:, kg * 4:(kg + 1) * 4, :],
                                                                       in_=ptb[:, 0:512].rearrange("p (k t) -> p k t", k=4), func=AF.Copy), reads=[pt], writes=[xbT])
            pg = S.ps(hold=True)
            pu = S.ps(hold=True)
            for grp in range(4):
                wg = WG[nw % 3]
                wu = WU[nw % 3]
                nw += 1
                for k8 in range(8):
                    kc = grp * 8 + k8
                    S.dma("pool", lambda e, wg=wg, i=i, kc=kc, k8=k8: e.indirect_dma_start(
                        out=wg.t[:, k8, :], out_offset=None, in_=EWG, in_offset=IOA(ap=IDXG.t[:, i:i + 1], axis=0), element_offset=kc * 128 * 512,
                        bounds_check=REG_G, oob_is_err=False), reads=[IDXG], writes=[wg])
                    S.dma("pool", lambda e, wu=wu, i=i, kc=kc, k8=k8: e.indirect_dma_start(
                        out=wu.t[:, k8, :], out_offset=None, in_=EWU, in_offset=IOA(ap=IDXG.t[:, i:i + 1], axis=0), element_offset=kc * 128 * 512,
                        bounds_check=REG_G, oob_is_err=False), reads=[IDXG], writes=[wu])
                for k8 in range(8):
                    kc = grp * 8 + k8
                    S.op("pe", lambda e, pg=pg, wg=wg, kc=kc, k8=k8: e.matmul(pg.t[:, :], lhsT=xbT.t[:, kc, :], rhs=wg.t[:, k8, :], start=(kc == 0), stop=(kc == 31)),
                         reads=[xbT, wg], writes=[pg])
                    S.op("pe", lambda e, pu=pu, wu=wu, kc=kc, k8=k8: e.matmul(pu.t[:, :], lhsT=xbT.t[:, kc, :], rhs=wu.t[:, k8, :], start=(kc == 0), stop=(kc == 31)),
                         reads=[xbT, wu], writes=[pu])
                if grp == 1 and i > 0:
                    emit_scatter(i - 1)
            S.ps_release(pg)
            S.ps_release(pu)
            S.op("act", lambda e, pg=pg: e.activation(out=hs.t[:], in_=pg.t[:, :], func=AF.Silu), reads=[pg], writes=[hs])
            S.op("dve", lambda e, pu=pu, b=b: e.scalar_tensor_tensor(out=hb.t[:], in0=pu.t[:, :], scalar=RW[b].t[:, 0:1], in1=hs.t[:], op0=ALU.mult, op1=ALU.mult),
                 reads=[pu, RW[b], hs], writes=[hb])
            pt = S.ps()
            ptb = pt.t[:, :].bitcast(BF16)
            for fc in range(4):
                S.op("pe", lambda e, ptb=ptb, fc=fc: e.transpose(ptb[:, fc * 128:(fc + 1) * 128], hb.t[:, fc * 128:(fc + 1) * 128], CONST_BF.t[:, :]),
                     reads=[hb, CONST_BF], writes=[pt])
            S.op("dve", lambda e, ptb=ptb: e.tensor_copy(out=hT.t[:], in_=ptb[:, 0:512].rearrange("p (k t) -> p k t", k=4)), reads=[pt], writes=[hT])
            for cg in range(8):
                py = S.ps()
                for fc in range(4):
                    S.op("pe", lambda e, py=py, fc=fc, cg=cg, wd=wd: e.matmul(py.t[:, :], lhsT=hT.t[:, fc, :], rhs=wd[fc].t[:, cg * 512:(cg + 1) * 512],
                                                                      start=(fc == 0), stop=(fc == 3)), reads=[hT, wd[fc]], writes=[py])
                if cg % 2 == 0:
                    S.op("act", lambda e, py=py, cg=cg: e.activation(out=yb.t[:, cg * 512:(cg + 1) * 512], in_=py.t[:, :], func=AF.Copy), reads=[py], writes=[yb])
                else:
                    S.op("dve", lambda e, py=py, cg=cg: e.tensor_copy(out=yb.t[:, cg * 512:(cg + 1) * 512], in_=py.t[:, :]), reads=[py], writes=[yb])
        emit_scatter(NB - 1)
        S.flush()
    io["_moe_stack"].close()


def phase_final(nc, S, io, out):
    X1 = io["X1"]; YACC = io["YACC"]; MOD = io["MODROW"]
    with contextlib.ExitStack() as ph:
        def tl(shape, dt=F32):
            return S.tile(ph, shape, dt)
        G2 = tl([128, D]); NF = tl([128, D])
        S.dma("sp", lambda e: e.dma_start(out=G2.t[:], in_=MOD[0, 5 * D:6 * D].partition_broadcast(128)), writes=[G2])
        S.dma("sp", lambda e: e.dma_start(out=NF.t[:], in_=io["norm_f_g"][0].partition_broadcast(128)), writes=[NF])
        xts = [tl([128, D]) for _ in range(2)]
        yas = [tl([128, D]) for _ in range(2)]
        scr = tl([128, D], BF16)
        ssq = tl([128, 1]); rstd = tl([128, 1])
        outdep = tl([1, 1])
        for tt in range(T // 128):
            xt = xts[tt % 2]
            ya = yas[tt % 2]
            r0 = tt * 128
            S.dma("sp", lambda e, xt=xt, r0=r0: e.dma_start(out=xt.t[:], in_=X1[r0:r0 + 128, :]), writes=[xt])
            S.dma("sp", lambda e, ya=ya, r0=r0: e.dma_start(out=ya.t[:], in_=YACC[r0:r0 + 128, :]), writes=[ya])
            S.op("dve", lambda e, ya=ya: e.tensor_tensor(out=ya.t[:], in0=ya.t[:], in1=G2.t[:], op=ALU.mult), reads=[ya, G2], writes=[ya])
            S.op("pool", lambda e, ya=ya, xt=xt: e.tensor_tensor(out=xt.t[:], in0=xt.t[:], in1=ya.t[:], op=ALU.add), reads=[xt, ya], writes=[xt])
            io["_norm_tile"](xt, scr, ssq, rstd)
            S.op("dve", lambda e, xt=xt: e.tensor_tensor(out=xt.t[:], in0=xt.t[:], in1=NF.t[:], op=ALU.mult), reads=[xt, NF], writes=[xt])
            S.dma("sp", lambda e, xt=xt, r0=r0: e.dma_start(out=out[r0:r0 + 128, :], in_=xt.t[:]), reads=[xt], writes=[outdep])
        S.flush()


def _in_maps(inputs, cores, with_experts=True):
    f = lambda a: np.ascontiguousarray(a, dtype=np.float32)
    shared = {
        "w_cond": f(inputs["w_cond"][0]), "b_cond": f(inputs["b_cond"][0]).reshape(1, -1),
        "norm1_g": f(inputs["norm1_g"][0]).reshape(1, -1), "w_in": f(inputs["w_in"][0]),
        "rwkv_mu": f(inputs["rwkv_mu"][0]).reshape(1, -1), "rwkv_w0": f(inputs["rwkv_w0"][0]).reshape(1, -1),
        "rwkv_w_up": f(inputs["rwkv_w_up"][0]), "rwkv_a0": f(inputs["rwkv_a0"][0]).reshape(1, -1),
        "rwkv_a_up": f(inputs["rwkv_a_up"][0]), "rwkv_g_up": f(inputs["rwkv_g_up"][0]),
        "rwkv_k_k": f(inputs["rwkv_k_k"][0]).reshape(1, -1), "rwkv_k_a": f(inputs["rwkv_k_a"][0]).reshape(1, -1),
        "rwkv_r_k": f(inputs["rwkv_r_k"][0]).reshape(1, -1), "rwkv_lnx_w": f(inputs["rwkv_lnx_w"][0]).reshape(1, -1),
        "rwkv_lnx_b": f(inputs["rwkv_lnx_b"][0]).reshape(1, -1), "attn_sinks": f(inputs["attn_sinks"][0]).reshape(1, -1),
        "attn_out_g": f(inputs["attn_out_g"][0]).reshape(1, -1), "w_out": f(inputs["w_out"][0]),
        "norm2_g": f(inputs["norm2_g"][0]).reshape(1, -1),
        "router": f(np.concatenate([inputs["router_group"][0], inputs["router_expert"][0]], axis=1)),
        "router_bias": f(np.concatenate([inputs["router_group_bias"][0], inputs["router_expert_bias"][0]])).reshape(1, -1),
        "ew_gate": f(inputs["expert_w_gate"][0]).reshape(64 * D, 512),
        "ew_up": f(inputs["expert_w_up"][0]).reshape(64 * D, 512),
        "ew_down": f(inputs["expert_w_down"][0]).reshape(64 * 512, D),
        "norm_f_g": f(inputs["norm_f_g"]).reshape(1, -1),
        "consts": make_consts(), "attn_bias": make_attn_bias(),
    }
    if not with_experts:
        for k in ("ew_gate", "ew_up", "ew_down"):
            del shared[k]
    maps = []
    for b in cores:
        m = dict(shared)
        m["x"] = f(inputs["x"][b])
        m["c"] = f(inputs["c"][b]).reshape(1, -1)
        maps.append(m)
    return maps


def kernel(**inputs):
    nc = build()
    res = run_bass_kernel_spmd(nc, _in_maps(inputs, range(4)), core_ids=list(range(4)))
    return np.stack([np.asarray(r["out"], dtype=np.float32) for r in res.results], axis=0)
```

```python
import contextlib
import numpy as np
import ml_dtypes
import concourse.bass as bass
import concourse.mybir as mybir
from concourse.bass_utils import run_bass_kernel_spmd

F32 = mybir.dt.float32
BF16 = mybir.dt.bfloat16
I32 = mybir.dt.int32
AF = mybir.ActivationFunctionType
ALU = mybir.AluOpType
AX = mybir.AxisListType

ENGS = ("pe", "act", "dve", "pool", "sp")
HANDLES = {"pe": "tensor", "act": "scalar", "dve": "vector", "pool": "gpsimd", "sp": "sync"}

D = 4096
T = 4096
NRW = 6592
NIN = 9664
DR = 2048
EPS = 1e-6
GN_EPS = 64e-5
NB = 128
NEG = -30000.0


class Buf:
    __slots__ = ("lw", "rd")

    def __init__(self):
        self.lw = None
        self.rd = []


class Tl:
    __slots__ = ("t", "b")

    def __init__(self, t):
        self.t = t
        self.b = Buf()


class Sched:
    NDMA = {"sp": 16, "pool": 48}

    def __init__(self, nc, stack):
        self.nc = nc
        self.ops = {e: [] for e in ENGS}
        self.cnt = {e: 0 for e in ENGS}
        self.esem = {e: stack.enter_context(nc.semaphore("es_" + e)) for e in ENGS if e != "sp"}
        self.dsem = {q: [stack.enter_context(nc.semaphore("ds_%s%d" % (q, i))) for i in range(self.NDMA[q])]
                     for q in ("sp", "pool")}
        self.dcnt = {q: [0] * self.NDMA[q] for q in self.dsem}
        self.dnext = {q: 0 for q in self.dsem}
        self.seen = {e: {} for e in ENGS}
        self.psb = [Tl(stack.enter_context(nc.psum_tensor("psb%d" % i, [128, 512], F32))) for i in range(8)]
        self.psn = 0
        self.held = set()
        self.ntile = 0

    def tile(self, st, shape, dtype=F32):
        self.ntile += 1
        return Tl(st.enter_context(self.nc.sbuf_tensor("t%d" % self.ntile, list(shape), dtype)))

    def ps(self, hold=False):
        while True:
            p = self.psb[self.psn % 8]
            self.psn += 1
            if id(p) not in self.held:
                break
        if hold:
            self.held.add(id(p))
        return p

    def ps_release(self, p):
        self.held.discard(id(p))

    def _need(self, eng, waits, ev):
        if ev is None:
            return
        sem, val, src = ev
        if eng == "pe" and src == "pe":
            return
        key = id(sem)
        if self.seen[eng].get(key, 0) >= val:
            return
        cur = waits.get(key)
        if cur is None or cur[1] < val:
            waits[key] = (sem, val)

    def _deps(self, eng, reads, writes):
        waits = {}
        for b in reads:
            self._need(eng, waits, b.b.lw)
        for b in writes:
            self._need(eng, waits, b.b.lw)
            for r in b.b.rd:
                self._need(eng, waits, r)
        for key, (sem, val) in waits.items():
            self.seen[eng][key] = val
        return list(waits.values())

    def _commit(self, ev, reads, writes):
        for b in reads:
            b.b.rd.append(ev)
        for b in writes:
            b.b.lw = ev
            b.b.rd = []

    def _emit(self, eng, fn, waits, inc):
        eh = getattr(self.nc, HANDLES[eng])
        for sem, val in waits:
            eh.wait_ge(sem, val)
        if fn is not None:
            fn(eh).then_inc(inc[0], inc[1])

    def op(self, eng, fn, reads=(), writes=()):
        waits = self._deps(eng, reads, writes)
        self.cnt[eng] += 1
        ev = (self.esem[eng], self.cnt[eng], eng)
        self._emit(eng, fn, waits, (self.esem[eng], 1))
        self._commit(ev, reads, writes)

    def dma(self, q, fn, reads=(), writes=()):
        waits = self._deps(q, reads, writes)
        k = self.dnext[q]
        self.dnext[q] = (k + 1) % self.NDMA[q]
        sem = self.dsem[q][k]
        prev = self.dcnt[q][k]
        if prev > 0 and self.seen[q].get(id(sem), 0) < prev:
            self.seen[q][id(sem)] = prev
            waits.append((sem, prev))
        self.dcnt[q][k] = prev + 16
        ev = (sem, prev + 16, "dma")
        self._emit(q, fn, waits, (sem, 16))
        self._commit(ev, reads, writes)

    def barrier(self):
        evs = [(self.esem[e], self.cnt[e], "bar") for e in self.esem if self.cnt[e] > 0]
        for q in self.dsem:
            for k in range(self.NDMA[q]):
                if self.dcnt[q][k] > 0:
                    evs.append((self.dsem[q][k], self.dcnt[q][k], "bar"))
        for e in ENGS:
            waits = {}
            for ev in evs:
                self._need(e, waits, ev)
            for key, (sem, val) in waits.items():
                self.seen[e][key] = val
            if waits:
                self._emit(e, None, list(waits.values()), None)

    def flush(self):
        self.barrier()


C_ID = 0
C_ONES = 128
C_TRI = 256
C_ML = 384
C_MU = 448
C_MUI = 512
C_SU = 576
C_SUI = 640
C_K128 = 704
C_PID = 768
C_P128 = 769
C_IOTA64 = 770
C_RM = 834
C_END = C_RM + 2048


def make_consts():
    c = np.zeros((128, C_END), np.float32)
    c[:, C_ID:C_ID + 128] = np.eye(128)
    c[:, C_ONES:C_ONES + 128] = 1.0
    i = np.arange(128)
    c[:, C_TRI:C_TRI + 128] = (i[:, None] < i[None, :])
    j = np.arange(64)
    c[:64, C_ML:C_ML + 64] = (j[None, :] < j[:, None])
    c[:64, C_MU:C_MU + 64] = (j[:, None] < j[None, :])
    c[:64, C_MUI:C_MUI + 64] = (j[:, None] <= j[None, :])
    c[:64, C_SU:C_SU + 64] = (j[:, None] < j[None, :])
    c[:64, C_SUI:C_SUI + 64] = (j[:, None] <= j[None, :])
    c[:, C_K128:C_K128 + 64] = 128.0 * j[None, :]
    c[:, C_PID] = i
    c[:, C_P128] = 128.0 * i
    c[:, C_IOTA64:C_IOTA64 + 64] = j[None, :]
    rm = np.ones(2048, np.float32)
    rm[::64] = 0.0
    c[:, C_RM:C_RM + 2048] = rm[None, :]
    return c


def make_attn_bias():
    qi = np.arange(128)[:, None]
    kj = np.arange(256)[None, :]
    dist = (qi + 128 - kj).astype(np.float32)
    inw = (dist >= 0) & (dist < 128)
    slopes = np.exp2(-8.0 * np.arange(1, 33, dtype=np.float32) / 32).astype(np.float32).reshape(8, 4)
    ab = np.zeros((128, 2, 8, 4, 256), np.float32)
    for h in range(8):
        for g in range(4):
            a = np.where(inw, -slopes[h, g] * dist, NEG).astype(np.float32)
            ab[:, 0, h, g, :] = a
            a0 = a.copy()
            a0[:, :128] = NEG
            ab[:, 1, h, g, :] = a0
    return ab.reshape(128, 2 * 8 * 4 * 256)


def build(last_phase=99, debug=False):
    nc = bass.Bass("TRN2", target_bir_lowering=False)
    io = {}

    def din(name, shape, dt=F32):
        io[name] = nc.dram_tensor(name, list(shape), dt, kind="ExternalInput").ap()

    din("x", [T, D]); din("c", [1, D]); din("w_cond", [D, 6 * D]); din("b_cond", [1, 6 * D])
    din("norm1_g", [1, D]); din("w_in", [D, NIN]); din("rwkv_mu", [1, NRW]); din("rwkv_w0", [1, DR])
    din("rwkv_w_up", [96, DR]); din("rwkv_a0", [1, DR]); din("rwkv_a_up", [96, DR]); din("rwkv_g_up", [256, DR])
    din("rwkv_k_k", [1, DR]); din("rwkv_k_a", [1, DR]); din("rwkv_r_k", [1, DR]); din("rwkv_lnx_w", [1, DR])
    din("rwkv_lnx_b", [1, DR]); din("attn_sinks", [1, 32]); din("attn_out_g", [1, DR]); din("w_out", [D, D])
    din("norm2_g", [1, D]); din("router", [D, 72]); din("router_bias", [1, 72])
    if last_phase >= 5:
        din("ew_gate", [64 * D, 512]); din("ew_up", [64 * D, 512]); din("ew_down", [64 * 512, D])
    din("norm_f_g", [1, D]); din("consts", [128, C_END]); din("attn_bias", [128, 2 * 8 * 1024])
    out = nc.dram_tensor("out", [T, D], F32, kind="ExternalOutput").ap()

    def scratch(name, shape, dt=F32):
        kind = "ExternalOutput" if debug else "Internal"
        io[name] = nc.dram_tensor(name, list(shape), dt, kind=kind).ap()

    scratch("MODROW", [1, 6 * D]); scratch("PT", [NIN, T]); scratch("YT", [D, T]); scratch("X1", [T, D])
    scratch("U2", [T + 1, D], BF16); scratch("ROWTOK", [NB * 128, 16]); scratch("ROWW", [NB * 128, 16])
    scratch("YACC", [T + 1, D])

    with contextlib.ExitStack() as gs:
        S = Sched(nc, gs)
        CONST = S.tile(gs, [128, C_END])
        S.dma("sp", lambda e: e.dma_start(out=CONST.t[:], in_=io["consts"]), writes=[CONST])
        ident = CONST.t[:, C_ID:C_ID + 128]
        A1 = S.tile(gs, [128, 32]); SH1 = S.tile(gs, [128, 32]); A2 = S.tile(gs, [128, 32]); SH2 = S.tile(gs, [128, 32])

        def col_from_row(ph, src_ap, dst_fn):
            r = S.tile(ph, [32, 128])
            S.dma("sp", lambda e: e.dma_start(out=r.t[:], in_=src_ap.rearrange("o (k p) -> (o k) p", p=128)), writes=[r])
            ps = S.ps()
            S.op("pe", lambda e: e.transpose(ps.t[:, 0:32], r.t[:], ident[0:32, 0:32]), reads=[r, CONST], writes=[ps])
            dst_fn(ps)

        with contextlib.ExitStack() as ph:
            scT = S.tile(ph, [128, 32])
            col_from_row(ph, io["c"], lambda ps: S.op(
                "act", lambda e: e.activation(out=scT.t[:], in_=ps.t[:, 0:32], func=AF.Silu), reads=[ps], writes=[scT]))
            bc = S.tile(ph, [1, 6 * D])
            S.dma("sp", lambda e: e.dma_start(out=bc.t[:], in_=io["b_cond"]), writes=[bc])
            wts = [S.tile(ph, [128, 2048]) for _ in range(3)]
            mrs = [S.tile(ph, [1, 2048]) for _ in range(2)]
            n = 0
            for ng in range(12):
                pbs = [S.ps() for _ in range(4)]
                for kc in range(32):
                    wt = wts[n % 3]
                    n += 1
                    S.dma("sp", lambda e, wt=wt, kc=kc, ng=ng: e.dma_start(
                        out=wt.t[:], in_=io["w_cond"][kc * 128:(kc + 1) * 128, ng * 2048:(ng + 1) * 2048]), writes=[wt])
                    for j in range(4):
                        S.op("pe", lambda e, wt=wt, kc=kc, j=j, pb=pbs[j]: e.matmul(
                            pb.t[0:1, :], lhsT=scT.t[:, kc:kc + 1], rhs=wt.t[:, j * 512:(j + 1) * 512],
                            start=(kc == 0), stop=(kc == 31)), reads=[wt, scT], writes=[pbs[j]])
                mr = mrs[ng % 2]
                for j in range(4):
                    S.op("dve", lambda e, mr=mr, j=j, pb=pbs[j], ng=ng: e.tensor_tensor(
                        out=mr.t[0:1, j * 512:(j + 1) * 512], in0=pb.t[0:1, :],
                        in1=bc.t[0:1, ng * 2048 + j * 512: ng * 2048 + (j + 1) * 512], op=ALU.add),
                        reads=[pbs[j], bc], writes=[mr])
                S.dma("sp", lambda e, mr=mr, ng=ng: e.dma_start(
                    out=io["MODROW"][0:1, ng * 2048:(ng + 1) * 2048], in_=mr.t[:]), reads=[mr], writes=[])
            S.flush()
            for (Ax, SHx, gname, ish, isc) in ((A1, SH1, "norm1_g", 0, 1), (A2, SH2, "norm2_g", 3, 4)):
                gcol = S.tile(ph, [128, 32])
                col_from_row(ph, io[gname], lambda ps, gcol=gcol: S.op(
                    "dve", lambda e: e.tensor_copy(out=gcol.t[:], in_=ps.t[:, 0:32]), reads=[ps], writes=[gcol]))
                col_from_row(ph, io["MODROW"][0:1, isc * D:(isc + 1) * D], lambda ps, gcol=gcol, Ax=Ax: S.op(
                    "dve", lambda e: e.scalar_tensor_tensor(out=Ax.t[:], in0=ps.t[:, 0:32], scalar=1.0, in1=gcol.t[:],
                                                            op0=ALU.add, op1=ALU.mult), reads=[ps, gcol], writes=[Ax]))
                col_from_row(ph, io["MODROW"][0:1, ish * D:(ish + 1) * D], lambda ps, SHx=SHx: S.op(
                    "dve", lambda e: e.tensor_copy(out=SHx.t[:], in_=ps.t[:, 0:32]), reads=[ps], writes=[SHx]))
            S.flush()
        if last_phase <= 0:
            return nc

        def norm_tile(xt, scr, ssq, rstd):
            S.op("act", lambda e: e.activation(out=scr.t[:], in_=xt.t[:], func=AF.Square, accum_out=ssq.t[:, 0:1]),
                 reads=[xt], writes=[scr, ssq])
            S.op("dve", lambda e: e.tensor_scalar(out=rstd.t[:, 0:1], in0=ssq.t[:, 0:1], scalar1=1.0 / D, scalar2=EPS,
                                                  op0=ALU.mult, op1=ALU.add), reads=[ssq], writes=[rstd])
            S.op("act", lambda e: e.activation(out=rstd.t[:, 0:1], in_=rstd.t[:, 0:1], func=AF.Sqrt), reads=[rstd], writes=[rstd])
            S.op("dve", lambda e: e.reciprocal(out=rstd.t[:, 0:1], in_=rstd.t[:, 0:1]), reads=[rstd], writes=[rstd])
            S.op("act", lambda e: e.activation(out=xt.t[:], in_=xt.t[:], func=AF.Copy, scale=rstd.t[:, 0:1]),
                 reads=[xt, rstd], writes=[xt])

        TQ = 1024
        with contextlib.ExitStack() as ph:
            uT = S.tile(ph, [128, 32, TQ], BF16)
            xts = [S.tile(ph, [128, D]) for _ in range(2)]
            scr = S.tile(ph, [128, D], BF16)
            ssq = S.tile(ph, [128, 1]); rstd = S.tile(ph, [128, 1])
            wbs = [S.tile(ph, [128, 32, 256], BF16) for _ in range(2)]
            ots = [S.tile(ph, [128, TQ]) for _ in range(2)]
            nw = 0
            no = 0
            for tq in range(T // TQ):
                for tt in range(TQ // 128):
                    xt = xts[tt % 2]
                    r0 = tq * TQ + tt * 128
                    S.dma("sp", lambda e, xt=xt, r0=r0: e.dma_start(out=xt.t[:], in_=io["x"][r0:r0 + 128, :]), writes=[xt])
                    norm_tile(xt, scr, ssq, rstd)
                    for kg in range(8):
                        ps = S.ps()
                        for k4 in range(4):
                            kc = kg * 4 + k4
                            S.op("pe", lambda e, ps=ps, xt=xt, kc=kc, k4=k4: e.transpose(
                                ps.t[:, k4 * 128:(k4 + 1) * 128], xt.t[:, kc * 128:(kc + 1) * 128], ident),
                                reads=[xt, CONST], writes=[ps])
                        for k4 in range(4):
                            kc = kg * 4 + k4
                            if k4 % 2 == 0:
                                S.op("dve", lambda e, ps=ps, kc=kc, k4=k4, tt=tt: e.tensor_scalar(
                                    out=uT.t[:, kc, tt * 128:(tt + 1) * 128], in0=ps.t[:, k4 * 128:(k4 + 1) * 128],
                                    scalar1=A1.t[:, kc:kc + 1], scalar2=SH1.t[:, kc:kc + 1], op0=ALU.mult, op1=ALU.add),
                                    reads=[ps, A1, SH1], writes=[uT])
                            else:
                                S.op("act", lambda e, ps=ps, kc=kc, k4=k4, tt=tt: e.activation(
                                    out=uT.t[:, kc, tt * 128:(tt + 1) * 128], in_=ps.t[:, k4 * 128:(k4 + 1) * 128],
                                    func=AF.Identity, scale=A1.t[:, kc:kc + 1], bias=SH1.t[:, kc:kc + 1]),
                                    reads=[ps, A1, SH1], writes=[uT])
                for cg in range(38):
                    c0 = cg * 256
                    ncol = min(256, NIN - c0)
                    wb = wbs[nw % 2]
                    nw += 1
                    S.dma("pool", lambda e, wb=wb, c0=c0, ncol=ncol: e.dma_start(
                        out=wb.t[:, :, 0:ncol], in_=io["w_in"][:, c0:c0 + ncol].rearrange("(k p) j -> p k j", p=128)),
                        writes=[wb])
                    for sc in range((ncol + 127) // 128):
                        m = min(128, ncol - sc * 128)
                        ot = ots[no % 2]
                        no += 1
                        for tg in range(TQ // 512):
                            ps = S.ps()
                            for kc in range(32):
                                S.op("pe", lambda e, ps=ps, wb=wb, kc=kc, sc=sc, m=m, tg=tg: e.matmul(
                                    ps.t[0:m, :], lhsT=wb.t[:, kc, sc * 128:sc * 128 + m], rhs=uT.t[:, kc, tg * 512:(tg + 1) * 512],
                                    start=(kc == 0), stop=(kc == 31)), reads=[wb, uT], writes=[ps])
                            if tg % 2 == 0:
                                S.op("dve", lambda e, ps=ps, ot=ot, m=m, tg=tg: e.tensor_copy(
                                    out=ot.t[0:m, tg * 512:(tg + 1) * 512], in_=ps.t[0:m, :]), reads=[ps], writes=[ot])
                            else:
                                S.op("act", lambda e, ps=ps, ot=ot, m=m, tg=tg: e.activation(
                                    out=ot.t[0:m, tg * 512:(tg + 1) * 512], in_=ps.t[0:m, :], func=AF.Copy), reads=[ps], writes=[ot])
                        S.dma("sp", lambda e, ot=ot, m=m, c0=c0, sc=sc, tq=tq: e.dma_start(
                            out=io["PT"][c0 + sc * 128:c0 + sc * 128 + m, tq * TQ:(tq + 1) * TQ], in_=ot.t[0:m, :]),
                            reads=[ot], writes=[])
            S.flush()
        if last_phase <= 1:
            return nc
        return build_rest(nc, S, gs, io, out, CONST, ident, A1, SH1, A2, SH2, norm_tile, last_phase)


def build_rest(nc, S, gs, io, out, CONST, ident, A1, SH1, A2, SH2, norm_tile, last_phase):
    io["_norm_tile"] = norm_tile
    phase_rwkv(nc, S, io, CONST, ident)
    if last_phase <= 2:
        return nc
    phase_attn(nc, S, io, CONST, ident)
    if last_phase <= 3:
        return nc
    phase_wout(nc, S, io, CONST, ident, A2, SH2, norm_tile, gs)
    if last_phase <= 4:
        return nc
    phase_moe(nc, S, io, CONST, ident)
    if last_phase <= 5:
        return nc
    phase_final(nc, S, io, out)
    return nc


def cview(CONST, c0, n, parts=64):
    return CONST.t[0:parts, c0:c0 + n]


def bc3(ap2, nh, last=True):
    p, n = ap2.shape
    if last:
        return ap2.unsqueeze(1).broadcast_to([p, nh, n])
    return ap2.unsqueeze(2).broadcast_to([p, n, nh])


def phase_rwkv(nc, S, io, CONST, ident):
    NH, NT, C = 8, 128, 64
    NCH = NT // C
    id64 = CONST.t[0:64, C_ID:C_ID + 64]
    ones64 = CONST.t[0:64, C_ONES:C_ONES + 64]
    ML = bc3(cview(CONST, C_ML, 64), NH)
    MU = bc3(cview(CONST, C_MU, 64), NH)
    MUI = bc3(cview(CONST, C_MUI, 64), NH)
    ID4 = bc3(id64, NH)
    RM = CONST.t[0:64, C_RM:C_RM + NH * NT]
    PT = io["PT"]
    mu = io["rwkv_mu"]
    with contextlib.ExitStack() as ph:
        def tl(shape, dt=F32):
            return S.tile(ph, shape, dt)

        def col_from(src_ap, rows, cols, dst):
            r = tl([rows, cols])
            S.dma("sp", lambda e: e.dma_start(out=r.t[:], in_=src_ap), writes=[r])
            ps = S.ps()
            S.op("pe", lambda e: e.transpose(ps.t[0:cols, 0:rows], r.t[:], ident[0:rows, 0:rows]), reads=[r, CONST], writes=[ps])
            S.op("dve", lambda e: e.tensor_copy(out=dst.t[:], in_=ps.t[0:cols, 0:rows]), reads=[ps], writes=[dst])

        def headcol(src_row):
            d = tl([64, 32])
            col_from(src_row.rearrange("o (h j) -> (o h) j", j=64), 32, 64, d)
            return d

        MUR = headcol(mu[0:1, 0:2048]); MUK = headcol(mu[0:1, 2048:4096]); MUV = headcol(mu[0:1, 4096:6144])
        W0 = headcol(io["rwkv_w0"]); A0 = headcol(io["rwkv_a0"]); KK_ = headcol(io["rwkv_k_k"]); KA = headcol(io["rwkv_k_a"])
        RK_ = headcol(io["rwkv_r_k"]); LNW = headcol(io["rwkv_lnx_w"]); LNB = headcol(io["rwkv_lnx_b"])
        NW0 = tl([64, 32])
        S.op("dve", lambda e: e.tensor_scalar(out=NW0.t[:], in0=W0.t[:], scalar1=-1.0, scalar2=None, op0=ALU.mult), reads=[W0], writes=[NW0])
        MUW = tl([96, 1]); col_from(mu[0:1, 6144:6240], 1, 96, MUW)
        MUA = tl([96, 1]); col_from(mu[0:1, 6240:6336], 1, 96, MUA)
        MUG = tl([128, 2]); col_from(mu[0:1, 6336:6592].rearrange("o (k p) -> (o k) p", p=128), 2, 128, MUG)
        WUP = tl([96, DR]); AUP = tl([96, DR]); GUP = tl([128, 2, DR])
        S.dma("sp", lambda e: e.dma_start(out=WUP.t[:], in_=io["rwkv_w_up"]), writes=[WUP])
        S.dma("sp", lambda e: e.dma_start(out=AUP.t[:], in_=io["rwkv_a_up"]), writes=[AUP])
        S.dma("sp", lambda e: e.dma_start(out=GUP.t[:], in_=io["rwkv_g_up"].rearrange("(k p) c -> p k c", p=128)), writes=[GUP])
        ST = tl([64, 32, 64])
        S.op("pool", lambda e: e.memset(ST.t[:], 0.0), writes=[ST])

        TW = tl([96, NT]); XA = tl([96, NT]); SG = tl([128, 2, NT])
        lc = tl([128, 2, NT]); lp = tl([128, 2, NT])
        big = lambda: tl([64, NH, NT])
        R = big(); K = big(); V = big(); PV = big(); E = big(); A = big(); G_ = big(); KKt = big(); NR = big()
        K2 = big(); B = big(); L = big(); GAM = big(); GI = big(); BON = big(); YN = big()
        GP = PV; KKD = KKt; BI = B; KI = K2; RD = R
        C1 = big(); C2 = big(); C3 = big(); P2 = big(); P3 = big()
        VT = tl([64, NCH, NH, 64]); BIT = tl([64, NCH, NH, 64]); KIT = tl([64, NCH, NH, 64])
        sm = lambda: tl([64, NH, 64])
        M = [[sm(), sm()] for _ in range(NCH)]; MT = [[sm(), sm()] for _ in range(NCH)]
        TT = [sm() for _ in range(NCH)]; BTm = [sm() for _ in range(NCH)]; PTm = [sm() for _ in range(NCH)]; QTm = [sm() for _ in range(NCH)]
        YS = [sm() for _ in range(NCH)]; XS = sm(); US = sm(); SQ = sm(); TMP = sm()
        s1 = tl([64, NH]); s2 = tl([64, NH]); mean = tl([64, NH]); rstd = tl([64, NH])

        def load_mix(dst, cur, prev, rows, t0, parts, nh, mucol):
            S.dma("sp", lambda e: e.dma_start(out=cur.t[0:parts, 0:nh, :], in_=rows(t0, t0 + NT)), writes=[cur])
            if t0 == 0:
                S.op("pool", lambda e: e.memset(prev.t[0:parts, 0:nh, 0:1], 0.0), writes=[prev])
                S.dma("sp", lambda e: e.dma_start(out=prev.t[0:parts, 0:nh, 1:NT], in_=rows(0, NT - 1)), writes=[prev])
            else:
                S.dma("sp", lambda e: e.dma_start(out=prev.t[0:parts, 0:nh, :], in_=rows(t0 - 1, t0 + NT - 1)), writes=[prev])
            S.op("pool", lambda e: e.tensor_tensor(out=prev.t[0:parts, 0:nh, :], in0=prev.t[0:parts, 0:nh, :],
                                                   in1=cur.t[0:parts, 0:nh, :], op=ALU.subtract), reads=[cur, prev], writes=[prev])
            for h in range(nh):
                S.op("dve", lambda e, h=h: e.scalar_tensor_tensor(
                    out=dst.t[0:parts, h, :], in0=prev.t[0:parts, h, :], scalar=mucol(h), in1=cur.t[0:parts, h, :],
                    op0=ALU.mult, op1=ALU.add), reads=[prev, cur], writes=[dst])

        for tq in range(T // NT):
            t0 = tq * NT
            load_mix(lp, lc, lp, lambda a, b: PT[6144:6240, a:b].unsqueeze(1), t0, 96, 1, lambda h: MUW.t[:, 0:1])
            S.op("act", lambda e: e.activation(out=TW.t[:], in_=lp.t[0:96, 0, :], func=AF.Tanh), reads=[lp], writes=[TW])
            load_mix(lp, lc, lp, lambda a, b: PT[6240:6336, a:b].unsqueeze(1), t0, 96, 1, lambda h: MUA.t[:, 0:1])
            S.op("act", lambda e: e.activation(out=XA.t[:], in_=lp.t[0:96, 0, :], func=AF.Copy), reads=[lp], writes=[XA])
            load_mix(lp, lc, lp, lambda a, b: PT[6336:6592, a:b].rearrange("(k p) t -> p k t", p=128), t0, 128, 2,
                     lambda h: MUG.t[:, h:h + 1])
            S.op("act", lambda e: e.activation(out=SG.t[:], in_=lp.t[:], func=AF.Sigmoid), reads=[lp], writes=[SG])
            for hg in range(32 // NH):
                H0 = hg * NH
                c0 = H0 * 64
                hv = lambda base: (lambda a, b: PT[base + c0:base + c0 + NH * 64, a:b].rearrange("(h j) t -> j h t", j=64))
                load_mix(R, C1, PV, hv(0), t0, 64, NH, lambda h: MUR.t[:, H0 + h:H0 + h + 1])
                load_mix(K, C2, P2, hv(2048), t0, 64, NH, lambda h: MUK.t[:, H0 + h:H0 + h + 1])
                load_mix(V, C3, P3, hv(4096), t0, 64, NH, lambda h: MUV.t[:, H0 + h:H0 + h + 1])
                for h in range(NH):
                    H = H0 + h
                    cs = slice(H * 64, H * 64 + 64)
                    p1 = S.ps()
                    S.op("pe", lambda e, p1=p1, cs=cs: e.matmul(p1.t[0:64, 0:NT], lhsT=WUP.t[:, cs], rhs=TW.t[:], start=True, stop=True),
                         reads=[WUP, TW], writes=[p1])
                    S.op("act", lambda e, p1=p1, h=h, H=H: e.activation(out=E.t[:, h, :], in_=p1.t[0:64, 0:NT], func=AF.Exp,
                                                                       scale=-1.0, bias=NW0.t[:, H:H + 1]), reads=[p1, NW0], writes=[E])
                    S.op("pool", lambda e, h=h, H=H: e.tensor_scalar(out=KKt.t[:, h, :], in0=K.t[:, h, :], scalar1=KK_.t[:, H:H + 1],
                                                                    scalar2=None, op0=ALU.mult), reads=[K, KK_], writes=[KKt])
                for h in range(NH):
                    H = H0 + h
                    cs = slice(H * 64, H * 64 + 64)
                    p3 = S.ps()
                    for kc in range(2):
                        S.op("pe", lambda e, p3=p3, cs=cs, kc=kc: e.matmul(p3.t[0:64, 0:NT], lhsT=GUP.t[:, kc, cs], rhs=SG.t[:, kc, :],
                                                                         start=(kc == 0), stop=(kc == 1)), reads=[GUP, SG], writes=[p3])
                    S.op("dve", lambda e, p3=p3, h=h: e.tensor_copy(out=G_.t[:, h, :], in_=p3.t[0:64, 0:NT]), reads=[p3], writes=[G_])
                S.op("act", lambda e: e.activation(out=E.t[:], in_=E.t[:], func=AF.Ln, bias=1.0), reads=[E], writes=[E])
                S.op("act", lambda e: e.activation(out=E.t[:], in_=E.t[:], func=AF.Exp, scale=-1.0, bias=-0.5), reads=[E], writes=[E])
                for h in range(NH):
                    H = H0 + h
                    cs = slice(H * 64, H * 64 + 64)
                    p2 = S.ps()
                    S.op("pe", lambda e, p2=p2, cs=cs: e.matmul(p2.t[0:64, 0:NT], lhsT=AUP.t[:, cs], rhs=XA.t[:], start=True, stop=True),
                         reads=[AUP, XA], writes=[p2])
                    S.op("act", lambda e, p2=p2, h=h, H=H: e.activation(out=A.t[:, h, :], in_=p2.t[0:64, 0:NT], func=AF.Sigmoid,
                                                                       bias=A0.t[:, H:H + 1]), reads=[p2, A0], writes=[A])
                S.op("pool", lambda e: e.tensor_tensor(out=NR.t[:], in0=KKt.t[:], in1=KKt.t[:], op=ALU.mult), reads=[KKt], writes=[NR])
                for h in range(NH):
                    p1 = S.ps()
                    S.op("pe", lambda e, p1=p1, h=h: e.matmul(p1.t[0:64, 0:NT], lhsT=ones64, rhs=NR.t[:, h, :], start=True, stop=True),
                         reads=[NR, CONST], writes=[p1])
                    S.op("dve", lambda e, p1=p1, h=h: e.tensor_scalar(out=L.t[:, h, :], in0=p1.t[0:64, 0:NT], scalar1=1e-19, scalar2=None, op0=ALU.max),
                         reads=[p1], writes=[L])
                S.op("act", lambda e: e.activation(out=L.t[:], in_=L.t[:], func=AF.Ln), reads=[L], writes=[L])
                S.op("act", lambda e: e.activation(out=NR.t[:], in_=L.t[:], func=AF.Exp, scale=-0.5), reads=[L], writes=[NR])
                S.op("dve", lambda e: e.tensor_tensor(out=KKt.t[:], in0=KKt.t[:], in1=NR.t[:], op=ALU.mult), reads=[KKt, NR], writes=[KKt])
                for h in range(NH):
                    H = H0 + h
                    S.op("dve", lambda e, h=h, H=H: e.tensor_scalar(out=K2.t[:, h, :], in0=A.t[:, h, :], scalar1=-1.0, scalar2=KA.t[:, H:H + 1],
                                                                   op0=ALU.add, op1=ALU.mult), reads=[A, KA], writes=[K2])
                S.op("dve", lambda e: e.scalar_tensor_tensor(out=K2.t[:], in0=K2.t[:], scalar=1.0, in1=K.t[:], op0=ALU.add, op1=ALU.mult),
                     reads=[K2, K], writes=[K2])
                S.op("pool", lambda e: e.tensor_tensor(out=B.t[:], in0=KKt.t[:], in1=A.t[:], op=ALU.mult), reads=[KKt, A], writes=[B])
                S.op("pool", lambda e: e.tensor_tensor(out=BON.t[:], in0=R.t[:], in1=K2.t[:], op=ALU.mult), reads=[R, K2], writes=[BON])
                for h in range(NH):
                    H = H0 + h
                    S.op("dve", lambda e, h=h, H=H: e.tensor_scalar(out=BON.t[:, h, :], in0=BON.t[:, h, :], scalar1=RK_.t[:, H:H + 1],
                                                                   scalar2=None, op0=ALU.mult), reads=[BON, RK_], writes=[BON])
                    p1 = S.ps()
                    S.op("pe", lambda e, p1=p1, h=h: e.matmul(p1.t[0:64, 0:NT], lhsT=ones64, rhs=BON.t[:, h, :], start=True, stop=True),
                         reads=[BON, CONST], writes=[p1])
                    S.op("dve", lambda e, p1=p1, h=h: e.tensor_tensor(out=BON.t[:, h, :], in0=p1.t[0:64, 0:NT], in1=V.t[:, h, :], op=ALU.mult),
                         reads=[p1, V, BON], writes=[BON])
                S.op("dve", lambda e: e.tensor_tensor_scan(out=L.t[:].rearrange("p h t -> p (h t)"), data0=RM,
                                                           data1=E.t[:].rearrange("p h t -> p (h t)"), initial=0.0,
                                                           op0=ALU.mult, op1=ALU.subtract), reads=[E, CONST, L], writes=[L])
                S.op("act", lambda e: e.activation(out=GAM.t[:], in_=L.t[:], func=AF.Exp), reads=[L], writes=[GAM])
                S.op("act", lambda e: e.activation(out=GI.t[:], in_=L.t[:], func=AF.Exp, scale=-1.0), reads=[L], writes=[GI])
                S.op("pool", lambda e: e.tensor_tensor(out=GP.t[:], in0=L.t[:], in1=E.t[:], op=ALU.add), reads=[L, E], writes=[GP])
                S.op("act", lambda e: e.activation(out=GP.t[:], in_=GP.t[:], func=AF.Exp), reads=[GP], writes=[GP])
                S.op("dve", lambda e: e.tensor_tensor(out=KKD.t[:], in0=KKt.t[:], in1=GP.t[:], op=ALU.mult), reads=[KKt, GP], writes=[KKD])
                S.op("pool", lambda e: e.tensor_tensor(out=BI.t[:], in0=B.t[:], in1=GI.t[:], op=ALU.mult), reads=[B, GI], writes=[BI])
                S.op("dve", lambda e: e.tensor_tensor(out=KI.t[:], in0=K2.t[:], in1=GI.t[:], op=ALU.mult), reads=[K2, GI], writes=[KI])
                S.op("pool", lambda e: e.tensor_tensor(out=RD.t[:], in0=R.t[:], in1=GAM.t[:], op=ALU.mult), reads=[R, GAM], writes=[RD])
                for (src, dst) in ((V, VT), (BI, BIT), (KI, KIT)):
                    for c in range(NCH):
                        pt = S.ps()
                        for h in range(NH):
                            S.op("pe", lambda e, pt=pt, src=src, c=c, h=h: e.transpose(
                                pt.t[0:64, h * 64:(h + 1) * 64], src.t[:, h, c * C:(c + 1) * C], id64), reads=[src, CONST], writes=[pt])
                        S.op("act" if c % 2 else "dve", (lambda e, pt=pt, dst=dst, c=c: e.activation(
                            out=dst.t[:, c, :, :], in_=pt.t[0:64, 0:NH * 64].rearrange("p (h i) -> p h i", h=NH), func=AF.Copy)) if c % 2 else
                            (lambda e, pt=pt, dst=dst, c=c: e.tensor_copy(
                                out=dst.t[:, c, :, :], in_=pt.t[0:64, 0:NH * 64].rearrange("p (h i) -> p h i", h=NH))),
                            reads=[pt], writes=[dst])

                def mm4(lhs_fn, rhs_fn, reads):
                    p = S.ps()
                    for h in range(NH):
                        l_ = lhs_fn(h)
                        r_ = rhs_fn(h)
                        S.op("pe", lambda e, p=p, h=h, l_=l_, r_=r_: e.matmul(p.t[0:64, h * 64:(h + 1) * 64], lhsT=l_, rhs=r_, start=True, stop=True),
                             reads=reads, writes=[p])
                    return p

                def pv(p):
                    return p.t[0:64, 0:NH * 64].rearrange("p (h i) -> p h i", h=NH)

                tcs = [slice(c * C, (c + 1) * C) for c in range(NCH)]
                for c in range(NCH):
                    tc = tcs[c]
                    p = mm4(lambda h: KKD.t[:, h, tc], lambda h: BI.t[:, h, tc], [KKD, BI])
                    S.op("dve", lambda e, p=p, c=c: e.tensor_tensor(out=M[c][0].t[:], in0=pv(p), in1=ML, op=ALU.mult), reads=[p, CONST], writes=[M[c][0]])
                    p = mm4(lambda h: BI.t[:, h, tc], lambda h: KKD.t[:, h, tc], [KKD, BI])
                    S.op("dve", lambda e, p=p, c=c: e.tensor_tensor(out=MT[c][0].t[:], in0=pv(p), in1=MU, op=ALU.mult), reads=[p, CONST], writes=[MT[c][0]])
                    S.op("pool", lambda e, c=c: e.tensor_tensor(out=TT[c].t[:], in0=ID4, in1=MT[c][0].t[:], op=ALU.subtract), reads=[MT[c][0], CONST], writes=[TT[c]])
                    p = mm4(lambda h: KI.t[:, h, tc], lambda h: KKD.t[:, h, tc], [KKD, KI])
                    S.op("dve", lambda e, p=p, c=c: e.tensor_tensor(out=BTm[c].t[:], in0=pv(p), in1=MU, op=ALU.mult), reads=[p, CONST], writes=[BTm[c]])
                    p = mm4(lambda h: BI.t[:, h, tc], lambda h: RD.t[:, h, tc], [RD, BI])
                    S.op("dve", lambda e, p=p, c=c: e.tensor_tensor(out=PTm[c].t[:], in0=pv(p), in1=MUI, op=ALU.mult), reads=[p, CONST], writes=[PTm[c]])
                    p = mm4(lambda h: KI.t[:, h, tc], lambda h: RD.t[:, h, tc], [RD, KI])
                    S.op("dve", lambda e, p=p, c=c: e.tensor_tensor(out=QTm[c].t[:], in0=pv(p), in1=MUI, op=ALU.mult), reads=[p, CONST], writes=[QTm[c]])
                cur = 0
                for lvl in range(5):
                    nxt = 1 - cur
                    for c in range(NCH):
                        Mc, MTc, Mn = M[c][cur], MT[c][cur], M[c][nxt]
                        p = mm4(lambda h, MTc=MTc: MTc.t[:, h, :], lambda h, Mc=Mc: Mc.t[:, h, :], [Mc, MTc])
                        S.op("act", lambda e, p=p, Mn=Mn: e.activation(out=Mn.t[:], in_=pv(p), func=AF.Copy), reads=[p], writes=[Mn])
                    if lvl < 4:
                        for c in range(NCH):
                            Mc, MTc, MTn = M[c][cur], MT[c][cur], MT[c][nxt]
                            p = mm4(lambda h, Mc=Mc: Mc.t[:, h, :], lambda h, MTc=MTc: MTc.t[:, h, :], [Mc, MTc])
                            if c % 2 == 0:
                                S.op("act", lambda e, p=p, MTn=MTn: e.activation(out=MTn.t[:], in_=pv(p), func=AF.Copy), reads=[p], writes=[MTn])
                            else:
                                S.op("dve", lambda e, p=p, MTn=MTn: e.tensor_copy(out=MTn.t[:], in_=pv(p)), reads=[p], writes=[MTn])
                    for c in range(NCH):
                        Mn = M[c][nxt]
                        TTc = TT[c]
                        p = mm4(lambda h, Mn=Mn: Mn.t[:, h, :], lambda h, TTc=TTc: TTc.t[:, h, :], [Mn, TTc])
                        S.op("dve", lambda e, p=p, TTc=TTc: e.tensor_tensor(out=TTc.t[:], in0=pv(p), in1=TTc.t[:], op=ALU.add), reads=[p, TTc], writes=[TTc])
                    cur = nxt

                def gn_out(c):
                    tc = tcs[c]
                    Y_ = YS[c]
                    S.op("dve", lambda e: e.tensor_reduce(out=s1.t[:], in_=Y_.t[:], axis=AX.X, op=ALU.add), reads=[Y_], writes=[s1])
                    S.op("pool", lambda e: e.tensor_tensor(out=SQ.t[:], in0=Y_.t[:], in1=Y_.t[:], op=ALU.mult), reads=[Y_], writes=[SQ])
                    S.op("dve", lambda e: e.tensor_reduce(out=s2.t[:], in_=SQ.t[:], axis=AX.X, op=ALU.add), reads=[SQ], writes=[s2])
                    S.op("dve", lambda e: e.tensor_scalar(out=mean.t[:], in0=s1.t[:], scalar1=1.0 / 64, scalar2=None, op0=ALU.mult), reads=[s1], writes=[mean])
                    S.op("dve", lambda e: e.tensor_tensor(out=s1.t[:], in0=mean.t[:], in1=mean.t[:], op=ALU.mult), reads=[mean], writes=[s1])
                    S.op("dve", lambda e: e.scalar_tensor_tensor(out=rstd.t[:], in0=s2.t[:], scalar=1.0 / 64, in1=s1.t[:], op0=ALU.mult, op1=ALU.subtract),
                         reads=[s2, s1], writes=[rstd])
                    S.op("dve", lambda e: e.tensor_scalar(out=rstd.t[:], in0=rstd.t[:], scalar1=GN_EPS, scalar2=None, op0=ALU.add), reads=[rstd], writes=[rstd])
                    S.op("act", lambda e: e.activation(out=rstd.t[:], in_=rstd.t[:], func=AF.Ln), reads=[rstd], writes=[rstd])
                    S.op("act", lambda e: e.activation(out=rstd.t[:], in_=rstd.t[:], func=AF.Exp, scale=-0.5), reads=[rstd], writes=[rstd])
                    S.op("pool", lambda e: e.tensor_tensor(out=Y_.t[:], in0=Y_.t[:], in1=bc3(mean.t[:, :], 64, last=False), op=ALU.subtract),
                         reads=[Y_, mean], writes=[Y_])
                    S.op("pool", lambda e: e.tensor_tensor(out=Y_.t[:], in0=Y_.t[:], in1=bc3(rstd.t[:, :], 64, last=False), op=ALU.mult),
                         reads=[Y_, rstd], writes=[Y_])
                    pt = S.ps()
                    for h in range(NH):
                        S.op("pe", lambda e, pt=pt, h=h: e.transpose(pt.t[0:64, h * 64:(h + 1) * 64], Y_.t[:, h, :], id64), reads=[Y_, CONST], writes=[pt])
                    for h in range(NH):
                        H = H0 + h
                        S.op("act", lambda e, pt=pt, h=h, H=H: e.activation(out=YN.t[:, h, tc], in_=pt.t[0:64, h * 64:(h + 1) * 64], func=AF.Identity,
                                                                           scale=LNW.t[:, H:H + 1], bias=LNB.t[:, H:H + 1]), reads=[pt, LNW, LNB], writes=[YN])

                for c in range(NCH):
                    tc = tcs[c]
                    p = S.ps()
                    for h in range(NH):
                        S.op("pe", lambda e, p=p, h=h: e.matmul(p.t[0:64, h * 64:(h + 1) * 64], lhsT=KKD.t[:, h, tc], rhs=ST.t[:, H0 + h, :],
                                                              start=True, stop=False), reads=[KKD, ST], writes=[p])
                        S.op("pe", lambda e, p=p, h=h: e.matmul(p.t[0:64, h * 64:(h + 1) * 64], lhsT=BTm[c].t[:, h, :], rhs=VT.t[:, c, h, :],
                                                              start=False, stop=True), reads=[BTm[c], VT], writes=[p])
                    S.op("act", lambda e, p=p: e.activation(out=XS.t[:], in_=pv(p), func=AF.Copy), reads=[p], writes=[XS])
                    TTc = TT[c]
                    p = mm4(lambda h: TTc.t[:, h, :], lambda h: XS.t[:, h, :], [TTc, XS])
                    S.op("dve", lambda e, p=p: e.tensor_scalar(out=US.t[:], in0=pv(p), scalar1=-1.0, scalar2=None, op0=ALU.mult), reads=[p], writes=[US])
                    p = S.ps()
                    for h in range(NH):
                        o = p.t[0:64, h * 64:(h + 1) * 64]
                        S.op("pe", lambda e, o=o, h=h: e.matmul(o, lhsT=BIT.t[:, c, h, :], rhs=US.t[:, h, :], start=True, stop=False),
                             reads=[BIT, US], writes=[p])
                        S.op("pe", lambda e, o=o, h=h: e.matmul(o, lhsT=KIT.t[:, c, h, :], rhs=VT.t[:, c, h, :], start=False, stop=True),
                             reads=[KIT, VT], writes=[p])
                    py = S.ps()
                    for h in range(NH):
                        o = py.t[0:64, h * 64:(h + 1) * 64]
                        S.op("pe", lambda e, o=o, h=h: e.matmul(o, lhsT=RD.t[:, h, tc], rhs=ST.t[:, H0 + h, :], start=True, stop=False),
                             reads=[RD, ST], writes=[py])
                        S.op("pe", lambda e, o=o, h=h: e.matmul(o, lhsT=PTm[c].t[:, h, :], rhs=US.t[:, h, :], start=False, stop=False),
                             reads=[PTm[c], US], writes=[py])
                        S.op("pe", lambda e, o=o, h=h: e.matmul(o, lhsT=QTm[c].t[:, h, :], rhs=VT.t[:, c, h, :], start=False, stop=True),
                             reads=[QTm[c], VT], writes=[py])
                    S.op("dve", lambda e, p=p: e.tensor_tensor(out=TMP.t[:], in0=pv(p), in1=ST.t[:, H0:H0 + NH, :], op=ALU.add),
                         reads=[p, ST], writes=[TMP])
                    S.op("dve", lambda e, c=c: e.tensor_tensor(out=ST.t[:, H0:H0 + NH, :], in0=TMP.t[:],
                                                               in1=GAM.t[:, :, c * C + C - 1:c * C + C].broadcast_to([64, NH, 64]), op=ALU.mult),
                         reads=[TMP, GAM], writes=[ST])
                    S.op("act", lambda e, py=py, c=c: e.activation(out=YS[c].t[:], in_=pv(py), func=AF.Copy), reads=[py], writes=[YS[c]])
                    if c > 0:
                        gn_out(c - 1)
                gn_out(NCH - 1)
                S.op("dve", lambda e: e.tensor_tensor(out=YN.t[:], in0=YN.t[:], in1=BON.t[:], op=ALU.add), reads=[YN, BON], writes=[YN])
                S.op("dve", lambda e: e.tensor_tensor(out=YN.t[:], in0=YN.t[:], in1=G_.t[:], op=ALU.mult), reads=[YN, G_], writes=[YN])
                S.dma("sp", lambda e, c0=c0, t0=t0: e.dma_start(
                    out=io["YT"][c0:c0 + NH * 64, t0:t0 + NT].rearrange("(h i) t -> i h t", i=64), in_=YN.t[:]), reads=[YN], writes=[])
        S.flush()


def phase_attn(nc, S, io, CONST, ident):
    TQ = 1024
    NBQ = TQ // 128
    PT = io["PT"]
    QB = NRW
    KB = NRW + 2048
    VB = NRW + 2048 + 512
    with contextlib.ExitStack() as ph:
        def tl(shape, dt=F32):
            return S.tile(ph, shape, dt)

        OG = tl([64, 32])
        r_ = tl([32, 64])
        S.dma("sp", lambda e: e.dma_start(out=r_.t[:], in_=io["attn_out_g"].rearrange("o (h j) -> (o h) j", j=64)), writes=[r_])
        ps = S.ps()
        S.op("pe", lambda e: e.transpose(ps.t[0:64, 0:32], r_.t[:], ident[0:32, 0:32]), reads=[r_, CONST], writes=[ps])
        S.op("dve", lambda e: e.tensor_copy(out=OG.t[:], in_=ps.t[0:64, 0:32]), reads=[ps], writes=[OG])
        SINK = tl([128, 32])
        S.dma("sp", lambda e: e.dma_start(out=SINK.t[:], in_=io["attn_sinks"][0].partition_broadcast(128)), writes=[SINK])

        QT = tl([64, 4, TQ]); YA = tl([64, 4, TQ]); KT = tl([64, TQ + 128]); VT_ = tl([64, TQ + 128])
        AB = tl([128, 4, 256]); AB0 = tl([128, 4, 256])
        SC = tl([128, 4, 256]); PTS = tl([128, 4, 2, 128]); VTM = tl([128, NBQ + 1, 64])
        O = tl([128, 4, 64]); SQ = tl([128, 4, 64])
        mx = tl([128, 4]); nmx = tl([128, 4]); rs = tl([128, 4]); es = tl([128, 4]); ss = tl([128, 4])
        abv = io["attn_bias"].rearrange("p (v h c) -> p v h c", v=2, h=8)
        for h in range(8):
            S.dma("sp", lambda e, h=h: e.dma_start(out=AB.t[:], in_=abv[:, 0, h, :].rearrange("p (g k) -> p g k", g=4)), writes=[AB])
            S.dma("sp", lambda e, h=h: e.dma_start(out=AB0.t[:], in_=abv[:, 1, h, :].rearrange("p (g k) -> p g k", g=4)), writes=[AB0])
            for tq in range(T // TQ):
                t0 = tq * TQ
                S.dma("sp", lambda e, h=h, t0=t0: e.dma_start(
                    out=QT.t[:], in_=PT[QB + h * 256:QB + (h + 1) * 256, t0:t0 + TQ].rearrange("(g d) t -> d g t", d=64)), writes=[QT])
                for (dst, base) in ((KT, KB), (VT_, VB)):
                    if tq == 0:
                        S.op("pool", lambda e, dst=dst: e.memset(dst.t[:, 0:128], 0.0), writes=[dst])
                        S.dma("sp", lambda e, dst=dst, base=base, h=h: e.dma_start(
                            out=dst.t[:, 128:128 + TQ], in_=PT[base + h * 64:base + (h + 1) * 64, 0:TQ]), writes=[dst])
                    else:
                        S.dma("sp", lambda e, dst=dst, base=base, h=h, t0=t0: e.dma_start(
                            out=dst.t[:], in_=PT[base + h * 64:base + (h + 1) * 64, t0 - 128:t0 + TQ]), writes=[dst])
                for half in range(2):
                    blks = list(range(half * 8, min(NBQ + 1, half * 8 + 8)))
                    pv_ = S.ps()
                    for i, bk in enumerate(blks):
                        S.op("pe", lambda e, pv_=pv_, i=i, bk=bk: e.transpose(pv_.t[:, i * 64:(i + 1) * 64], VT_.t[:, bk * 128:(bk + 1) * 128],
                                                                             ident[0:64, 0:64]), reads=[VT_, CONST], writes=[pv_])
                    nb_ = len(blks)
                    S.op("dve", lambda e, pv_=pv_, b0=blks[0], nb_=nb_: e.tensor_copy(
                        out=VTM.t[:, b0:b0 + nb_, :], in_=pv_.t[:, 0:nb_ * 64].rearrange("p (b d) -> p b d", d=64)), reads=[pv_], writes=[VTM])
                for n in range(NBQ):
                    qs = slice(n * 128, (n + 1) * 128)
                    ABn = AB0 if (tq == 0 and n == 0) else AB
                    pss = [S.ps(), S.ps()]
                    for g in range(4):
                        S.op("pe", lambda e, g=g, p=pss[g // 2]: e.matmul(p.t[:, (g % 2) * 256:(g % 2 + 1) * 256], lhsT=QT.t[:, g, qs],
                                                                        rhs=KT.t[:, n * 128:n * 128 + 256], start=True, stop=True),
                             reads=[QT, KT], writes=[pss[g // 2]])
                    for b2 in range(2):
                        S.op("dve", lambda e, b2=b2, p=pss[b2], ABn=ABn: e.scalar_tensor_tensor(
                            out=SC.t[:, 2 * b2:2 * b2 + 2, :], in0=p.t[:, 0:512].rearrange("p (g k) -> p g k", g=2), scalar=0.125,
                            in1=ABn.t[:, 2 * b2:2 * b2 + 2, :], op0=ALU.mult, op1=ALU.add), reads=[pss[b2], ABn], writes=[SC])
                    S.op("dve", lambda e: e.tensor_reduce(out=mx.t[:], in_=SC.t[:], axis=AX.X, op=ALU.max), reads=[SC], writes=[mx])
                    S.op("dve", lambda e, h=h: e.tensor_tensor(out=mx.t[:], in0=mx.t[:], in1=SINK.t[:, 4 * h:4 * h + 4], op=ALU.max),
                         reads=[mx, SINK], writes=[mx])
                    S.op("dve", lambda e: e.tensor_scalar(out=nmx.t[:], in0=mx.t[:], scalar1=-1.0, scalar2=None, op0=ALU.mult), reads=[mx], writes=[nmx])
                    for g in range(4):
                        S.op("act", lambda e, g=g: e.activation(out=SC.t[:, g, :], in_=SC.t[:, g, :], func=AF.Exp, bias=nmx.t[:, g:g + 1],
                                                                accum_out=rs.t[:, g:g + 1]), reads=[SC, nmx], writes=[SC, rs])
                    S.op("dve", lambda e, h=h: e.tensor_tensor(out=es.t[:], in0=SINK.t[:, 4 * h:4 * h + 4], in1=mx.t[:], op=ALU.subtract),
                         reads=[SINK, mx], writes=[es])
                    S.op("act", lambda e: e.activation(out=es.t[:], in_=es.t[:], func=AF.Exp), reads=[es], writes=[es])
                    S.op("dve", lambda e: e.tensor_tensor(out=rs.t[:], in0=rs.t[:], in1=es.t[:], op=ALU.add), reads=[rs, es], writes=[rs])
                    S.op("dve", lambda e: e.reciprocal(out=rs.t[:], in_=rs.t[:]), reads=[rs], writes=[rs])
                    for b2 in range(2):
                        pt = S.ps()
                        for gi in range(2):
                            g = 2 * b2 + gi
                            for kh in range(2):
                                S.op("pe", lambda e, pt=pt, g=g, gi=gi, kh=kh: e.transpose(
                                    pt.t[:, (gi * 2 + kh) * 128:(gi * 2 + kh + 1) * 128], SC.t[:, g, kh * 128:(kh + 1) * 128], ident),
                                    reads=[SC, CONST], writes=[pt])
                        if b2 == 0:
                            S.op("act", lambda e, pt=pt, b2=b2: e.activation(out=PTS.t[:, 2 * b2:2 * b2 + 2, :, :],
                                                                             in_=pt.t[:, 0:512].rearrange("p (g k q) -> p g k q", g=2, k=2), func=AF.Copy),
                                 reads=[pt], writes=[PTS])
                        else:
                            S.op("dve", lambda e, pt=pt, b2=b2: e.tensor_copy(out=PTS.t[:, 2 * b2:2 * b2 + 2, :, :],
                                                                              in_=pt.t[:, 0:512].rearrange("p (g k q) -> p g k q", g=2, k=2)),
                                 reads=[pt], writes=[PTS])
                    po = S.ps()
                    for g in range(4):
                        for kh in range(2):
                            S.op("pe", lambda e, g=g, kh=kh, po=po: e.matmul(po.t[:, g * 64:(g + 1) * 64], lhsT=PTS.t[:, g, kh, :], rhs=VTM.t[:, n + kh, :],
                                                                           start=(kh == 0), stop=(kh == 1)), reads=[PTS, VTM], writes=[po])
                    S.op("dve", lambda e, po=po: e.tensor_tensor(out=O.t[:], in0=po.t[:, 0:256].rearrange("p (g d) -> p g d", g=4),
                                                                 in1=rs.t[:, :].unsqueeze(2).broadcast_to([128, 4, 64]), op=ALU.mult),
                         reads=[po, rs], writes=[O])
                    S.op("pool", lambda e: e.tensor_tensor(out=SQ.t[:], in0=O.t[:], in1=O.t[:], op=ALU.mult), reads=[O], writes=[SQ])
                    S.op("dve", lambda e: e.tensor_reduce(out=ss.t[:], in_=SQ.t[:], axis=AX.X, op=ALU.add), reads=[SQ], writes=[ss])
                    S.op("dve", lambda e: e.tensor_scalar(out=ss.t[:], in0=ss.t[:], scalar1=1.0 / 64, scalar2=EPS, op0=ALU.mult, op1=ALU.add),
                         reads=[ss], writes=[ss])
                    S.op("act", lambda e: e.activation(out=ss.t[:], in_=ss.t[:], func=AF.Sqrt), reads=[ss], writes=[ss])
                    S.op("dve", lambda e: e.reciprocal(out=ss.t[:], in_=ss.t[:]), reads=[ss], writes=[ss])
                    S.op("dve", lambda e: e.tensor_tensor(out=O.t[:], in0=O.t[:], in1=ss.t[:, :].unsqueeze(2).broadcast_to([128, 4, 64]), op=ALU.mult),
                         reads=[O, ss], writes=[O])
                    pT = S.ps()
                    for g in range(4):
                        S.op("pe", lambda e, g=g, pT=pT: e.transpose(pT.t[0:64, g * 128:(g + 1) * 128], O.t[:, g, :], ident), reads=[O, CONST], writes=[pT])
                    for g in range(4):
                        S.op("act", lambda e, g=g, pT=pT, h=h: e.activation(out=YA.t[:, g, qs], in_=pT.t[0:64, g * 128:(g + 1) * 128], func=AF.Copy,
                                                                           scale=OG.t[:, 4 * h + g:4 * h + g + 1]), reads=[pT, OG], writes=[YA])
                S.dma("sp", lambda e, h=h, t0=t0: e.dma_start(
                    out=io["YT"][2048 + h * 256:2048 + (h + 1) * 256, t0:t0 + TQ].rearrange("(g d) t -> d g t", d=64), in_=YA.t[:]),
                    reads=[YA], writes=[])
        S.flush()


def phase_wout(nc, S, io, CONST, ident, A2, SH2, norm_tile, gs=None):
    TQ = 1024
    X1 = io["X1"]
    MOD = io["MODROW"]
    with contextlib.ExitStack() as ph:
        def tl(shape, dt=F32):
            return S.tile(ph, shape, dt)
        G1 = tl([128, D])
        S.dma("sp", lambda e: e.dma_start(out=G1.t[:], in_=MOD[0, 2 * D:3 * D].partition_broadcast(128)), writes=[G1])
        yT = tl([128, 32, TQ], BF16)
        wos = [tl([128, 32, 512], BF16) for _ in range(2)]
        yTv = [Tl(yT.t) for _ in range(4)]
        wosv = [[Tl(w_.t) for _ in range(4)] for w_ in wos]
        xps = [tl([128, 512]) for _ in range(3)]
        tmps = [tl([128, 512]) for _ in range(2)]
        nw = 0
        nx = 0
        for tq in range(T // TQ):
            t0 = tq * TQ
            for kh in range(4):
                S.dma("pool", lambda e, t0=t0, kh=kh: e.dma_start(
                    out=yT.t[:, kh * 8:(kh + 1) * 8, :],
                    in_=io["YT"][kh * 1024:(kh + 1) * 1024, t0:t0 + TQ].rearrange("(k p) t -> p k t", p=128)), writes=[yTv[kh]])
            for cg in range(8):
                wo = wos[nw % 2]
                wov = wosv[nw % 2]
                nw += 1
                for kh in range(4):
                    S.dma("pool", lambda e, wo=wo, cg=cg, kh=kh: e.dma_start(
                        out=wo.t[:, kh * 8:(kh + 1) * 8, :],
                        in_=io["w_out"][kh * 1024:(kh + 1) * 1024, cg * 512:(cg + 1) * 512].rearrange("(k p) j -> p k j", p=128)), writes=[wov[kh]])
                for tt in range(TQ // 128):
                    r0 = t0 + tt * 128
                    xp = xps[nx % 3]
                    tmp = tmps[nx % 2]
                    nx += 1
                    S.dma("sp", lambda e, xp=xp, r0=r0, cg=cg: e.dma_start(out=xp.t[:], in_=io["x"][r0:r0 + 128, cg * 512:(cg + 1) * 512]), writes=[xp])
                    ps = S.ps()
                    for kc in range(32):
                        S.op("pe", lambda e, ps=ps, wo=wo, kc=kc, tt=tt: e.matmul(
                            ps.t[:, :], lhsT=yT.t[:, kc, tt * 128:(tt + 1) * 128], rhs=wo.t[:, kc, :], start=(kc == 0), stop=(kc == 31)),
                            reads=[yTv[kc // 8], wov[kc // 8]], writes=[ps])
                    S.op("dve", lambda e, ps=ps, tmp=tmp, cg=cg: e.tensor_tensor(out=tmp.t[:], in0=ps.t[:, :], in1=G1.t[:, cg * 512:(cg + 1) * 512], op=ALU.mult),
                         reads=[ps, G1], writes=[tmp])
                    S.op("pool", lambda e, tmp=tmp, xp=xp: e.tensor_tensor(out=xp.t[:], in0=tmp.t[:], in1=xp.t[:], op=ALU.add), reads=[tmp, xp], writes=[xp])
                    S.dma("sp", lambda e, xp=xp, r0=r0, cg=cg: e.dma_start(out=X1[r0:r0 + 128, cg * 512:(cg + 1) * 512], in_=xp.t[:]), reads=[xp], writes=[])
        S.flush()

    R_ = {}
    io["_route"] = R_
    ms = contextlib.ExitStack()
    io["_moe_stack"] = ms
    io["_IDXG"] = S.tile(ms, [128, NB], I32)
    io["_IDXD"] = S.tile(ms, [128, NB], I32)
    io["_CBF"] = S.tile(ms, [128, 128], BF16)
    gs = contextlib.ExitStack()
    io["_route_stack"] = gs
    R_["OH1"] = S.tile(gs, [128, 32, 64]); R_["OH2"] = S.tile(gs, [128, 32, 64]); R_["RANK"] = S.tile(gs, [128, 32, 64])
    R_["W1"] = S.tile(gs, [128, 32]); R_["W2"] = S.tile(gs, [128, 32]); R_["SELSUM"] = S.tile(gs, [128, 64])
    OH1, OH2, RANK, W1, W2, SELSUM = R_["OH1"], R_["OH2"], R_["RANK"], R_["W1"], R_["W2"], R_["SELSUM"]
    TRI = CONST.t[:, C_TRI:C_TRI + 128]
    ONES = CONST.t[:, C_ONES:C_ONES + 128]
    with contextlib.ExitStack() as ph:
        def tl(shape, dt=F32):
            return S.tile(ph, shape, dt)
        A2R = tl([128, D]); SH2R = tl([128, D])
        xts = [tl([128, D]) for _ in range(2)]
        scr = tl([128, D], BF16)
        ssq = tl([128, 1]); rstd = tl([128, 1])
        S.dma("sp", lambda e: e.dma_start(out=xts[0].t[:], in_=io["norm2_g"][0].partition_broadcast(128)), writes=[xts[0]])
        S.dma("sp", lambda e: e.dma_start(out=A2R.t[:], in_=MOD[0, 4 * D:5 * D].partition_broadcast(128)), writes=[A2R])
        S.dma("sp", lambda e: e.dma_start(out=SH2R.t[:], in_=MOD[0, 3 * D:4 * D].partition_broadcast(128)), writes=[SH2R])
        S.op("dve", lambda e: e.scalar_tensor_tensor(out=A2R.t[:], in0=A2R.t[:], scalar=1.0, in1=xts[0].t[:], op0=ALU.add, op1=ALU.mult),
             reads=[A2R, xts[0]], writes=[A2R])
        RT = tl([128, 32, 72])
        S.dma("sp", lambda e: e.dma_start(out=RT.t[:], in_=io["router"].rearrange("(k p) j -> p k j", p=128)), writes=[RT])
        RB = tl([128, 72])
        S.dma("sp", lambda e: e.dma_start(out=RB.t[:], in_=io["router_bias"][0].partition_broadcast(128)), writes=[RB])
        uTs = [tl([128, 4, 128]) for _ in range(2)]
        LG = tl([128, 72]); T88 = tl([128, 8, 8]); SEL = tl([128, 64])
        mg = tl([128, 1]); nmg = tl([128, 1]); sg = tl([128, 1]); eg = tl([128, 8]); ohg = tl([128, 8]); le = tl([128, 8]); le2 = tl([128, 8])
        m1 = tl([128, 1]); m2 = tl([128, 1]); oh1 = tl([128, 8]); oh2 = tl([128, 8]); dd = tl([128, 1]); w1 = tl([128, 1])
        S.op("pool", lambda e: e.memset(SELSUM.t[:], 0.0), writes=[SELSUM])
        zr = tl([1, D], BF16)
        S.op("pool", lambda e: e.memset(zr.t[:], 0.0), writes=[zr])
        S.dma("sp", lambda e: e.dma_start(out=io["U2"][T:T + 1, :], in_=zr.t[:]), reads=[zr], writes=[])
        nu = 0
        for tt in range(T // 128):
            xt = xts[tt % 2]
            r0 = tt * 128
            S.dma("sp", lambda e, xt=xt, r0=r0: e.dma_start(out=xt.t[:], in_=X1[r0:r0 + 128, :]), writes=[xt])
            norm_tile(xt, scr, ssq, rstd)
            S.op("dve", lambda e, xt=xt: e.tensor_tensor(out=xt.t[:], in0=xt.t[:], in1=A2R.t[:], op=ALU.mult), reads=[xt, A2R], writes=[xt])
            S.op("pool", lambda e, xt=xt: e.tensor_tensor(out=xt.t[:], in0=xt.t[:], in1=SH2R.t[:], op=ALU.add), reads=[xt, SH2R], writes=[xt])
            for hf in range(2):
                S.dma("pool", lambda e, xt=xt, r0=r0, hf=hf: e.dma_start(out=io["U2"][r0:r0 + 128, hf * 2048:(hf + 1) * 2048],
                                                                        in_=xt.t[:, hf * 2048:(hf + 1) * 2048]), reads=[xt], writes=[])
            pl = S.ps(hold=True)
            for kg in range(8):
                pt = S.ps()
                for k4 in range(4):
                    kc = kg * 4 + k4
                    S.op("pe", lambda e, pt=pt, xt=xt, kc=kc, k4=k4: e.transpose(pt.t[:, k4 * 128:(k4 + 1) * 128], xt.t[:, kc * 128:(kc + 1) * 128], ident),
                         reads=[xt, CONST], writes=[pt])
                uT = uTs[nu % 2]
                nu += 1
                if kg % 2 == 0:
                    S.op("dve", lambda e, pt=pt, uT=uT: e.tensor_copy(out=uT.t[:], in_=pt.t[:, :].rearrange("p (k t) -> p k t", k=4)), reads=[pt], writes=[uT])
                else:
                    S.op("act", lambda e, pt=pt, uT=uT: e.activation(out=uT.t[:], in_=pt.t[:, :].rearrange("p (k t) -> p k t", k=4), func=AF.Copy),
                         reads=[pt], writes=[uT])
                for k4 in range(4):
                    kc = kg * 4 + k4
                    S.op("pe", lambda e, pl=pl, uT=uT, kc=kc, k4=k4: e.matmul(pl.t[:, 0:72], lhsT=uT.t[:, k4, :], rhs=RT.t[:, kc, :],
                                                                             start=(kc == 0), stop=(kc == 31)), reads=[uT, RT], writes=[pl])
            S.ps_release(pl)
            S.op("dve", lambda e, pl=pl: e.tensor_tensor(out=LG.t[:], in0=pl.t[:, 0:72], in1=RB.t[:], op=ALU.add), reads=[pl, RB], writes=[LG])
            S.op("dve", lambda e: e.tensor_reduce(out=mg.t[:], in_=LG.t[:, 0:8], axis=AX.X, op=ALU.max), reads=[LG], writes=[mg])
            S.op("dve", lambda e: e.tensor_scalar(out=nmg.t[:], in0=mg.t[:], scalar1=-1.0, scalar2=None, op0=ALU.mult), reads=[mg], writes=[nmg])
            S.op("act", lambda e: e.activation(out=eg.t[:], in_=LG.t[:, 0:8], func=AF.Exp, bias=nmg.t[:, 0:1], accum_out=sg.t[:, 0:1]),
                 reads=[LG, nmg], writes=[eg, sg])
            S.op("dve", lambda e: e.reciprocal(out=sg.t[:], in_=sg.t[:]), reads=[sg], writes=[sg])
            S.op("dve", lambda e: e.tensor_scalar(out=ohg.t[:], in0=LG.t[:, 0:8], scalar1=mg.t[:, 0:1], scalar2=None, op0=ALU.is_equal),
                 reads=[LG, mg], writes=[ohg])
            S.op("dve", lambda e: e.tensor_tensor(out=T88.t[:], in0=LG.t[:, 8:72].rearrange("p (g x) -> p g x", g=8),
                                                  in1=ohg.t[:, :].unsqueeze(2).broadcast_to([128, 8, 8]), op=ALU.mult), reads=[LG, ohg], writes=[T88])
            S.op("dve", lambda e: e.tensor_reduce(out=le.t[:], in_=T88.t[:].rearrange("p g x -> p x g"), axis=AX.X, op=ALU.add), reads=[T88], writes=[le])
            S.op("dve", lambda e: e.tensor_reduce(out=m1.t[:], in_=le.t[:], axis=AX.X, op=ALU.max), reads=[le], writes=[m1])
            S.op("dve", lambda e: e.tensor_scalar(out=oh1.t[:], in0=le.t[:], scalar1=m1.t[:, 0:1], scalar2=None, op0=ALU.is_equal), reads=[le, m1], writes=[oh1])
            S.op("dve", lambda e: e.scalar_tensor_tensor(out=le2.t[:], in0=oh1.t[:], scalar=-1e30, in1=le.t[:], op0=ALU.mult, op1=ALU.add),
                 reads=[oh1, le], writes=[le2])
            S.op("dve", lambda e: e.tensor_reduce(out=m2.t[:], in_=le2.t[:], axis=AX.X, op=ALU.max), reads=[le2], writes=[m2])
            S.op("dve", lambda e: e.tensor_scalar(out=oh2.t[:], in0=le2.t[:], scalar1=m2.t[:, 0:1], scalar2=None, op0=ALU.is_equal), reads=[le2, m2], writes=[oh2])
            S.op("dve", lambda e: e.tensor_tensor(out=dd.t[:], in0=m2.t[:], in1=m1.t[:], op=ALU.subtract), reads=[m1, m2], writes=[dd])
            S.op("act", lambda e: e.activation(out=dd.t[:], in_=dd.t[:], func=AF.Exp), reads=[dd], writes=[dd])
            S.op("dve", lambda e: e.tensor_scalar(out=w1.t[:], in0=dd.t[:], scalar1=1.0, scalar2=None, op0=ALU.add), reads=[dd], writes=[w1])
            S.op("dve", lambda e: e.reciprocal(out=w1.t[:], in_=w1.t[:]), reads=[w1], writes=[w1])
            S.op("dve", lambda e: e.tensor_tensor(out=dd.t[:], in0=dd.t[:], in1=w1.t[:], op=ALU.mult), reads=[dd, w1], writes=[dd])
            S.op("dve", lambda e, tt=tt: e.tensor_tensor(out=W1.t[:, tt:tt + 1], in0=w1.t[:], in1=sg.t[:], op=ALU.mult), reads=[w1, sg], writes=[W1])
            S.op("dve", lambda e, tt=tt: e.tensor_tensor(out=W2.t[:, tt:tt + 1], in0=dd.t[:], in1=sg.t[:], op=ALU.mult), reads=[dd, sg], writes=[W2])
            for (oh, OHA) in ((oh1, OH1), (oh2, OH2)):
                S.op("dve", lambda e, oh=oh, OHA=OHA, tt=tt: e.tensor_tensor(
                    out=OHA.t[:, tt, :].rearrange("p (g x) -> p g x", g=8), in0=ohg.t[:, :].unsqueeze(2).broadcast_to([128, 8, 8]),
                    in1=oh.t[:, :].unsqueeze(1).broadcast_to([128, 8, 8]), op=ALU.mult), reads=[ohg, oh], writes=[OHA])
            S.op("dve", lambda e, tt=tt: e.tensor_tensor(out=SEL.t[:], in0=OH1.t[:, tt, :], in1=OH2.t[:, tt, :], op=ALU.add), reads=[OH1, OH2], writes=[SEL])
            pr = S.ps()
            S.op("pe", lambda e, pr=pr: e.matmul(pr.t[:, 0:64], lhsT=TRI, rhs=SEL.t[:], start=True, stop=False), reads=[SEL, CONST], writes=[pr])
            S.op("pe", lambda e, pr=pr: e.matmul(pr.t[:, 0:64], lhsT=ONES, rhs=SELSUM.t[:], start=False, stop=True), reads=[SELSUM, CONST], writes=[pr])
            S.op("act", lambda e, pr=pr, tt=tt: e.activation(out=RANK.t[:, tt, :], in_=pr.t[:, 0:64], func=AF.Copy), reads=[pr], writes=[RANK])
            S.op("pool", lambda e: e.tensor_tensor(out=SELSUM.t[:], in0=SELSUM.t[:], in1=SEL.t[:], op=ALU.add), reads=[SELSUM, SEL], writes=[SELSUM])
        S.flush()


def phase_moe(nc, S, io, CONST, ident):
    R_ = io["_route"]
    OH1, OH2, RANK, W1, W2, SELSUM = R_["OH1"], R_["OH2"], R_["RANK"], R_["W1"], R_["W2"], R_["SELSUM"]
    ONES = CONST.t[:, C_ONES:C_ONES + 128]
    K128 = CONST.t[:, C_K128:C_K128 + 64]
    PID = CONST.t[:, C_PID:C_PID + 1]
    P128 = CONST.t[:, C_P128:C_P128 + 1]
    ROWTOK = io["ROWTOK"]
    ROWW = io["ROWW"]
    YACC = io["YACC"]
    U2 = io["U2"]
    IOA = bass.IndirectOffsetOnAxis
    with contextlib.ExitStack() as ph:
        def tl(shape, dt=F32):
            return S.tile(ph, shape, dt)
        IDXG = io["_IDXG"]
        CONST_BF = io["_CBF"]
        S.op("dve", lambda e: e.tensor_copy(out=CONST_BF.t[:], in_=ident), reads=[CONST], writes=[CONST_BF])
        IDXD = io["_IDXD"]
        with contextlib.ExitStack() as p2:
            def t2(shape, dt=F32):
                return S.tile(p2, shape, dt)
            cnt = t2([64, 1]); nblk = t2([64, 1]); cmp = t2([64, 64]); pbc = t2([64, 128])
            PST = t2([128, 64]); PEND = t2([128, 64])
            pc = S.ps()
            S.op("pe", lambda e: e.matmul(pc.t[0:64, 0:128], lhsT=SELSUM.t[:], rhs=ONES, start=True, stop=True), reads=[SELSUM, CONST], writes=[pc])
            S.op("dve", lambda e: e.tensor_copy(out=cnt.t[:], in_=pc.t[0:64, 0:1]), reads=[pc], writes=[cnt])
            S.op("dve", lambda e: e.tensor_scalar(out=cmp.t[:], in0=K128[0:64, :], scalar1=cnt.t[:, 0:1], scalar2=None, op0=ALU.is_lt),
                 reads=[cnt, CONST], writes=[cmp])
            S.op("dve", lambda e: e.tensor_reduce(out=nblk.t[:], in_=cmp.t[:], axis=AX.X, op=ALU.add), reads=[cmp], writes=[nblk])
            S.op("dve", lambda e: e.tensor_scalar(out=nblk.t[:], in0=nblk.t[:], scalar1=128.0, scalar2=None, op0=ALU.mult), reads=[nblk], writes=[nblk])
            S.op("dve", lambda e: e.tensor_scalar(out=pbc.t[:], in0=ONES[0:64, :], scalar1=nblk.t[:, 0:1], scalar2=None, op0=ALU.mult),
                 reads=[nblk, CONST], writes=[pbc])
            pp = S.ps()
            S.op("pe", lambda e: e.matmul(pp.t[:, 0:64], lhsT=pbc.t[:], rhs=CONST.t[0:64, C_SU:C_SU + 64], start=True, stop=True), reads=[pbc, CONST], writes=[pp])
            S.op("dve", lambda e: e.tensor_copy(out=PST.t[:], in_=pp.t[:, 0:64]), reads=[pp], writes=[PST])
            pe_ = S.ps()
            S.op("pe", lambda e: e.matmul(pe_.t[:, 0:64], lhsT=pbc.t[:], rhs=CONST.t[0:64, C_SUI:C_SUI + 64], start=True, stop=True), reads=[pbc, CONST], writes=[pe_])
            S.op("dve", lambda e: e.tensor_copy(out=PEND.t[:], in_=pe_.t[:, 0:64]), reads=[pe_], writes=[PEND])
            TMPA = t2([128, 32, 64]); TMPB = t2([128, 32, 64])
            DEST = [t2([128, 32]), t2([128, 32])]
            DESTI = [t2([128, 32], I32), t2([128, 32], I32)]
            S.op("dve", lambda e: e.tensor_tensor(out=TMPA.t[:], in0=RANK.t[:], in1=PST.t[:, :].unsqueeze(1).broadcast_to([128, 32, 64]), op=ALU.add),
                 reads=[RANK, PST], writes=[TMPA])
            for k, OH in enumerate((OH1, OH2)):
                S.op("dve", lambda e, OH=OH: e.tensor_tensor(out=TMPB.t[:], in0=TMPA.t[:], in1=OH.t[:], op=ALU.mult), reads=[TMPA, OH], writes=[TMPB])
                S.op("dve", lambda e, k=k: e.tensor_reduce(out=DEST[k].t[:], in_=TMPB.t[:], axis=AX.X, op=ALU.add), reads=[TMPB], writes=[DEST[k]])
                S.op("dve", lambda e, k=k: e.tensor_copy(out=DESTI[k].t[:], in_=DEST[k].t[:]), reads=[DEST[k]], writes=[DESTI[k]])
            TOK = t2([128, 32])
            S.op("dve", lambda e: e.tensor_scalar(out=TOK.t[:], in0=K128[:, 0:32], scalar1=PID, scalar2=None, op0=ALU.add), reads=[CONST], writes=[TOK])
            TOK16 = t2([128, 32, 16])
            S.op("dve", lambda e: e.tensor_copy(out=TOK16.t[:], in_=TOK.t[:, :].unsqueeze(2).broadcast_to([128, 32, 16])), reads=[TOK], writes=[TOK16])
            W16 = [t2([128, 32, 16]), t2([128, 32, 16])]
            for k, W in enumerate((W1, W2)):
                S.op("dve", lambda e, k=k, W=W: e.tensor_copy(out=W16[k].t[:], in_=W.t[:, :].unsqueeze(2).broadcast_to([128, 32, 16])), reads=[W], writes=[W16[k]])
            FILL = t2([128, 128, 16])
            S.op("pool", lambda e: e.memset(FILL.t[:], float(T)), writes=[FILL])
            S.dma("sp", lambda e: e.dma_start(out=ROWTOK.rearrange("(p r) c -> p r c", p=128), in_=FILL.t[:]), reads=[FILL], writes=[])
            FILL0 = t2([128, 128, 16])
            S.op("pool", lambda e: e.memset(FILL0.t[:], 0.0), writes=[FILL0])
            S.dma("sp", lambda e: e.dma_start(out=ROWW.rearrange("(p r) c -> p r c", p=128), in_=FILL0.t[:]), reads=[FILL0], writes=[])
            S.flush()
            for tt in range(32):
                for k in range(2):
                    S.dma("pool", lambda e, tt=tt, k=k: e.indirect_dma_start(
                        out=ROWTOK, out_offset=IOA(ap=DESTI[k].t[:, tt:tt + 1], axis=0), in_=TOK16.t[:, tt, :], in_offset=None),
                        reads=[DESTI[k], TOK16], writes=[])
                    S.dma("pool", lambda e, tt=tt, k=k: e.indirect_dma_start(
                        out=ROWW, out_offset=IOA(ap=DESTI[k].t[:, tt:tt + 1], axis=0), in_=W16[k].t[:, tt, :], in_offset=None),
                        reads=[DESTI[k], W16[k]], writes=[])
            BE = t2([128, 1]); cmp2 = t2([128, 64]); DG = t2([128, 128]); BEROW = t2([128, 128]); BASE = t2([128, 128])
            S.op("dve", lambda e: e.tensor_scalar(out=cmp2.t[:], in0=PEND.t[:], scalar1=P128, scalar2=None, op0=ALU.is_le), reads=[PEND, CONST], writes=[cmp2])
            S.op("dve", lambda e: e.tensor_reduce(out=BE.t[:], in_=cmp2.t[:], axis=AX.X, op=ALU.add), reads=[cmp2], writes=[BE])
            S.op("dve", lambda e: e.tensor_scalar(out=BE.t[:], in0=BE.t[:], scalar1=63.0, scalar2=None, op0=ALU.min), reads=[BE], writes=[BE])
            EMP = t2([128, 1])
            S.op("dve", lambda e: e.tensor_tensor(out=EMP.t[:], in0=P128, in1=PEND.t[:, 63:64], op=ALU.is_ge), reads=[PEND, CONST], writes=[EMP])
            S.op("dve", lambda e: e.scalar_tensor_tensor(out=BE.t[:], in0=EMP.t[:], scalar=1000.0, in1=BE.t[:], op0=ALU.mult, op1=ALU.add),
                 reads=[EMP, BE], writes=[BE])
            S.op("dve", lambda e: e.tensor_scalar(out=DG.t[:], in0=ident, scalar1=BE.t[:, 0:1], scalar2=None, op0=ALU.mult), reads=[BE, CONST], writes=[DG])
            pb = S.ps()
            S.op("pe", lambda e: e.matmul(pb.t[:, 0:128], lhsT=ONES, rhs=DG.t[:], start=True, stop=True), reads=[DG, CONST], writes=[pb])
            S.op("dve", lambda e: e.tensor_copy(out=BEROW.t[:], in_=pb.t[:, 0:128]), reads=[pb], writes=[BEROW])
            S.op("dve", lambda e: e.tensor_scalar(out=BASE.t[:], in0=BEROW.t[:], scalar1=4096.0, scalar2=PID, op0=ALU.mult, op1=ALU.add),
                 reads=[BEROW, CONST], writes=[BASE])
            S.op("dve", lambda e: e.tensor_copy(out=IDXG.t[:], in_=BASE.t[:]), reads=[BASE], writes=[IDXG])
            S.op("dve", lambda e: e.tensor_scalar(out=BASE.t[:], in0=BEROW.t[:], scalar1=512.0, scalar2=PID, op0=ALU.mult, op1=ALU.add),
                 reads=[BEROW, CONST], writes=[BASE])
            S.op("dve", lambda e: e.tensor_scalar(out=BASE.t[:], in0=BASE.t[:], scalar1=2.0, scalar2=None, op0=ALU.mult), reads=[BASE], writes=[BASE])
            S.op("dve", lambda e: e.tensor_copy(out=IDXD.t[:], in_=BASE.t[:]), reads=[BASE], writes=[IDXD])
            S.flush()
        io["_route_stack"].close()

        WG = [tl([128, 8, 512], BF16) for _ in range(3)]
        WU = [tl([128, 8, 512], BF16) for _ in range(3)]
        WDS = [[tl([128, D], BF16) for _ in range(4)] for _ in range(2)]
        WGv = [[Tl(w.t) for _ in range(8)] for w in WG]
        WUv = [[Tl(w.t) for _ in range(8)] for w in WU]
        WDv = [[[Tl(w.t) for _ in range(2)] for w in ws] for ws in WDS]
        xbs = [tl([128, D], BF16) for _ in range(2)]
        xbT = tl([128, 32, 128], BF16)
        hs = tl([128, 512]); hb = tl([128, 512], BF16); hT = tl([128, 4, 128], BF16)
        yb = tl([128, D])
        TIF = [tl([128, 1]) for _ in range(3)]; RW = [tl([128, 1]) for _ in range(3)]
        TI = [tl([128, 1], I32) for _ in range(3)]; TI2 = [tl([128, 2], I32) for _ in range(3)]; TF2 = [tl([128, 2]) for _ in range(3)]
        yacc_dep = tl([1, 1])
        S.op("pool", lambda e: e.memset(yb.t[:], 0.0), writes=[yb])
        for tt in range(T // 128):
            S.dma("sp", lambda e, tt=tt: e.dma_start(out=YACC[tt * 128:(tt + 1) * 128, :], in_=yb.t[:]), reads=[yb], writes=[yacc_dep])
        S.dma("sp", lambda e: e.dma_start(out=YACC[T:T + 1, :], in_=yb.t[0:1, :]), reads=[yb], writes=[yacc_dep])
        S.flush()
        REG_G = nc.gpsimd.alloc_register("bc_gate")
        nc.gpsimd.reg_mov(REG_G, 64 * D)
        REG_D = nc.gpsimd.alloc_register("bc_down")
        nc.gpsimd.reg_mov(REG_D, 70000)
        EWG = io["ew_gate"]; EWU = io["ew_up"]
        EWD = io["ew_down"].rearrange("r (two c) -> (r two) c", two=2)
        YACC2 = YACC.rearrange("r (two c) -> (r two) c", two=2)
        nw = 0

        def emit_idx(i):
            b = i % 3
            S.dma("sp", lambda e: e.dma_start(out=TIF[b].t[:], in_=ROWTOK[i * 128:(i + 1) * 128, 0:1], allow_slow_non_contiguous=True), writes=[TIF[b]])
            S.dma("sp", lambda e: e.dma_start(out=RW[b].t[:], in_=ROWW[i * 128:(i + 1) * 128, 0:1], allow_slow_non_contiguous=True), writes=[RW[b]])
            S.op("dve", lambda e: e.tensor_copy(out=TI[b].t[:], in_=TIF[b].t[:]), reads=[TIF[b]], writes=[TI[b]])
            S.op("dve", lambda e: e.tensor_scalar(out=TF2[b].t[:, 0:1], in0=TIF[b].t[:], scalar1=2.0, scalar2=None, op0=ALU.mult), reads=[TIF[b]], writes=[TF2[b]])
            S.op("dve", lambda e: e.tensor_scalar(out=TF2[b].t[:, 1:2], in0=TIF[b].t[:], scalar1=2.0, scalar2=1.0, op0=ALU.mult, op1=ALU.add),
                 reads=[TIF[b]], writes=[TF2[b]])
            S.op("dve", lambda e: e.tensor_copy(out=TI2[b].t[:], in_=TF2[b].t[:]), reads=[TF2[b]], writes=[TI2[b]])
            xb = xbs[i % 2]
            S.dma("pool", lambda e: e.indirect_dma_start(out=xb.t[:], out_offset=None, in_=U2, in_offset=IOA(ap=TI[b].t[:, 0:1], axis=0)),
                  reads=[TI[b]], writes=[xb])

        def emit_wd(i):
            wd = WDS[i % 2]
            for fc in range(4):
                for hf in range(2):
                    S.dma("pool", lambda e, fc=fc, hf=hf: e.indirect_dma_start(
                        out=wd[fc].t[:, hf * 2048:(hf + 1) * 2048], out_offset=None, in_=EWD, in_offset=IOA(ap=IDXD.t[:, i:i + 1], axis=0),
                        element_offset=(fc * 256 + hf) * 2048, bounds_check=REG_D, oob_is_err=False), reads=[IDXD], writes=[WDv[i % 2][fc][hf]])

        def emit_scatter(i):
            b = i % 3
            for hf in range(2):
                S.dma("pool", lambda e, hf=hf: e.indirect_dma_start(
                    out=YACC2, out_offset=IOA(ap=TI2[b].t[:, hf:hf + 1], axis=0), in_=yb.t[:, hf * 2048:(hf + 1) * 2048], in_offset=None,
                    compute_op=ALU.add), reads=[yb, TI2[b]], writes=[yacc_dep])

        emit_idx(0)
        emit_wd(0)
        for i in range(NB):
            b = i % 3
            if i + 1 < NB:
                emit_idx(i + 1)
                emit_wd(i + 1)
            xb = xbs[i % 2]
            wd = WDS[i % 2]
            for kg in range(8):
                pt = S.ps()
                ptb = pt.t[:, :].bitcast(BF16)
                for k4 in range(4):
                    kc = kg * 4 + k4
                    S.op("pe", lambda e, ptb=ptb, xb=xb, kc=kc, k4=k4: e.transpose(ptb[:, k4 * 128:(k4 + 1) * 128], xb.t[:, kc * 128:(kc + 1) * 128],
                                                                                  CONST_BF.t[:, :]), reads=[xb, CONST_BF], writes=[pt])
                if kg % 2 == 0:
                    S.op("dve", lambda e, ptb=ptb, kg=kg: e.tensor_copy(out=xbT.t[:, kg * 4:(kg + 1) * 4, :],
                                                                        in_=ptb[:, 0:512].rearrange("p (k t) -> p k t", k=4)), reads=[pt], writes=[xbT])
                else:
                    S.op("act", lambda e, ptb=ptb, kg=kg: e.activation(out=xbT.t[:, kg * 4:(kg + 1) * 4, :],
                                                                       in_=ptb[:, 0:512].rearrange("p (k t) -> p k t", k=4), func=AF.Copy), reads=[pt], writes=[xbT])
            pg = S.ps(hold=True)
            pu = S.ps(hold=True)
            for grp in range(4):
                wg = WG[nw % 3]
                wu = WU[nw % 3]
                wgv = WGv[nw % 3]
                wuv = WUv[nw % 3]
                nw += 1
                for k8 in range(8):
                    kc = grp * 8 + k8
                    S.dma("pool", lambda e, wg=wg, i=i, kc=kc, k8=k8: e.indirect_dma_start(
                        out=wg.t[:, k8, :], out_offset=None, in_=EWG, in_offset=IOA(ap=IDXG.t[:, i:i + 1], axis=0), element_offset=kc * 128 * 512,
                        bounds_check=REG_G, oob_is_err=False), reads=[IDXG], writes=[wgv[k8]])
                    S.dma("pool", lambda e, wu=wu, i=i, kc=kc, k8=k8: e.indirect_dma_start(
                        out=wu.t[:, k8, :], out_offset=None, in_=EWU, in_offset=IOA(ap=IDXG.t[:, i:i + 1], axis=0), element_offset=kc * 128 * 512,
                        bounds_check=REG_G, oob_is_err=False), reads=[IDXG], writes=[wuv[k8]])
                for k8 in range(8):
                    kc = grp * 8 + k8
                    S.op("pe", lambda e, pg=pg, wg=wg, kc=kc, k8=k8: e.matmul(pg.t[:, :], lhsT=xbT.t[:, kc, :], rhs=wg.t[:, k8, :], start=(kc == 0), stop=(kc == 31)),
                         reads=[xbT, wgv[k8]], writes=[pg])
                    S.op("pe", lambda e, pu=pu, wu=wu, kc=kc, k8=k8: e.matmul(pu.t[:, :], lhsT=xbT.t[:, kc, :], rhs=wu.t[:, k8, :], start=(kc == 0), stop=(kc == 31)),
                         reads=[xbT, wuv[k8]], writes=[pu])
                if grp == 1 and i > 0:
                    emit_scatter(i - 1)
            S.ps_release(pg)
            S.ps_release(pu)
            S.op("act", lambda e, pg=pg: e.activation(out=hs.t[:], in_=pg.t[:, :], func=AF.Silu), reads=[pg], writes=[hs])
            S.op("dve", lambda e, pu=pu, b=b: e.scalar_tensor_tensor(out=hb.t[:], in0=pu.t[:, :], scalar=RW[b].t[:, 0:1], in1=hs.t[:], op0=ALU.mult, op1=ALU.mult),
                 reads=[pu, RW[b], hs], writes=[hb])
            pt = S.ps()
            ptb = pt.t[:, :].bitcast(BF16)
            for fc in range(4):
                S.op("pe", lambda e, ptb=ptb, fc=fc: e.transpose(ptb[:, fc * 128:(fc + 1) * 128], hb.t[:, fc * 128:(fc + 1) * 128], CONST_BF.t[:, :]),
                     reads=[hb, CONST_BF], writes=[pt])
            S.op("dve", lambda e, ptb=ptb: e.tensor_copy(out=hT.t[:], in_=ptb[:, 0:512].rearrange("p (k t) -> p k t", k=4)), reads=[pt], writes=[hT])
            for cg in range(8):
                py = S.ps()
                for fc in range(4):
                    S.op("pe", lambda e, py=py, fc=fc, cg=cg, wd=wd: e.matmul(py.t[:, :], lhsT=hT.t[:, fc, :], rhs=wd[fc].t[:, cg * 512:(cg + 1) * 512],
                                                                      start=(fc == 0), stop=(fc == 3)), reads=[hT, WDv[i % 2][fc][cg // 4]], writes=[py])
                if cg % 2 == 0:
                    S.op("act", lambda e, py=py, cg=cg: e.activation(out=yb.t[:, cg * 512:(cg + 1) * 512], in_=py.t[:, :], func=AF.Copy), reads=[py], writes=[yb])
                else:
                    S.op("dve", lambda e, py=py, cg=cg: e.tensor_copy(out=yb.t[:, cg * 512:(cg + 1) * 512], in_=py.t[:, :]), reads=[py], writes=[yb])
        emit_scatter(NB - 1)
        S.flush()
    io["_moe_stack"].close()


def phase_final(nc, S, io, out):
    X1 = io["X1"]; YACC = io["YACC"]; MOD = io["MODROW"]
    with contextlib.ExitStack() as ph:
        def tl(shape, dt=F32):
            return S.tile(ph, shape, dt)
        G2 = tl([128, D]); NF = tl([128, D])
        S.dma("sp", lambda e: e.dma_start(out=G2.t[:], in_=MOD[0, 5 * D:6 * D].partition_broadcast(128)), writes=[G2])
        S.dma("sp", lambda e: e.dma_start(out=NF.t[:], in_=io["norm_f_g"][0].partition_broadcast(128)), writes=[NF])
        xts = [tl([128, D]) for _ in range(2)]
        yas = [tl([128, D]) for _ in range(2)]
        scr = tl([128, D], BF16)
        ssq = tl([128, 1]); rstd = tl([128, 1])
        outdep = tl([1, 1])
        for tt in range(T // 128):
            xt = xts[tt % 2]
            ya = yas[tt % 2]
            r0 = tt * 128
            S.dma("sp", lambda e, xt=xt, r0=r0: e.dma_start(out=xt.t[:], in_=X1[r0:r0 + 128, :]), writes=[xt])
            S.dma("sp", lambda e, ya=ya, r0=r0: e.dma_start(out=ya.t[:], in_=YACC[r0:r0 + 128, :]), writes=[ya])
            S.op("dve", lambda e, ya=ya: e.tensor_tensor(out=ya.t[:], in0=ya.t[:], in1=G2.t[:], op=ALU.mult), reads=[ya, G2], writes=[ya])
            S.op("pool", lambda e, ya=ya, xt=xt: e.tensor_tensor(out=xt.t[:], in0=xt.t[:], in1=ya.t[:], op=ALU.add), reads=[xt, ya], writes=[xt])
            io["_norm_tile"](xt, scr, ssq, rstd)
            S.op("dve", lambda e, xt=xt: e.tensor_tensor(out=xt.t[:], in0=xt.t[:], in1=NF.t[:], op=ALU.mult), reads=[xt, NF], writes=[xt])
            S.dma("sp", lambda e, xt=xt, r0=r0: e.dma_start(out=out[r0:r0 + 128, :], in_=xt.t[:]), reads=[xt], writes=[outdep])
        S.flush()


def _in_maps(inputs, cores, with_experts=True):
    f = lambda a: np.ascontiguousarray(a, dtype=np.float32)
    shared = {
        "w_cond": f(inputs["w_cond"][0]), "b_cond": f(inputs["b_cond"][0]).reshape(1, -1),
        "norm1_g": f(inputs["norm1_g"][0]).reshape(1, -1), "w_in": f(inputs["w_in"][0]),
        "rwkv_mu": f(inputs["rwkv_mu"][0]).reshape(1, -1), "rwkv_w0": f(inputs["rwkv_w0"][0]).reshape(1, -1),
        "rwkv_w_up": f(inputs["rwkv_w_up"][0]), "rwkv_a0": f(inputs["rwkv_a0"][0]).reshape(1, -1),
        "rwkv_a_up": f(inputs["rwkv_a_up"][0]), "rwkv_g_up": f(inputs["rwkv_g_up"][0]),
        "rwkv_k_k": f(inputs["rwkv_k_k"][0]).reshape(1, -1), "rwkv_k_a": f(inputs["rwkv_k_a"][0]).reshape(1, -1),
        "rwkv_r_k": f(inputs["rwkv_r_k"][0]).reshape(1, -1), "rwkv_lnx_w": f(inputs["rwkv_lnx_w"][0]).reshape(1, -1),
        "rwkv_lnx_b": f(inputs["rwkv_lnx_b"][0]).reshape(1, -1), "attn_sinks": f(inputs["attn_sinks"][0]).reshape(1, -1),
        "attn_out_g": f(inputs["attn_out_g"][0]).reshape(1, -1), "w_out": f(inputs["w_out"][0]),
        "norm2_g": f(inputs["norm2_g"][0]).reshape(1, -1),
        "router": f(np.concatenate([inputs["router_group"][0], inputs["router_expert"][0]], axis=1)),
        "router_bias": f(np.concatenate([inputs["router_group_bias"][0], inputs["router_expert_bias"][0]])).reshape(1, -1),
        "ew_gate": f(inputs["expert_w_gate"][0]).reshape(64 * D, 512),
        "ew_up": f(inputs["expert_w_up"][0]).reshape(64 * D, 512),
        "ew_down": f(inputs["expert_w_down"][0]).reshape(64 * 512, D),
        "norm_f_g": f(inputs["norm_f_g"]).reshape(1, -1),
        "consts": make_consts(), "attn_bias": make_attn_bias(),
    }
    if not with_experts:
        for k in ("ew_gate", "ew_up", "ew_down"):
            del shared[k]
    maps = []
    for b in cores:
        m = dict(shared)
        m["x"] = f(inputs["x"][b])
        m["c"] = f(inputs["c"][b]).reshape(1, -1)
        maps.append(m)
    return maps


def kernel(**inputs):
    nc = build()
    res = run_bass_kernel_spmd(nc, _in_maps(inputs, range(4)), core_ids=list(range(4)))
    return np.stack([np.asarray(r["out"], dtype=np.float32) for r in res.results], axis=0)
```

```python
import contextlib
import numpy as np
import ml_dtypes
import concourse.bass as bass
import concourse.mybir as mybir
from concourse.bass_utils import run_bass_kernel_spmd

F32 = mybir.dt.float32
BF16 = mybir.dt.bfloat16
I32 = mybir.dt.int32
AF = mybir.ActivationFunctionType
ALU = mybir.AluOpType
AX = mybir.AxisListType

ENGS = ("pe", "act", "dve", "pool", "sp")
HANDLES = {"pe": "tensor", "act": "scalar", "dve": "vector", "pool": "gpsimd", "sp": "sync"}

D = 4096
T = 4096
NRW = 6592
NIN = 9664
DR = 2048
EPS = 1e-6
GN_EPS = 64e-5
NB = 128
NEG = -30000.0


class Buf:
    __slots__ = ("lw", "rd")

    def __init__(self):
        self.lw = None
        self.rd = []


class Tl:
    __slots__ = ("t", "b")

    def __init__(self, t):
        self.t = t
        self.b = Buf()


class Sched:
    NDMA = {"sp": 16, "pool": 48}

    def __init__(self, nc, stack):
        self.nc = nc
        self.ops = {e: [] for e in ENGS}
        self.cnt = {e: 0 for e in ENGS}
        self.esem = {e: stack.enter_context(nc.semaphore("es_" + e)) for e in ENGS if e != "sp"}
        self.dsem = {q: [stack.enter_context(nc.semaphore("ds_%s%d" % (q, i))) for i in range(self.NDMA[q])]
                     for q in ("sp", "pool")}
        self.dcnt = {q: [0] * self.NDMA[q] for q in self.dsem}
        self.dnext = {q: 0 for q in self.dsem}
        self.seen = {e: {} for e in ENGS}
        self.psb = [Tl(stack.enter_context(nc.psum_tensor("psb%d" % i, [128, 512], F32))) for i in range(8)]
        self.psn = 0
        self.held = set()
        self.ntile = 0

    def tile(self, st, shape, dtype=F32):
        self.ntile += 1
        return Tl(st.enter_context(self.nc.sbuf_tensor("t%d" % self.ntile, list(shape), dtype)))

    def ps(self, hold=False):
        while True:
            p = self.psb[self.psn % 8]
            self.psn += 1
            if id(p) not in self.held:
                break
        if hold:
            self.held.add(id(p))
        return p

    def ps_release(self, p):
        self.held.discard(id(p))

    def _need(self, eng, waits, ev):
        if ev is None:
            return
        sem, val, src = ev
        if eng == "pe" and src == "pe":
            return
        key = id(sem)
        if self.seen[eng].get(key, 0) >= val:
            return
        cur = waits.get(key)
        if cur is None or cur[1] < val:
            waits[key] = (sem, val)

    def _deps(self, eng, reads, writes):
        waits = {}
        for b in reads:
            self._need(eng, waits, b.b.lw)
        for b in writes:
            self._need(eng, waits, b.b.lw)
            for r in b.b.rd:
                self._need(eng, waits, r)
        for key, (sem, val) in waits.items():
            self.seen[eng][key] = val
        return list(waits.values())

    def _commit(self, ev, reads, writes):
        for b in reads:
            b.b.rd.append(ev)
        for b in writes:
            b.b.lw = ev
            b.b.rd = []

    def _emit(self, eng, fn, waits, inc):
        eh = getattr(self.nc, HANDLES[eng])
        for sem, val in waits:
            eh.wait_ge(sem, val)
        if fn is not None:
            fn(eh).then_inc(inc[0], inc[1])

    def op(self, eng, fn, reads=(), writes=()):
        waits = self._deps(eng, reads, writes)
        self.cnt[eng] += 1
        ev = (self.esem[eng], self.cnt[eng], eng)
        self._emit(eng, fn, waits, (self.esem[eng], 1))
        self._commit(ev, reads, writes)

    def dma(self, q, fn, reads=(), writes=()):
        waits = self._deps(q, reads, writes)
        k = self.dnext[q]
        self.dnext[q] = (k + 1) % self.NDMA[q]
        sem = self.dsem[q][k]
        prev = self.dcnt[q][k]
        if prev > 0 and self.seen[q].get(id(sem), 0) < prev:
            self.seen[q][id(sem)] = prev
            waits.append((sem, prev))
        self.dcnt[q][k] = prev + 16
        ev = (sem, prev + 16, "dma")
        self._emit(q, fn, waits, (sem, 16))
        self._commit(ev, reads, writes)

    def barrier(self):
        evs = [(self.esem[e], self.cnt[e], "bar") for e in self.esem if self.cnt[e] > 0]
        for q in self.dsem:
            for k in range(self.NDMA[q]):
                if self.dcnt[q][k] > 0:
                    evs.append((self.dsem[q][k], self.dcnt[q][k], "bar"))
        for e in ENGS:
            waits = {}
            for ev in evs:
                self._need(e, waits, ev)
            for key, (sem, val) in waits.items():
                self.seen[e][key] = val
            if waits:
                self._emit(e, None, list(waits.values()), None)

    def flush(self):
        self.barrier()


C_ID = 0
C_ONES = 128
C_TRI = 256
C_ML = 384
C_MU = 448
C_MUI = 512
C_SU = 576
C_SUI = 640
C_K128 = 704
C_PID = 768
C_P128 = 769
C_IOTA64 = 770
C_RM = 834
C_END = C_RM + 2048


def make_consts():
    c = np.zeros((128, C_END), np.float32)
    c[:, C_ID:C_ID + 128] = np.eye(128)
    c[:, C_ONES:C_ONES + 128] = 1.0
    i = np.arange(128)
    c[:, C_TRI:C_TRI + 128] = (i[:, None] < i[None, :])
    j = np.arange(64)
    c[:64, C_ML:C_ML + 64] = (j[None, :] < j[:, None])
    c[:64, C_MU:C_MU + 64] = (j[:, None] < j[None, :])
    c[:64, C_MUI:C_MUI + 64] = (j[:, None] <= j[None, :])
    c[:64, C_SU:C_SU + 64] = (j[:, None] < j[None, :])
    c[:64, C_SUI:C_SUI + 64] = (j[:, None] <= j[None, :])
    c[:, C_K128:C_K128 + 64] = 128.0 * j[None, :]
    c[:, C_PID] = i
    c[:, C_P128] = 128.0 * i
    c[:, C_IOTA64:C_IOTA64 + 64] = j[None, :]
    rm = np.ones(2048, np.float32)
    rm[::64] = 0.0
    c[:, C_RM:C_RM + 2048] = rm[None, :]
    return c


def make_attn_bias():
    qi = np.arange(128)[:, None]
    kj = np.arange(256)[None, :]
    dist = (qi + 128 - kj).astype(np.float32)
    inw = (dist >= 0) & (dist < 128)
    slopes = np.exp2(-8.0 * np.arange(1, 33, dtype=np.float32) / 32).astype(np.float32).reshape(8, 4)
    ab = np.zeros((128, 2, 8, 4, 256), np.float32)
    for h in range(8):
        for g in range(4):
            a = np.where(inw, -slopes[h, g] * dist, NEG).astype(np.float32)
            ab[:, 0, h, g, :] = a
            a0 = a.copy()
            a0[:, :128] = NEG
            ab[:, 1, h, g, :] = a0
    return ab.reshape(128, 2 * 8 * 4 * 256)


def build(last_phase=99, debug=False):
    nc = bass.Bass("TRN2", target_bir_lowering=False)
    io = {}

    def din(name, shape, dt=F32):
        io[name] = nc.dram_tensor(name, list(shape), dt, kind="ExternalInput").ap()

    din("x", [T, D]); din("c", [1, D]); din("w_cond", [D, 6 * D]); din("b_cond", [1, 6 * D])
    din("norm1_g", [1, D]); din("w_in", [D, NIN]); din("rwkv_mu", [1, NRW]); din("rwkv_w0", [1, DR])
    din("rwkv_w_up", [96, DR]); din("rwkv_a0", [1, DR]); din("rwkv_a_up", [96, DR]); din("rwkv_g_up", [256, DR])
    din("rwkv_k_k", [1, DR]); din("rwkv_k_a", [1, DR]); din("rwkv_r_k", [1, DR]); din("rwkv_lnx_w", [1, DR])
    din("rwkv_lnx_b", [1, DR]); din("attn_sinks", [1, 32]); din("attn_out_g", [1, DR]); din("w_out", [D, D])
    din("norm2_g", [1, D]); din("router", [D, 72]); din("router_bias", [1, 72])
    if last_phase >= 5:
        din("ew_gate", [64 * D, 512]); din("ew_up", [64 * D, 512]); din("ew_down", [64 * 512, D])
    din("norm_f_g", [1, D]); din("consts", [128, C_END]); din("attn_bias", [128, 2 * 8 * 1024])
    out = nc.dram_tensor("out", [T, D], F32, kind="ExternalOutput").ap()

    def scratch(name, shape, dt=F32):
        kind = "ExternalOutput" if debug else "Internal"
        io[name] = nc.dram_tensor(name, list(shape), dt, kind=kind).ap()

    scratch("MODROW", [1, 6 * D]); scratch("PT", [NIN, T]); scratch("YT", [D, T]); scratch("X1", [T, D])
    scratch("U2", [T + 1, D], BF16); scratch("ROWTOK", [NB * 128, 16]); scratch("ROWW", [NB * 128, 16])
    scratch("YACC", [T + 1, D])

    with contextlib.ExitStack() as gs:
        S = Sched(nc, gs)
        CONST = S.tile(gs, [128, C_END])
        S.dma("sp", lambda e: e.dma_start(out=CONST.t[:], in_=io["consts"]), writes=[CONST])
        ident = CONST.t[:, C_ID:C_ID + 128]
        A1 = S.tile(gs, [128, 32]); SH1 = S.tile(gs, [128, 32]); A2 = S.tile(gs, [128, 32]); SH2 = S.tile(gs, [128, 32])

        def col_from_row(ph, src_ap, dst_fn):
            r = S.tile(ph, [32, 128])
            S.dma("sp", lambda e: e.dma_start(out=r.t[:], in_=src_ap.rearrange("o (k p) -> (o k) p", p=128)), writes=[r])
            ps = S.ps()
            S.op("pe", lambda e: e.transpose(ps.t[:, 0:32], r.t[:], ident[0:32, 0:32]), reads=[r, CONST], writes=[ps])
            dst_fn(ps)

        with contextlib.ExitStack() as ph:
            scT = S.tile(ph, [128, 32])
            col_from_row(ph, io["c"], lambda ps: S.op(
                "act", lambda e: e.activation(out=scT.t[:], in_=ps.t[:, 0:32], func=AF.Silu), reads=[ps], writes=[scT]))
            bc = S.tile(ph, [1, 6 * D])
            S.dma("sp", lambda e: e.dma_start(out=bc.t[:], in_=io["b_cond"]), writes=[bc])
            wts = [S.tile(ph, [128, 2048]) for _ in range(3)]
            mrs = [S.tile(ph, [1, 2048]) for _ in range(2)]
            n = 0
            for ng in range(12):
                pbs = [S.ps() for _ in range(4)]
                for kc in range(32):
                    wt = wts[n % 3]
                    n += 1
                    S.dma("sp", lambda e, wt=wt, kc=kc, ng=ng: e.dma_start(
                        out=wt.t[:], in_=io["w_cond"][kc * 128:(kc + 1) * 128, ng * 2048:(ng + 1) * 2048]), writes=[wt])
                    for j in range(4):
                        S.op("pe", lambda e, wt=wt, kc=kc, j=j, pb=pbs[j]: e.matmul(
                            pb.t[0:1, :], lhsT=scT.t[:, kc:kc + 1], rhs=wt.t[:, j * 512:(j + 1) * 512],
                            start=(kc == 0), stop=(kc == 31)), reads=[wt, scT], writes=[pbs[j]])
                mr = mrs[ng % 2]
                for j in range(4):
                    S.op("dve", lambda e, mr=mr, j=j, pb=pbs[j], ng=ng: e.tensor_tensor(
                        out=mr.t[0:1, j * 512:(j + 1) * 512], in0=pb.t[0:1, :],
                        in1=bc.t[0:1, ng * 2048 + j * 512: ng * 2048 + (j + 1) * 512], op=ALU.add),
                        reads=[pbs[j], bc], writes=[mr])
                S.dma("sp", lambda e, mr=mr, ng=ng: e.dma_start(
                    out=io["MODROW"][0:1, ng * 2048:(ng + 1) * 2048], in_=mr.t[:]), reads=[mr], writes=[])
            S.flush()
            for (Ax, SHx, gname, ish, isc) in ((A1, SH1, "norm1_g", 0, 1), (A2, SH2, "norm2_g", 3, 4)):
                gcol = S.tile(ph, [128, 32])
                col_from_row(ph, io[gname], lambda ps, gcol=gcol: S.op(
                    "dve", lambda e: e.tensor_copy(out=gcol.t[:], in_=ps.t[:, 0:32]), reads=[ps], writes=[gcol]))
                col_from_row(ph, io["MODROW"][0:1, isc * D:(isc + 1) * D], lambda ps, gcol=gcol, Ax=Ax: S.op(
                    "dve", lambda e: e.scalar_tensor_tensor(out=Ax.t[:], in0=ps.t[:, 0:32], scalar=1.0, in1=gcol.t[:],
                                                            op0=ALU.add, op1=ALU.mult), reads=[ps, gcol], writes=[Ax]))
                col_from_row(ph, io["MODROW"][0:1, ish * D:(ish + 1) * D], lambda ps, SHx=SHx: S.op(
                    "dve", lambda e: e.tensor_copy(out=SHx.t[:], in_=ps.t[:, 0:32]), reads=[ps], writes=[SHx]))
            S.flush()
        if last_phase <= 0:
            return nc

        def norm_tile(xt, scr, ssq, rstd):
            S.op("act", lambda e: e.activation(out=scr.t[:], in_=xt.t[:], func=AF.Square, accum_out=ssq.t[:, 0:1]),
                 reads=[xt], writes=[scr, ssq])
            S.op("dve", lambda e: e.tensor_scalar(out=rstd.t[:, 0:1], in0=ssq.t[:, 0:1], scalar1=1.0 / D, scalar2=EPS,
                                                  op0=ALU.mult, op1=ALU.add), reads=[ssq], writes=[rstd])
            S.op("act", lambda e: e.activation(out=rstd.t[:, 0:1], in_=rstd.t[:, 0:1], func=AF.Sqrt), reads=[rstd], writes=[rstd])
            S.op("dve", lambda e: e.reciprocal(out=rstd.t[:, 0:1], in_=rstd.t[:, 0:1]), reads=[rstd], writes=[rstd])
            S.op("act", lambda e: e.activation(out=xt.t[:], in_=xt.t[:], func=AF.Copy, scale=rstd.t[:, 0:1]),
                 reads=[xt, rstd], writes=[xt])

        TQ = 1024
        with contextlib.ExitStack() as ph:
            uT = S.tile(ph, [128, 32, TQ], BF16)
            xts = [S.tile(ph, [128, D]) for _ in range(2)]
            scr = S.tile(ph, [128, D], BF16)
            ssq = S.tile(ph, [128, 1]); rstd = S.tile(ph, [128, 1])
            wbs = [S.tile(ph, [128, 32, 256], BF16) for _ in range(2)]
            ots = [S.tile(ph, [128, TQ]) for _ in range(2)]
            nw = 0
            no = 0
            for tq in range(T // TQ):
                for tt in range(TQ // 128):
                    xt = xts[tt % 2]
                    r0 = tq * TQ + tt * 128
                    S.dma("sp", lambda e, xt=xt, r0=r0: e.dma_start(out=xt.t[:], in_=io["x"][r0:r0 + 128, :]), writes=[xt])
                    norm_tile(xt, scr, ssq, rstd)
                    for kg in range(8):
                        ps = S.ps()
                        for k4 in range(4):
                            kc = kg * 4 + k4
                            S.op("pe", lambda e, ps=ps, xt=xt, kc=kc, k4=k4: e.transpose(
                                ps.t[:, k4 * 128:(k4 + 1) * 128], xt.t[:, kc * 128:(kc + 1) * 128], ident),
                                reads=[xt, CONST], writes=[ps])
                        for k4 in range(4):
                            kc = kg * 4 + k4
                            if k4 % 2 == 0:
                                S.op("dve", lambda e, ps=ps, kc=kc, k4=k4, tt=tt: e.tensor_scalar(
                                    out=uT.t[:, kc, tt * 128:(tt + 1) * 128], in0=ps.t[:, k4 * 128:(k4 + 1) * 128],
                                    scalar1=A1.t[:, kc:kc + 1], scalar2=SH1.t[:, kc:kc + 1], op0=ALU.mult, op1=ALU.add),
                                    reads=[ps, A1, SH1], writes=[uT])
                            else:
                                S.op("act", lambda e, ps=ps, kc=kc, k4=k4, tt=tt: e.activation(
                                    out=uT.t[:, kc, tt * 128:(tt + 1) * 128], in_=ps.t[:, k4 * 128:(k4 + 1) * 128],
                                    func=AF.Identity, scale=A1.t[:, kc:kc + 1], bias=SH1.t[:, kc:kc + 1]),
                                    reads=[ps, A1, SH1], writes=[uT])
                for cg in range(38):
                    c0 = cg * 256
                    ncol = min(256, NIN - c0)
                    wb = wbs[nw % 2]
                    nw += 1
                    S.dma("pool", lambda e, wb=wb, c0=c0, ncol=ncol: e.dma_start(
                        out=wb.t[:, :, 0:ncol], in_=io["w_in"][:, c0:c0 + ncol].rearrange("(k p) j -> p k j", p=128)),
                        writes=[wb])
                    for sc in range((ncol + 127) // 128):
                        m = min(128, ncol - sc * 128)
                        ot = ots[no % 2]
                        no += 1
                        for tg in range(TQ // 512):
                            ps = S.ps()
                            for kc in range(32):
                                S.op("pe", lambda e, ps=ps, wb=wb, kc=kc, sc=sc, m=m, tg=tg: e.matmul(
                                    ps.t[0:m, :], lhsT=wb.t[:, kc, sc * 128:sc * 128 + m], rhs=uT.t[:, kc, tg * 512:(tg + 1) * 512],
                                    start=(kc == 0), stop=(kc == 31)), reads=[wb, uT], writes=[ps])
                            if tg % 2 == 0:
                                S.op("dve", lambda e, ps=ps, ot=ot, m=m, tg=tg: e.tensor_copy(
                                    out=ot.t[0:m, tg * 512:(tg + 1) * 512], in_=ps.t[0:m, :]), reads=[ps], writes=[ot])
                            else:
                                S.op("act", lambda e, ps=ps, ot=ot, m=m, tg=tg: e.activation(
                                    out=ot.t[0:m, tg * 512:(tg + 1) * 512], in_=ps.t[0:m, :], func=AF.Copy), reads=[ps], writes=[ot])
                        S.dma("sp", lambda e, ot=ot, m=m, c0=c0, sc=sc, tq=tq: e.dma_start(
                            out=io["PT"][c0 + sc * 128:c0 + sc * 128 + m, tq * TQ:(tq + 1) * TQ], in_=ot.t[0:m, :]),
                            reads=[ot], writes=[])
            S.flush()
        if last_phase <= 1:
            return nc
        return build_rest(nc, S, gs, io, out, CONST, ident, A1, SH1, A2, SH2, norm_tile, last_phase)


def build_rest(nc, S, gs, io, out, CONST, ident, A1, SH1, A2, SH2, norm_tile, last_phase):
    io["_norm_tile"] = norm_tile
    phase_rwkv(nc, S, io, CONST, ident)
    if last_phase <= 2:
        return nc
    phase_attn(nc, S, io, CONST, ident)
    if last_phase <= 3:
        return nc
    phase_wout(nc, S, io, CONST, ident, A2, SH2, norm_tile, gs)
    if last_phase <= 4:
        return nc
    phase_moe(nc, S, io, CONST, ident)
    if last_phase <= 5:
        return nc
    phase_final(nc, S, io, out)
    return nc


def cview(CONST, c0, n, parts=64):
    return CONST.t[0:parts, c0:c0 + n]


def bc3(ap2, nh, last=True):
    p, n = ap2.shape
    if last:
        return ap2.unsqueeze(1).broadcast_to([p, nh, n])
    return ap2.unsqueeze(2).broadcast_to([p, n, nh])


def phase_rwkv(nc, S, io, CONST, ident):
    NH, NT, C = 8, 128, 64
    NCH = NT // C
    id64 = CONST.t[0:64, C_ID:C_ID + 64]
    ones64 = CONST.t[0:64, C_ONES:C_ONES + 64]
    ML = bc3(cview(CONST, C_ML, 64), NH)
    MU = bc3(cview(CONST, C_MU, 64), NH)
    MUI = bc3(cview(CONST, C_MUI, 64), NH)
    ID4 = bc3(id64, NH)
    RM = CONST.t[0:64, C_RM:C_RM + NH * NT]
    PT = io["PT"]
    mu = io["rwkv_mu"]
    with contextlib.ExitStack() as ph:
        def tl(shape, dt=F32):
            return S.tile(ph, shape, dt)

        def col_from(src_ap, rows, cols, dst):
            r = tl([rows, cols])
            S.dma("sp", lambda e: e.dma_start(out=r.t[:], in_=src_ap), writes=[r])
            ps = S.ps()
            S.op("pe", lambda e: e.transpose(ps.t[0:cols, 0:rows], r.t[:], ident[0:rows, 0:rows]), reads=[r, CONST], writes=[ps])
            S.op("dve", lambda e: e.tensor_copy(out=dst.t[:], in_=ps.t[0:cols, 0:rows]), reads=[ps], writes=[dst])

        def headcol(src_row):
            d = tl([64, 32])
            col_from(src_row.rearrange("o (h j) -> (o h) j", j=64), 32, 64, d)
            return d

        MUR = headcol(mu[0:1, 0:2048]); MUK = headcol(mu[0:1, 2048:4096]); MUV = headcol(mu[0:1, 4096:6144])
        W0 = headcol(io["rwkv_w0"]); A0 = headcol(io["rwkv_a0"]); KK_ = headcol(io["rwkv_k_k"]); KA = headcol(io["rwkv_k_a"])
        RK_ = headcol(io["rwkv_r_k"]); LNW = headcol(io["rwkv_lnx_w"]); LNB = headcol(io["rwkv_lnx_b"])
        NW0 = tl([64, 32])
        S.op("dve", lambda e: e.tensor_scalar(out=NW0.t[:], in0=W0.t[:], scalar1=-1.0, scalar2=None, op0=ALU.mult), reads=[W0], writes=[NW0])
        MUW = tl([96, 1]); col_from(mu[0:1, 6144:6240], 1, 96, MUW)
        MUA = tl([96, 1]); col_from(mu[0:1, 6240:6336], 1, 96, MUA)
        MUG = tl([128, 2]); col_from(mu[0:1, 6336:6592].rearrange("o (k p) -> (o k) p", p=128), 2, 128, MUG)
        WUP = tl([96, DR]); AUP = tl([96, DR]); GUP = tl([128, 2, DR])
        S.dma("sp", lambda e: e.dma_start(out=WUP.t[:], in_=io["rwkv_w_up"]), writes=[WUP])
        S.dma("sp", lambda e: e.dma_start(out=AUP.t[:], in_=io["rwkv_a_up"]), writes=[AUP])
        S.dma("sp", lambda e: e.dma_start(out=GUP.t[:], in_=io["rwkv_g_up"].rearrange("(k p) c -> p k c", p=128)), writes=[GUP])
        ST = tl([64, 32, 64])
        S.op("pool", lambda e: e.memset(ST.t[:], 0.0), writes=[ST])

        TW = tl([96, NT]); XA = tl([96, NT]); SG = tl([128, 2, NT])
        lc = tl([128, 2, NT]); lp = tl([128, 2, NT])
        big = lambda: tl([64, NH, NT])
        R = big(); K = big(); V = big(); PV = big(); E = big(); A = big(); G_ = big(); KKt = big(); NR = big()
        K2 = big(); B = big(); L = big(); GAM = big(); GI = big(); BON = big(); YN = big()
        GP = PV; KKD = KKt; BI = B; KI = K2; RD = R
        C1 = big(); C2 = big(); C3 = big(); P2 = big(); P3 = big()
        VT = tl([64, NCH, NH, 64]); BIT = tl([64, NCH, NH, 64]); KIT = tl([64, NCH, NH, 64])
        sm = lambda: tl([64, NH, 64])
        M = [[sm(), sm()] for _ in range(NCH)]; MT = [[sm(), sm()] for _ in range(NCH)]
        TT = [sm() for _ in range(NCH)]; BTm = [sm() for _ in range(NCH)]; PTm = [sm() for _ in range(NCH)]; QTm = [sm() for _ in range(NCH)]
        YS = [sm() for _ in range(NCH)]; XS = sm(); US = sm(); SQ = sm(); TMP = sm()
        s1 = tl([64, NH]); s2 = tl([64, NH]); mean = tl([64, NH]); rstd = tl([64, NH])

        def load_mix(dst, cur, prev, rows, t0, parts, nh, mucol):
            S.dma("sp", lambda e: e.dma_start(out=cur.t[0:parts, 0:nh, :], in_=rows(t0, t0 + NT)), writes=[cur])
            if t0 == 0:
                S.op("pool", lambda e: e.memset(prev.t[0:parts, 0:nh, 0:1], 0.0), writes=[prev])
                S.dma("sp", lambda e: e.dma_start(out=prev.t[0:parts, 0:nh, 1:NT], in_=rows(0, NT - 1)), writes=[prev])
            else:
                S.dma("sp", lambda e: e.dma_start(out=prev.t[0:parts, 0:nh, :], in_=rows(t0 - 1, t0 + NT - 1)), writes=[prev])
            S.op("pool", lambda e: e.tensor_tensor(out=prev.t[0:parts, 0:nh, :], in0=prev.t[0:parts, 0:nh, :],
                                                   in1=cur.t[0:parts, 0:nh, :], op=ALU.subtract), reads=[cur, prev], writes=[prev])
            for h in range(nh):
                S.op("dve", lambda e, h=h: e.scalar_tensor_tensor(
                    out=dst.t[0:parts, h, :], in0=prev.t[0:parts, h, :], scalar=mucol(h), in1=cur.t[0:parts, h, :],
                    op0=ALU.mult, op1=ALU.add), reads=[prev, cur], writes=[dst])

        for tq in range(T // NT):
            t0 = tq * NT
            load_mix(lp, lc, lp, lambda a, b: PT[6144:6240, a:b].unsqueeze(1), t0, 96, 1, lambda h: MUW.t[:, 0:1])
            S.op("act", lambda e: e.activation(out=TW.t[:], in_=lp.t[0:96, 0, :], func=AF.Tanh), reads=[lp], writes=[TW])
            load_mix(lp, lc, lp, lambda a, b: PT[6240:6336, a:b].unsqueeze(1), t0, 96, 1, lambda h: MUA.t[:, 0:1])
            S.op("act", lambda e: e.activation(out=XA.t[:], in_=lp.t[0:96, 0, :], func=AF.Copy), reads=[lp], writes=[XA])
            load_mix(lp, lc, lp, lambda a, b: PT[6336:6592, a:b].rearrange("(k p) t -> p k t", p=128), t0, 128, 2,
                     lambda h: MUG.t[:, h:h + 1])
            S.op("act", lambda e: e.activation(out=SG.t[:], in_=lp.t[:], func=AF.Sigmoid), reads=[lp], writes=[SG])
            for hg in range(32 // NH):
                H0 = hg * NH
                c0 = H0 * 64
                hv = lambda base: (lambda a, b: PT[base + c0:base + c0 + NH * 64, a:b].rearrange("(h j) t -> j h t", j=64))
                load_mix(R, C1, PV, hv(0), t0, 64, NH, lambda h: MUR.t[:, H0 + h:H0 + h + 1])
                load_mix(K, C2, P2, hv(2048), t0, 64, NH, lambda h: MUK.t[:, H0 + h:H0 + h + 1])
                load_mix(V, C3, P3, hv(4096), t0, 64, NH, lambda h: MUV.t[:, H0 + h:H0 + h + 1])
                for h in range(NH):
                    H = H0 + h
                    cs = slice(H * 64, H * 64 + 64)
                    p1 = S.ps()
                    S.op("pe", lambda e, p1=p1, cs=cs: e.matmul(p1.t[0:64, 0:NT], lhsT=WUP.t[:, cs], rhs=TW.t[:], start=True, stop=True),
                         reads=[WUP, TW], writes=[p1])
                    S.op("act", lambda e, p1=p1, h=h, H=H: e.activation(out=E.t[:, h, :], in_=p1.t[0:64, 0:NT], func=AF.Exp,
                                                                       scale=-1.0, bias=NW0.t[:, H:H + 1]), reads=[p1, NW0], writes=[E])
                    S.op("pool", lambda e, h=h, H=H: e.tensor_scalar(out=KKt.t[:, h, :], in0=K.t[:, h, :], scalar1=KK_.t[:, H:H + 1],
                                                                    scalar2=None, op0=ALU.mult), reads=[K, KK_], writes=[KKt])
                for h in range(NH):
                    H = H0 + h
                    cs = slice(H * 64, H * 64 + 64)
                    p3 = S.ps()
                    for kc in range(2):
                        S.op("pe", lambda e, p3=p3, cs=cs, kc=kc: e.matmul(p3.t[0:64, 0:NT], lhsT=GUP.t[:, kc, cs], rhs=SG.t[:, kc, :],
                                                                         start=(kc == 0), stop=(kc == 1)), reads=[GUP, SG], writes=[p3])
                    S.op("dve", lambda e, p3=p3, h=h: e.tensor_copy(out=G_.t[:, h, :], in_=p3.t[0:64, 0:NT]), reads=[p3], writes=[G_])
                S.op("act", lambda e: e.activation(out=E.t[:], in_=E.t[:], func=AF.Ln, bias=1.0), reads=[E], writes=[E])
                S.op("act", lambda e: e.activation(out=E.t[:], in_=E.t[:], func=AF.Exp, scale=-1.0, bias=-0.5), reads=[E], writes=[E])
                for h in range(NH):
                    H = H0 + h
                    cs = slice(H * 64, H * 64 + 64)
                    p2 = S.ps()
                    S.op("pe", lambda e, p2=p2, cs=cs: e.matmul(p2.t[0:64, 0:NT], lhsT=AUP.t[:, cs], rhs=XA.t[:], start=True, stop=True),
                         reads=[AUP, XA], writes=[p2])
                    S.op("act", lambda e, p2=p2, h=h, H=H: e.activation(out=A.t[:, h, :], in_=p2.t[0:64, 0:NT], func=AF.Sigmoid,
                                                                       bias=A0.t[:, H:H + 1]), reads=[p2, A0], writes=[A])
                S.op("pool", lambda e: e.tensor_tensor(out=NR.t[:], in0=KKt.t[:], in1=KKt.t[:], op=ALU.mult), reads=[KKt], writes=[NR])
                for h in range(NH):
                    p1 = S.ps()
                    S.op("pe", lambda e, p1=p1, h=h: e.matmul(p1.t[0:64, 0:NT], lhsT=ones64, rhs=NR.t[:, h, :], start=True, stop=True),
                         reads=[NR, CONST], writes=[p1])
                    S.op("dve", lambda e, p1=p1, h=h: e.tensor_scalar(out=L.t[:, h, :], in0=p1.t[0:64, 0:NT], scalar1=1e-19, scalar2=None, op0=ALU.max),
                         reads=[p1], writes=[L])
                S.op("act", lambda e: e.activation(out=L.t[:], in_=L.t[:], func=AF.Ln), reads=[L], writes=[L])
                S.op("act", lambda e: e.activation(out=NR.t[:], in_=L.t[:], func=AF.Exp, scale=-0.5), reads=[L], writes=[NR])
                S.op("dve", lambda e: e.tensor_tensor(out=KKt.t[:], in0=KKt.t[:], in1=NR.t[:], op=ALU.mult), reads=[KKt, NR], writes=[KKt])
                for h in range(NH):
                    H = H0 + h
                    S.op("dve", lambda e, h=h, H=H: e.tensor_scalar(out=K2.t[:, h, :], in0=A.t[:, h, :], scalar1=-1.0, scalar2=KA.t[:, H:H + 1],
                                                                   op0=ALU.add, op1=ALU.mult), reads=[A, KA], writes=[K2])
                S.op("dve", lambda e: e.scalar_tensor_tensor(out=K2.t[:], in0=K2.t[:], scalar=1.0, in1=K.t[:], op0=ALU.add, op1=ALU.mult),
                     reads=[K2, K], writes=[K2])
                S.op("pool", lambda e: e.tensor_tensor(out=B.t[:], in0=KKt.t[:], in1=A.t[:], op=ALU.mult), reads=[KKt, A], writes=[B])
                S.op("pool", lambda e: e.tensor_tensor(out=BON.t[:], in0=R.t[:], in1=K2.t[:], op=ALU.mult), reads=[R, K2], writes=[BON])
                for h in range(NH):
                    H = H0 + h
                    S.op("dve", lambda e, h=h, H=H: e.tensor_scalar(out=BON.t[:, h, :], in0=BON.t[:, h, :], scalar1=RK_.t[:, H:H + 1],
                                                                   scalar2=None, op0=ALU.mult), reads=[BON, RK_], writes=[BON])
                pbs_ = []
                for h in range(NH):
                    p1 = S.ps()
                    pbs_.append(p1)
                    S.op("pe", lambda e, p1=p1, h=h: e.matmul(p1.t[0:64, 0:NT], lhsT=ones64, rhs=BON.t[:, h, :], start=True, stop=True),
                         reads=[BON, CONST], writes=[p1])
                for h in range(NH):
                    p1 = pbs_[h]
                    S.op("dve", lambda e, p1=p1, h=h: e.tensor_tensor(out=BON.t[:, h, :], in0=p1.t[0:64, 0:NT], in1=V.t[:, h, :], op=ALU.mult),
                         reads=[p1, V, BON], writes=[BON])
                S.op("dve", lambda e: e.tensor_tensor_scan(out=L.t[:].rearrange("p h t -> p (h t)"), data0=RM,
                                                           data1=E.t[:].rearrange("p h t -> p (h t)"), initial=0.0,
                                                           op0=ALU.mult, op1=ALU.subtract), reads=[E, CONST, L], writes=[L])
                S.op("act", lambda e: e.activation(out=GAM.t[:], in_=L.t[:], func=AF.Exp), reads=[L], writes=[GAM])
                S.op("act", lambda e: e.activation(out=GI.t[:], in_=L.t[:], func=AF.Exp, scale=-1.0), reads=[L], writes=[GI])
                S.op("pool", lambda e: e.tensor_tensor(out=GP.t[:], in0=L.t[:], in1=E.t[:], op=ALU.add), reads=[L, E], writes=[GP])
                S.op("act", lambda e: e.activation(out=GP.t[:], in_=GP.t[:], func=AF.Exp), reads=[GP], writes=[GP])
                S.op("dve", lambda e: e.tensor_tensor(out=KKD.t[:], in0=KKt.t[:], in1=GP.t[:], op=ALU.mult), reads=[KKt, GP], writes=[KKD])
                S.op("pool", lambda e: e.tensor_tensor(out=BI.t[:], in0=B.t[:], in1=GI.t[:], op=ALU.mult), reads=[B, GI], writes=[BI])
                S.op("dve", lambda e: e.tensor_tensor(out=KI.t[:], in0=K2.t[:], in1=GI.t[:], op=ALU.mult), reads=[K2, GI], writes=[KI])
                S.op("pool", lambda e: e.tensor_tensor(out=RD.t[:], in0=R.t[:], in1=GAM.t[:], op=ALU.mult), reads=[R, GAM], writes=[RD])
                for (src, dst) in ((V, VT), (BI, BIT), (KI, KIT)):
                    for c in range(NCH):
                        pt = S.ps()
                        for h in range(NH):
                            S.op("pe", lambda e, pt=pt, src=src, c=c, h=h: e.transpose(
                                pt.t[0:64, h * 64:(h + 1) * 64], src.t[:, h, c * C:(c + 1) * C], id64), reads=[src, CONST], writes=[pt])
                        S.op("act" if c % 2 else "dve", (lambda e, pt=pt, dst=dst, c=c: e.activation(
                            out=dst.t[:, c, :, :], in_=pt.t[0:64, 0:NH * 64].rearrange("p (h i) -> p h i", h=NH), func=AF.Copy)) if c % 2 else
                            (lambda e, pt=pt, dst=dst, c=c: e.tensor_copy(
                                out=dst.t[:, c, :, :], in_=pt.t[0:64, 0:NH * 64].rearrange("p (h i) -> p h i", h=NH))),
                            reads=[pt], writes=[dst])

                def mm4(lhs_fn, rhs_fn, reads):
                    p = S.ps()
                    for h in range(NH):
                        l_ = lhs_fn(h)
                        r_ = rhs_fn(h)
                        S.op("pe", lambda e, p=p, h=h, l_=l_, r_=r_: e.matmul(p.t[0:64, h * 64:(h + 1) * 64], lhsT=l_, rhs=r_, start=True, stop=True),
                             reads=reads, writes=[p])
                    return p

                def pv(p):
                    return p.t[0:64, 0:NH * 64].rearrange("p (h i) -> p h i", h=NH)

                tcs = [slice(c * C, (c + 1) * C) for c in range(NCH)]
                for c in range(NCH):
                    tc = tcs[c]
                    p = mm4(lambda h: KKD.t[:, h, tc], lambda h: BI.t[:, h, tc], [KKD, BI])
                    S.op("dve", lambda e, p=p, c=c: e.tensor_tensor(out=M[c][0].t[:], in0=pv(p), in1=ML, op=ALU.mult), reads=[p, CONST], writes=[M[c][0]])
                    p = mm4(lambda h: BI.t[:, h, tc], lambda h: KKD.t[:, h, tc], [KKD, BI])
                    S.op("dve", lambda e, p=p, c=c: e.tensor_tensor(out=MT[c][0].t[:], in0=pv(p), in1=MU, op=ALU.mult), reads=[p, CONST], writes=[MT[c][0]])
                    S.op("pool", lambda e, c=c: e.tensor_tensor(out=TT[c].t[:], in0=ID4, in1=MT[c][0].t[:], op=ALU.subtract), reads=[MT[c][0], CONST], writes=[TT[c]])
                    p = mm4(lambda h: KI.t[:, h, tc], lambda h: KKD.t[:, h, tc], [KKD, KI])
                    S.op("dve", lambda e, p=p, c=c: e.tensor_tensor(out=BTm[c].t[:], in0=pv(p), in1=MU, op=ALU.mult), reads=[p, CONST], writes=[BTm[c]])
                    p = mm4(lambda h: BI.t[:, h, tc], lambda h: RD.t[:, h, tc], [RD, BI])
                    S.op("dve", lambda e, p=p, c=c: e.tensor_tensor(out=PTm[c].t[:], in0=pv(p), in1=MUI, op=ALU.mult), reads=[p, CONST], writes=[PTm[c]])
                    p = mm4(lambda h: KI.t[:, h, tc], lambda h: RD.t[:, h, tc], [RD, KI])
                    S.op("dve", lambda e, p=p, c=c: e.tensor_tensor(out=QTm[c].t[:], in0=pv(p), in1=MUI, op=ALU.mult), reads=[p, CONST], writes=[QTm[c]])
                cur = 0
                for lvl in range(5):
                    nxt = 1 - cur
                    for c in range(NCH):
                        Mc, MTc, Mn = M[c][cur], MT[c][cur], M[c][nxt]
                        p = mm4(lambda h, MTc=MTc: MTc.t[:, h, :], lambda h, Mc=Mc: Mc.t[:, h, :], [Mc, MTc])
                        S.op("act", lambda e, p=p, Mn=Mn: e.activation(out=Mn.t[:], in_=pv(p), func=AF.Copy), reads=[p], writes=[Mn])
                    if lvl < 4:
                        for c in range(NCH):
                            Mc, MTc, MTn = M[c][cur], MT[c][cur], MT[c][nxt]
                            p = mm4(lambda h, Mc=Mc: Mc.t[:, h, :], lambda h, MTc=MTc: MTc.t[:, h, :], [Mc, MTc])
                            if c % 2 == 0:
                                S.op("act", lambda e, p=p, MTn=MTn: e.activation(out=MTn.t[:], in_=pv(p), func=AF.Copy), reads=[p], writes=[MTn])
                            else:
                                S.op("dve", lambda e, p=p, MTn=MTn: e.tensor_copy(out=MTn.t[:], in_=pv(p)), reads=[p], writes=[MTn])
                    for c in range(NCH):
                        Mn = M[c][nxt]
                        TTc = TT[c]
                        p = mm4(lambda h, Mn=Mn: Mn.t[:, h, :], lambda h, TTc=TTc: TTc.t[:, h, :], [Mn, TTc])
                        S.op("dve", lambda e, p=p, TTc=TTc: e.tensor_tensor(out=TTc.t[:], in0=pv(p), in1=TTc.t[:], op=ALU.add), reads=[p, TTc], writes=[TTc])
                    cur = nxt

                def gn_out(c):
                    tc = tcs[c]
                    Y_ = YS[c]
                    S.op("dve", lambda e: e.tensor_reduce(out=s1.t[:], in_=Y_.t[:], axis=AX.X, op=ALU.add), reads=[Y_], writes=[s1])
                    S.op("pool", lambda e: e.tensor_tensor(out=SQ.t[:], in0=Y_.t[:], in1=Y_.t[:], op=ALU.mult), reads=[Y_], writes=[SQ])
                    S.op("dve", lambda e: e.tensor_reduce(out=s2.t[:], in_=SQ.t[:], axis=AX.X, op=ALU.add), reads=[SQ], writes=[s2])
                    S.op("dve", lambda e: e.tensor_scalar(out=mean.t[:], in0=s1.t[:], scalar1=1.0 / 64, scalar2=None, op0=ALU.mult), reads=[s1], writes=[mean])
                    S.op("dve", lambda e: e.tensor_tensor(out=s1.t[:], in0=mean.t[:], in1=mean.t[:], op=ALU.mult), reads=[mean], writes=[s1])
                    S.op("dve", lambda e: e.scalar_tensor_tensor(out=rstd.t[:], in0=s2.t[:], scalar=1.0 / 64, in1=s1.t[:], op0=ALU.mult, op1=ALU.subtract),
                         reads=[s2, s1], writes=[rstd])
                    S.op("dve", lambda e: e.tensor_scalar(out=rstd.t[:], in0=rstd.t[:], scalar1=GN_EPS, scalar2=None, op0=ALU.add), reads=[rstd], writes=[rstd])
                    S.op("act", lambda e: e.activation(out=rstd.t[:], in_=rstd.t[:], func=AF.Ln), reads=[rstd], writes=[rstd])
                    S.op("act", lambda e: e.activation(out=rstd.t[:], in_=rstd.t[:], func=AF.Exp, scale=-0.5), reads=[rstd], writes=[rstd])
                    S.op("pool", lambda e: e.tensor_tensor(out=Y_.t[:], in0=Y_.t[:], in1=bc3(mean.t[:, :], 64, last=False), op=ALU.subtract),
                         reads=[Y_, mean], writes=[Y_])
                    S.op("pool", lambda e: e.tensor_tensor(out=Y_.t[:], in0=Y_.t[:], in1=bc3(rstd.t[:, :], 64, last=False), op=ALU.mult),
                         reads=[Y_, rstd], writes=[Y_])
                    pt = S.ps()
                    for h in range(NH):
                        S.op("pe", lambda e, pt=pt, h=h: e.transpose(pt.t[0:64, h * 64:(h + 1) * 64], Y_.t[:, h, :], id64), reads=[Y_, CONST], writes=[pt])
                    for h in range(NH):
                        H = H0 + h
                        S.op("act", lambda e, pt=pt, h=h, H=H: e.activation(out=YN.t[:, h, tc], in_=pt.t[0:64, h * 64:(h + 1) * 64], func=AF.Identity,
                                                                           scale=LNW.t[:, H:H + 1], bias=LNB.t[:, H:H + 1]), reads=[pt, LNW, LNB], writes=[YN])

                for c in range(NCH):
                    tc = tcs[c]
                    p = S.ps()
                    for h in range(NH):
                        S.op("pe", lambda e, p=p, h=h: e.matmul(p.t[0:64, h * 64:(h + 1) * 64], lhsT=KKD.t[:, h, tc], rhs=ST.t[:, H0 + h, :],
                                                              start=True, stop=False), reads=[KKD, ST], writes=[p])
                        S.op("pe", lambda e, p=p, h=h: e.matmul(p.t[0:64, h * 64:(h + 1) * 64], lhsT=BTm[c].t[:, h, :], rhs=VT.t[:, c, h, :],
                                                              start=False, stop=True), reads=[BTm[c], VT], writes=[p])
                    S.op("act", lambda e, p=p: e.activation(out=XS.t[:], in_=pv(p), func=AF.Copy), reads=[p], writes=[XS])
                    TTc = TT[c]
                    p = mm4(lambda h: TTc.t[:, h, :], lambda h: XS.t[:, h, :], [TTc, XS])
                    S.op("dve", lambda e, p=p: e.tensor_scalar(out=US.t[:], in0=pv(p), scalar1=-1.0, scalar2=None, op0=ALU.mult), reads=[p], writes=[US])
                    p = S.ps()
                    for h in range(NH):
                        o = p.t[0:64, h * 64:(h + 1) * 64]
                        S.op("pe", lambda e, o=o, h=h: e.matmul(o, lhsT=BIT.t[:, c, h, :], rhs=US.t[:, h, :], start=True, stop=False),
                             reads=[BIT, US], writes=[p])
                        S.op("pe", lambda e, o=o, h=h: e.matmul(o, lhsT=KIT.t[:, c, h, :], rhs=VT.t[:, c, h, :], start=False, stop=True),
                             reads=[KIT, VT], writes=[p])
                    py = S.ps()
                    for h in range(NH):
                        o = py.t[0:64, h * 64:(h + 1) * 64]
                        S.op("pe", lambda e, o=o, h=h: e.matmul(o, lhsT=RD.t[:, h, tc], rhs=ST.t[:, H0 + h, :], start=True, stop=False),
                             reads=[RD, ST], writes=[py])
                        S.op("pe", lambda e, o=o, h=h: e.matmul(o, lhsT=PTm[c].t[:, h, :], rhs=US.t[:, h, :], start=False, stop=False),
                             reads=[PTm[c], US], writes=[py])
                        S.op("pe", lambda e, o=o, h=h: e.matmul(o, lhsT=QTm[c].t[:, h, :], rhs=VT.t[:, c, h, :], start=False, stop=True),
                             reads=[QTm[c], VT], writes=[py])
                    S.op("dve", lambda e, p=p: e.tensor_tensor(out=TMP.t[:], in0=pv(p), in1=ST.t[:, H0:H0 + NH, :], op=ALU.add),
                         reads=[p, ST], writes=[TMP])
                    S.op("dve", lambda e, c=c: e.tensor_tensor(out=ST.t[:, H0:H0 + NH, :], in0=TMP.t[:],
                                                               in1=GAM.t[:, :, c * C + C - 1:c * C + C].broadcast_to([64, NH, 64]), op=ALU.mult),
                         reads=[TMP, GAM], writes=[ST])
                    S.op("act", lambda e, py=py, c=c: e.activation(out=YS[c].t[:], in_=pv(py), func=AF.Copy), reads=[py], writes=[YS[c]])
                    if c > 0:
                        gn_out(c - 1)
                gn_out(NCH - 1)
                S.op("dve", lambda e: e.tensor_tensor(out=YN.t[:], in0=YN.t[:], in1=BON.t[:], op=ALU.add), reads=[YN, BON], writes=[YN])
                S.op("dve", lambda e: e.tensor_tensor(out=YN.t[:], in0=YN.t[:], in1=G_.t[:], op=ALU.mult), reads=[YN, G_], writes=[YN])
                S.dma("sp", lambda e, c0=c0, t0=t0: e.dma_start(
                    out=io["YT"][c0:c0 + NH * 64, t0:t0 + NT].rearrange("(h i) t -> i h t", i=64), in_=YN.t[:]), reads=[YN], writes=[])
        S.flush()


def phase_attn(nc, S, io, CONST, ident):
    TQ = 1024
    NBQ = TQ // 128
    PT = io["PT"]
    QB = NRW
    KB = NRW + 2048
    VB = NRW + 2048 + 512
    with contextlib.ExitStack() as ph:
        def tl(shape, dt=F32):
            return S.tile(ph, shape, dt)

        OG = tl([64, 32])
        r_ = tl([32, 64])
        S.dma("sp", lambda e: e.dma_start(out=r_.t[:], in_=io["attn_out_g"].rearrange("o (h j) -> (o h) j", j=64)), writes=[r_])
        ps = S.ps()
        S.op("pe", lambda e: e.transpose(ps.t[0:64, 0:32], r_.t[:], ident[0:32, 0:32]), reads=[r_, CONST], writes=[ps])
        S.op("dve", lambda e: e.tensor_copy(out=OG.t[:], in_=ps.t[0:64, 0:32]), reads=[ps], writes=[OG])
        SINK = tl([128, 32])
        S.dma("sp", lambda e: e.dma_start(out=SINK.t[:], in_=io["attn_sinks"][0].partition_broadcast(128)), writes=[SINK])

        QT = tl([64, 4, TQ]); YA = tl([64, 4, TQ]); KT = tl([64, TQ + 128]); VT_ = tl([64, TQ + 128])
        AB = tl([128, 4, 256]); AB0 = tl([128, 4, 256])
        SC = tl([128, 4, 256]); PTS = tl([128, 4, 2, 128]); VTM = tl([128, NBQ + 1, 64])
        O = tl([128, 4, 64]); SQ = tl([128, 4, 64])
        mx = tl([128, 4]); nmx = tl([128, 4]); rs = tl([128, 4]); es = tl([128, 4]); ss = tl([128, 4])
        abv = io["attn_bias"].rearrange("p (v h c) -> p v h c", v=2, h=8)
        for h in range(8):
            S.dma("sp", lambda e, h=h: e.dma_start(out=AB.t[:], in_=abv[:, 0, h, :].rearrange("p (g k) -> p g k", g=4)), writes=[AB])
            S.dma("sp", lambda e, h=h: e.dma_start(out=AB0.t[:], in_=abv[:, 1, h, :].rearrange("p (g k) -> p g k", g=4)), writes=[AB0])
            for tq in range(T // TQ):
                t0 = tq * TQ
                S.dma("sp", lambda e, h=h, t0=t0: e.dma_start(
                    out=QT.t[:], in_=PT[QB + h * 256:QB + (h + 1) * 256, t0:t0 + TQ].rearrange("(g d) t -> d g t", d=64)), writes=[QT])
                for (dst, base) in ((KT, KB), (VT_, VB)):
                    if tq == 0:
                        S.op("pool", lambda e, dst=dst: e.memset(dst.t[:, 0:128], 0.0), writes=[dst])
                        S.dma("sp", lambda e, dst=dst, base=base, h=h: e.dma_start(
                            out=dst.t[:, 128:128 + TQ], in_=PT[base + h * 64:base + (h + 1) * 64, 0:TQ]), writes=[dst])
                    else:
                        S.dma("sp", lambda e, dst=dst, base=base, h=h, t0=t0: e.dma_start(
                            out=dst.t[:], in_=PT[base + h * 64:base + (h + 1) * 64, t0 - 128:t0 + TQ]), writes=[dst])
                for half in range(2):
                    blks = list(range(half * 8, min(NBQ + 1, half * 8 + 8)))
                    pv_ = S.ps()
                    for i, bk in enumerate(blks):
                        S.op("pe", lambda e, pv_=pv_, i=i, bk=bk: e.transpose(pv_.t[:, i * 64:(i + 1) * 64], VT_.t[:, bk * 128:(bk + 1) * 128],
                                                                             ident[0:64, 0:64]), reads=[VT_, CONST], writes=[pv_])
                    nb_ = len(blks)
                    S.op("dve", lambda e, pv_=pv_, b0=blks[0], nb_=nb_: e.tensor_copy(
                        out=VTM.t[:, b0:b0 + nb_, :], in_=pv_.t[:, 0:nb_ * 64].rearrange("p (b d) -> p b d", d=64)), reads=[pv_], writes=[VTM])
                for n in range(NBQ):
                    qs = slice(n * 128, (n + 1) * 128)
                    ABn = AB0 if (tq == 0 and n == 0) else AB
                    pss = [S.ps(), S.ps()]
                    for g in range(4):
                        S.op("pe", lambda e, g=g, p=pss[g // 2]: e.matmul(p.t[:, (g % 2) * 256:(g % 2 + 1) * 256], lhsT=QT.t[:, g, qs],
                                                                        rhs=KT.t[:, n * 128:n * 128 + 256], start=True, stop=True),
                             reads=[QT, KT], writes=[pss[g // 2]])
                    for b2 in range(2):
                        S.op("dve", lambda e, b2=b2, p=pss[b2], ABn=ABn: e.scalar_tensor_tensor(
                            out=SC.t[:, 2 * b2:2 * b2 + 2, :], in0=p.t[:, 0:512].rearrange("p (g k) -> p g k", g=2), scalar=0.125,
                            in1=ABn.t[:, 2 * b2:2 * b2 + 2, :], op0=ALU.mult, op1=ALU.add), reads=[pss[b2], ABn], writes=[SC])
                    S.op("dve", lambda e: e.tensor_reduce(out=mx.t[:], in_=SC.t[:], axis=AX.X, op=ALU.max), reads=[SC], writes=[mx])
                    S.op("dve", lambda e, h=h: e.tensor_tensor(out=mx.t[:], in0=mx.t[:], in1=SINK.t[:, 4 * h:4 * h + 4], op=ALU.max),
                         reads=[mx, SINK], writes=[mx])
                    S.op("dve", lambda e: e.tensor_scalar(out=nmx.t[:], in0=mx.t[:], scalar1=-1.0, scalar2=None, op0=ALU.mult), reads=[mx], writes=[nmx])
                    for g in range(4):
                        S.op("act", lambda e, g=g: e.activation(out=SC.t[:, g, :], in_=SC.t[:, g, :], func=AF.Exp, bias=nmx.t[:, g:g + 1],
                                                                accum_out=rs.t[:, g:g + 1]), reads=[SC, nmx], writes=[SC, rs])
                    S.op("dve", lambda e, h=h: e.tensor_tensor(out=es.t[:], in0=SINK.t[:, 4 * h:4 * h + 4], in1=mx.t[:], op=ALU.subtract),
                         reads=[SINK, mx], writes=[es])
                    S.op("act", lambda e: e.activation(out=es.t[:], in_=es.t[:], func=AF.Exp), reads=[es], writes=[es])
                    S.op("dve", lambda e: e.tensor_tensor(out=rs.t[:], in0=rs.t[:], in1=es.t[:], op=ALU.add), reads=[rs, es], writes=[rs])
                    S.op("dve", lambda e: e.reciprocal(out=rs.t[:], in_=rs.t[:]), reads=[rs], writes=[rs])
                    for b2 in range(2):
                        pt = S.ps()
                        for gi in range(2):
                            g = 2 * b2 + gi
                            for kh in range(2):
                                S.op("pe", lambda e, pt=pt, g=g, gi=gi, kh=kh: e.transpose(
                                    pt.t[:, (gi * 2 + kh) * 128:(gi * 2 + kh + 1) * 128], SC.t[:, g, kh * 128:(kh + 1) * 128], ident),
                                    reads=[SC, CONST], writes=[pt])
                        if b2 == 0:
                            S.op("act", lambda e, pt=pt, b2=b2: e.activation(out=PTS.t[:, 2 * b2:2 * b2 + 2, :, :],
                                                                             in_=pt.t[:, 0:512].rearrange("p (g k q) -> p g k q", g=2, k=2), func=AF.Copy),
                                 reads=[pt], writes=[PTS])
                        else:
                            S.op("dve", lambda e, pt=pt, b2=b2: e.tensor_copy(out=PTS.t[:, 2 * b2:2 * b2 + 2, :, :],
                                                                              in_=pt.t[:, 0:512].rearrange("p (g k q) -> p g k q", g=2, k=2)),
                                 reads=[pt], writes=[PTS])
                    po = S.ps()
                    for g in range(4):
                        for kh in range(2):
                            S.op("pe", lambda e, g=g, kh=kh, po=po: e.matmul(po.t[:, g * 64:(g + 1) * 64], lhsT=PTS.t[:, g, kh, :], rhs=VTM.t[:, n + kh, :],
                                                                           start=(kh == 0), stop=(kh == 1)), reads=[PTS, VTM], writes=[po])
                    S.op("dve", lambda e, po=po: e.tensor_tensor(out=O.t[:], in0=po.t[:, 0:256].rearrange("p (g d) -> p g d", g=4),
                                                                 in1=rs.t[:, :].unsqueeze(2).broadcast_to([128, 4, 64]), op=ALU.mult),
                         reads=[po, rs], writes=[O])
                    S.op("pool", lambda e: e.tensor_tensor(out=SQ.t[:], in0=O.t[:], in1=O.t[:], op=ALU.mult), reads=[O], writes=[SQ])
                    S.op("dve", lambda e: e.tensor_reduce(out=ss.t[:], in_=SQ.t[:], axis=AX.X, op=ALU.add), reads=[SQ], writes=[ss])
                    S.op("dve", lambda e: e.tensor_scalar(out=ss.t[:], in0=ss.t[:], scalar1=1.0 / 64, scalar2=EPS, op0=ALU.mult, op1=ALU.add),
                         reads=[ss], writes=[ss])
                    S.op("act", lambda e: e.activation(out=ss.t[:], in_=ss.t[:], func=AF.Sqrt), reads=[ss], writes=[ss])
                    S.op("dve", lambda e: e.reciprocal(out=ss.t[:], in_=ss.t[:]), reads=[ss], writes=[ss])
                    S.op("dve", lambda e: e.tensor_tensor(out=O.t[:], in0=O.t[:], in1=ss.t[:, :].unsqueeze(2).broadcast_to([128, 4, 64]), op=ALU.mult),
                         reads=[O, ss], writes=[O])
                    pT = S.ps()
                    for g in range(4):
                        S.op("pe", lambda e, g=g, pT=pT: e.transpose(pT.t[0:64, g * 128:(g + 1) * 128], O.t[:, g, :], ident), reads=[O, CONST], writes=[pT])
                    for g in range(4):
                        S.op("act", lambda e, g=g, pT=pT, h=h: e.activation(out=YA.t[:, g, qs], in_=pT.t[0:64, g * 128:(g + 1) * 128], func=AF.Copy,
                                                                           scale=OG.t[:, 4 * h + g:4 * h + g + 1]), reads=[pT, OG], writes=[YA])
                S.dma("sp", lambda e, h=h, t0=t0: e.dma_start(
                    out=io["YT"][2048 + h * 256:2048 + (h + 1) * 256, t0:t0 + TQ].rearrange("(g d) t -> d g t", d=64), in_=YA.t[:]),
                    reads=[YA], writes=[])
        S.flush()


def phase_wout(nc, S, io, CONST, ident, A2, SH2, norm_tile, gs=None):
    TQ = 1024
    X1 = io["X1"]
    MOD = io["MODROW"]
    with contextlib.ExitStack() as ph:
        def tl(shape, dt=F32):
            return S.tile(ph, shape, dt)
        G1 = tl([128, D])
        S.dma("sp", lambda e: e.dma_start(out=G1.t[:], in_=MOD[0, 2 * D:3 * D].partition_broadcast(128)), writes=[G1])
        yT = tl([128, 32, TQ], BF16)
        wos = [tl([128, 32, 512], BF16) for _ in range(2)]
        yTv = [Tl(yT.t) for _ in range(4)]
        wosv = [[Tl(w_.t) for _ in range(4)] for w_ in wos]
        xps = [tl([128, 512]) for _ in range(3)]
        tmps = [tl([128, 512]) for _ in range(2)]
        nw = 0
        nx = 0
        for tq in range(T // TQ):
            t0 = tq * TQ
            for kh in range(4):
                S.dma("pool", lambda e, t0=t0, kh=kh: e.dma_start(
                    out=yT.t[:, kh * 8:(kh + 1) * 8, :],
                    in_=io["YT"][kh * 1024:(kh + 1) * 1024, t0:t0 + TQ].rearrange("(k p) t -> p k t", p=128)), writes=[yTv[kh]])
            for cg in range(8):
                wo = wos[nw % 2]
                wov = wosv[nw % 2]
                nw += 1
                for kh in range(4):
                    S.dma("pool", lambda e, wo=wo, cg=cg, kh=kh: e.dma_start(
                        out=wo.t[:, kh * 8:(kh + 1) * 8, :],
                        in_=io["w_out"][kh * 1024:(kh + 1) * 1024, cg * 512:(cg + 1) * 512].rearrange("(k p) j -> p k j", p=128)), writes=[wov[kh]])
                for tt in range(TQ // 128):
                    r0 = t0 + tt * 128
                    xp = xps[nx % 3]
                    tmp = tmps[nx % 2]
                    nx += 1
                    S.dma("sp", lambda e, xp=xp, r0=r0, cg=cg: e.dma_start(out=xp.t[:], in_=io["x"][r0:r0 + 128, cg * 512:(cg + 1) * 512]), writes=[xp])
                    ps = S.ps()
                    for kc in range(32):
                        S.op("pe", lambda e, ps=ps, wo=wo, kc=kc, tt=tt: e.matmul(
                            ps.t[:, :], lhsT=yT.t[:, kc, tt * 128:(tt + 1) * 128], rhs=wo.t[:, kc, :], start=(kc == 0), stop=(kc == 31)),
                            reads=[yTv[kc // 8], wov[kc // 8]], writes=[ps])
                    S.op("dve", lambda e, ps=ps, tmp=tmp, cg=cg: e.tensor_tensor(out=tmp.t[:], in0=ps.t[:, :], in1=G1.t[:, cg * 512:(cg + 1) * 512], op=ALU.mult),
                         reads=[ps, G1], writes=[tmp])
                    S.op("pool", lambda e, tmp=tmp, xp=xp: e.tensor_tensor(out=xp.t[:], in0=tmp.t[:], in1=xp.t[:], op=ALU.add), reads=[tmp, xp], writes=[xp])
                    S.dma("sp", lambda e, xp=xp, r0=r0, cg=cg: e.dma_start(out=X1[r0:r0 + 128, cg * 512:(cg + 1) * 512], in_=xp.t[:]), reads=[xp], writes=[])
        S.flush()

    R_ = {}
    io["_route"] = R_
    ms = contextlib.ExitStack()
    io["_moe_stack"] = ms
    io["_IDXG"] = S.tile(ms, [128, NB], I32)
    io["_IDXD"] = S.tile(ms, [128, NB], I32)
    io["_CBF"] = S.tile(ms, [128, 128], BF16)
    gs = contextlib.ExitStack()
    io["_route_stack"] = gs
    R_["OH1"] = S.tile(gs, [128, 32, 64]); R_["OH2"] = S.tile(gs, [128, 32, 64]); R_["RANK"] = S.tile(gs, [128, 32, 64])
    R_["W1"] = S.tile(gs, [128, 32]); R_["W2"] = S.tile(gs, [128, 32]); R_["SELSUM"] = S.tile(gs, [128, 64])
    OH1, OH2, RANK, W1, W2, SELSUM = R_["OH1"], R_["OH2"], R_["RANK"], R_["W1"], R_["W2"], R_["SELSUM"]
    TRI = CONST.t[:, C_TRI:C_TRI + 128]
    ONES = CONST.t[:, C_ONES:C_ONES + 128]
    with contextlib.ExitStack() as ph:
        def tl(shape, dt=F32):
            return S.tile(ph, shape, dt)
        A2R = tl([128, D]); SH2R = tl([128, D])
        xts = [tl([128, D]) for _ in range(2)]
        scr = tl([128, D], BF16)
        ssq = tl([128, 1]); rstd = tl([128, 1])
        S.dma("sp", lambda e: e.dma_start(out=xts[0].t[:], in_=io["norm2_g"][0].partition_broadcast(128)), writes=[xts[0]])
        S.dma("sp", lambda e: e.dma_start(out=A2R.t[:], in_=MOD[0, 4 * D:5 * D].partition_broadcast(128)), writes=[A2R])
        S.dma("sp", lambda e: e.dma_start(out=SH2R.t[:], in_=MOD[0, 3 * D:4 * D].partition_broadcast(128)), writes=[SH2R])
        S.op("dve", lambda e: e.scalar_tensor_tensor(out=A2R.t[:], in0=A2R.t[:], scalar=1.0, in1=xts[0].t[:], op0=ALU.add, op1=ALU.mult),
             reads=[A2R, xts[0]], writes=[A2R])
        RT = tl([128, 32, 72])
        S.dma("sp", lambda e: e.dma_start(out=RT.t[:], in_=io["router"].rearrange("(k p) j -> p k j", p=128)), writes=[RT])
        RB = tl([128, 72])
        S.dma("sp", lambda e: e.dma_start(out=RB.t[:], in_=io["router_bias"][0].partition_broadcast(128)), writes=[RB])
        uTs = [tl([128, 4, 128]) for _ in range(2)]
        LG = tl([128, 72]); T88 = tl([128, 8, 8]); SEL = tl([128, 64])
        mg = tl([128, 1]); nmg = tl([128, 1]); sg = tl([128, 1]); eg = tl([128, 8]); ohg = tl([128, 8]); le = tl([128, 8]); le2 = tl([128, 8])
        m1 = tl([128, 1]); m2 = tl([128, 1]); oh1 = tl([128, 8]); oh2 = tl([128, 8]); dd = tl([128, 1]); w1 = tl([128, 1])
        S.op("pool", lambda e: e.memset(SELSUM.t[:], 0.0), writes=[SELSUM])
        zr = tl([1, D], BF16)
        S.op("pool", lambda e: e.memset(zr.t[:], 0.0), writes=[zr])
        S.dma("sp", lambda e: e.dma_start(out=io["U2"][T:T + 1, :], in_=zr.t[:]), reads=[zr], writes=[])
        nu = 0
        for tt in range(T // 128):
            xt = xts[tt % 2]
            r0 = tt * 128
            S.dma("sp", lambda e, xt=xt, r0=r0: e.dma_start(out=xt.t[:], in_=X1[r0:r0 + 128, :]), writes=[xt])
            norm_tile(xt, scr, ssq, rstd)
            S.op("dve", lambda e, xt=xt: e.tensor_tensor(out=xt.t[:], in0=xt.t[:], in1=A2R.t[:], op=ALU.mult), reads=[xt, A2R], writes=[xt])
            S.op("pool", lambda e, xt=xt: e.tensor_tensor(out=xt.t[:], in0=xt.t[:], in1=SH2R.t[:], op=ALU.add), reads=[xt, SH2R], writes=[xt])
            for hf in range(2):
                S.dma("pool", lambda e, xt=xt, r0=r0, hf=hf: e.dma_start(out=io["U2"][r0:r0 + 128, hf * 2048:(hf + 1) * 2048],
                                                                        in_=xt.t[:, hf * 2048:(hf + 1) * 2048]), reads=[xt], writes=[])
            pl = S.ps(hold=True)
            for kg in range(8):
                pt = S.ps()
                for k4 in range(4):
                    kc = kg * 4 + k4
                    S.op("pe", lambda e, pt=pt, xt=xt, kc=kc, k4=k4: e.transpose(pt.t[:, k4 * 128:(k4 + 1) * 128], xt.t[:, kc * 128:(kc + 1) * 128], ident),
                         reads=[xt, CONST], writes=[pt])
                uT = uTs[nu % 2]
                nu += 1
                if kg % 2 == 0:
                    S.op("dve", lambda e, pt=pt, uT=uT: e.tensor_copy(out=uT.t[:], in_=pt.t[:, :].rearrange("p (k t) -> p k t", k=4)), reads=[pt], writes=[uT])
                else:
                    S.op("act", lambda e, pt=pt, uT=uT: e.activation(out=uT.t[:], in_=pt.t[:, :].rearrange("p (k t) -> p k t", k=4), func=AF.Copy),
                         reads=[pt], writes=[uT])
                for k4 in range(4):
                    kc = kg * 4 + k4
                    S.op("pe", lambda e, pl=pl, uT=uT, kc=kc, k4=k4: e.matmul(pl.t[:, 0:72], lhsT=uT.t[:, k4, :], rhs=RT.t[:, kc, :],
                                                                             start=(kc == 0), stop=(kc == 31)), reads=[uT, RT], writes=[pl])
            S.ps_release(pl)
            S.op("dve", lambda e, pl=pl: e.tensor_tensor(out=LG.t[:], in0=pl.t[:, 0:72], in1=RB.t[:], op=ALU.add), reads=[pl, RB], writes=[LG])
            S.op("dve", lambda e: e.tensor_reduce(out=mg.t[:], in_=LG.t[:, 0:8], axis=AX.X, op=ALU.max), reads=[LG], writes=[mg])
            S.op("dve", lambda e: e.tensor_scalar(out=nmg.t[:], in0=mg.t[:], scalar1=-1.0, scalar2=None, op0=ALU.mult), reads=[mg], writes=[nmg])
            S.op("act", lambda e: e.activation(out=eg.t[:], in_=LG.t[:, 0:8], func=AF.Exp, bias=nmg.t[:, 0:1], accum_out=sg.t[:, 0:1]),
                 reads=[LG, nmg], writes=[eg, sg])
            S.op("dve", lambda e: e.reciprocal(out=sg.t[:], in_=sg.t[:]), reads=[sg], writes=[sg])
            S.op("dve", lambda e: e.tensor_scalar(out=ohg.t[:], in0=LG.t[:, 0:8], scalar1=mg.t[:, 0:1], scalar2=None, op0=ALU.is_equal),
                 reads=[LG, mg], writes=[ohg])
            S.op("dve", lambda e: e.tensor_tensor(out=T88.t[:], in0=LG.t[:, 8:72].rearrange("p (g x) -> p g x", g=8),
                                                  in1=ohg.t[:, :].unsqueeze(2).broadcast_to([128, 8, 8]), op=ALU.mult), reads=[LG, ohg], writes=[T88])
            S.op("dve", lambda e: e.tensor_reduce(out=le.t[:], in_=T88.t[:].rearrange("p g x -> p x g"), axis=AX.X, op=ALU.add), reads=[T88], writes=[le])
            S.op("dve", lambda e: e.tensor_reduce(out=m1.t[:], in_=le.t[:], axis=AX.X, op=ALU.max), reads=[le], writes=[m1])
            S.op("dve", lambda e: e.tensor_scalar(out=oh1.t[:], in0=le.t[:], scalar1=m1.t[:, 0:1], scalar2=None, op0=ALU.is_equal), reads=[le, m1], writes=[oh1])
            S.op("dve", lambda e: e.scalar_tensor_tensor(out=le2.t[:], in0=oh1.t[:], scalar=-1e30, in1=le.t[:], op0=ALU.mult, op1=ALU.add),
                 reads=[oh1, le], writes=[le2])
            S.op("dve", lambda e: e.tensor_reduce(out=m2.t[:], in_=le2.t[:], axis=AX.X, op=ALU.max), reads=[le2], writes=[m2])
            S.op("dve", lambda e: e.tensor_scalar(out=oh2.t[:], in0=le2.t[:], scalar1=m2.t[:, 0:1], scalar2=None, op0=ALU.is_equal), reads=[le2, m2], writes=[oh2])
            S.op("dve", lambda e: e.tensor_tensor(out=dd.t[:], in0=m2.t[:], in1=m1.t[:], op=ALU.subtract), reads=[m1, m2], writes=[dd])
            S.op("act", lambda e: e.activation(out=dd.t[:], in_=dd.t[:], func=AF.Exp), reads=[dd], writes=[dd])
            S.op("dve", lambda e: e.tensor_scalar(out=w1.t[:], in0=dd.t[:], scalar1=1.0, scalar2=None, op0=ALU.add), reads=[dd], writes=[w1])
            S.op("dve", lambda e: e.reciprocal(out=w1.t[:], in_=w1.t[:]), reads=[w1], writes=[w1])
            S.op("dve", lambda e: e.tensor_tensor(out=dd.t[:], in0=dd.t[:], in1=w1.t[:], op=ALU.mult), reads=[dd, w1], writes=[dd])
            S.op("dve", lambda e, tt=tt: e.tensor_tensor(out=W1.t[:, tt:tt + 1], in0=w1.t[:], in1=sg.t[:], op=ALU.mult), reads=[w1, sg], writes=[W1])
            S.op("dve", lambda e, tt=tt: e.tensor_tensor(out=W2.t[:, tt:tt + 1], in0=dd.t[:], in1=sg.t[:], op=ALU.mult), reads=[dd, sg], writes=[W2])
            for (oh, OHA) in ((oh1, OH1), (oh2, OH2)):
                S.op("dve", lambda e, oh=oh, OHA=OHA, tt=tt: e.tensor_tensor(
                    out=OHA.t[:, tt, :].rearrange("p (g x) -> p g x", g=8), in0=ohg.t[:, :].unsqueeze(2).broadcast_to([128, 8, 8]),
                    in1=oh.t[:, :].unsqueeze(1).broadcast_to([128, 8, 8]), op=ALU.mult), reads=[ohg, oh], writes=[OHA])
            S.op("dve", lambda e, tt=tt: e.tensor_tensor(out=SEL.t[:], in0=OH1.t[:, tt, :], in1=OH2.t[:, tt, :], op=ALU.add), reads=[OH1, OH2], writes=[SEL])
            pr = S.ps()
            S.op("pe", lambda e, pr=pr: e.matmul(pr.t[:, 0:64], lhsT=TRI, rhs=SEL.t[:], start=True, stop=False), reads=[SEL, CONST], writes=[pr])
            S.op("pe", lambda e, pr=pr: e.matmul(pr.t[:, 0:64], lhsT=ONES, rhs=SELSUM.t[:], start=False, stop=True), reads=[SELSUM, CONST], writes=[pr])
            S.op("act", lambda e, pr=pr, tt=tt: e.activation(out=RANK.t[:, tt, :], in_=pr.t[:, 0:64], func=AF.Copy), reads=[pr], writes=[RANK])
            S.op("pool", lambda e: e.tensor_tensor(out=SELSUM.t[:], in0=SELSUM.t[:], in1=SEL.t[:], op=ALU.add), reads=[SELSUM, SEL], writes=[SELSUM])
        S.flush()


def phase_moe(nc, S, io, CONST, ident):
    R_ = io["_route"]
    OH1, OH2, RANK, W1, W2, SELSUM = R_["OH1"], R_["OH2"], R_["RANK"], R_["W1"], R_["W2"], R_["SELSUM"]
    ONES = CONST.t[:, C_ONES:C_ONES + 128]
    K128 = CONST.t[:, C_K128:C_K128 + 64]
    PID = CONST.t[:, C_PID:C_PID + 1]
    P128 = CONST.t[:, C_P128:C_P128 + 1]
    ROWTOK = io["ROWTOK"]
    ROWW = io["ROWW"]
    YACC = io["YACC"]
    U2 = io["U2"]
    IOA = bass.IndirectOffsetOnAxis
    with contextlib.ExitStack() as ph:
        def tl(shape, dt=F32):
            return S.tile(ph, shape, dt)
        IDXG = io["_IDXG"]
        CONST_BF = io["_CBF"]
        S.op("dve", lambda e: e.tensor_copy(out=CONST_BF.t[:], in_=ident), reads=[CONST], writes=[CONST_BF])
        IDXD = io["_IDXD"]
        with contextlib.ExitStack() as p2:
            def t2(shape, dt=F32):
                return S.tile(p2, shape, dt)
            cnt = t2([64, 1]); nblk = t2([64, 1]); cmp = t2([64, 64]); pbc = t2([64, 128])
            PST = t2([128, 64]); PEND = t2([128, 64])
            pc = S.ps()
            S.op("pe", lambda e: e.matmul(pc.t[0:64, 0:128], lhsT=SELSUM.t[:], rhs=ONES, start=True, stop=True), reads=[SELSUM, CONST], writes=[pc])
            S.op("dve", lambda e: e.tensor_copy(out=cnt.t[:], in_=pc.t[0:64, 0:1]), reads=[pc], writes=[cnt])
            S.op("dve", lambda e: e.tensor_scalar(out=cmp.t[:], in0=K128[0:64, :], scalar1=cnt.t[:, 0:1], scalar2=None, op0=ALU.is_lt),
                 reads=[cnt, CONST], writes=[cmp])
            S.op("dve", lambda e: e.tensor_reduce(out=nblk.t[:], in_=cmp.t[:], axis=AX.X, op=ALU.add), reads=[cmp], writes=[nblk])
            S.op("dve", lambda e: e.tensor_scalar(out=nblk.t[:], in0=nblk.t[:], scalar1=128.0, scalar2=None, op0=ALU.mult), reads=[nblk], writes=[nblk])
            S.op("dve", lambda e: e.tensor_scalar(out=pbc.t[:], in0=ONES[0:64, :], scalar1=nblk.t[:, 0:1], scalar2=None, op0=ALU.mult),
                 reads=[nblk, CONST], writes=[pbc])
            pp = S.ps()
            S.op("pe", lambda e: e.matmul(pp.t[:, 0:64], lhsT=pbc.t[:], rhs=CONST.t[0:64, C_SU:C_SU + 64], start=True, stop=True), reads=[pbc, CONST], writes=[pp])
            S.op("dve", lambda e: e.tensor_copy(out=PST.t[:], in_=pp.t[:, 0:64]), reads=[pp], writes=[PST])
            pe_ = S.ps()
            S.op("pe", lambda e: e.matmul(pe_.t[:, 0:64], lhsT=pbc.t[:], rhs=CONST.t[0:64, C_SUI:C_SUI + 64], start=True, stop=True), reads=[pbc, CONST], writes=[pe_])
            S.op("dve", lambda e: e.tensor_copy(out=PEND.t[:], in_=pe_.t[:, 0:64]), reads=[pe_], writes=[PEND])
            TMPA = t2([128, 32, 64]); TMPB = t2([128, 32, 64])
            DEST = [t2([128, 32]), t2([128, 32])]
            DESTI = [t2([128, 32], I32), t2([128, 32], I32)]
            S.op("dve", lambda e: e.tensor_tensor(out=TMPA.t[:], in0=RANK.t[:], in1=PST.t[:, :].unsqueeze(1).broadcast_to([128, 32, 64]), op=ALU.add),
                 reads=[RANK, PST], writes=[TMPA])
            for k, OH in enumerate((OH1, OH2)):
                S.op("dve", lambda e, OH=OH: e.tensor_tensor(out=TMPB.t[:], in0=TMPA.t[:], in1=OH.t[:], op=ALU.mult), reads=[TMPA, OH], writes=[TMPB])
                S.op("dve", lambda e, k=k: e.tensor_reduce(out=DEST[k].t[:], in_=TMPB.t[:], axis=AX.X, op=ALU.add), reads=[TMPB], writes=[DEST[k]])
                S.op("dve", lambda e, k=k: e.tensor_copy(out=DESTI[k].t[:], in_=DEST[k].t[:]), reads=[DEST[k]], writes=[DESTI[k]])
            TOK = t2([128, 32])
            S.op("dve", lambda e: e.tensor_scalar(out=TOK.t[:], in0=K128[:, 0:32], scalar1=PID, scalar2=None, op0=ALU.add), reads=[CONST], writes=[TOK])
            TOK16 = t2([128, 32, 16])
            S.op("dve", lambda e: e.tensor_copy(out=TOK16.t[:], in_=TOK.t[:, :].unsqueeze(2).broadcast_to([128, 32, 16])), reads=[TOK], writes=[TOK16])
            W16 = [t2([128, 32, 16]), t2([128, 32, 16])]
            for k, W in enumerate((W1, W2)):
                S.op("dve", lambda e, k=k, W=W: e.tensor_copy(out=W16[k].t[:], in_=W.t[:, :].unsqueeze(2).broadcast_to([128, 32, 16])), reads=[W], writes=[W16[k]])
            FILL = t2([128, 128, 16])
            S.op("pool", lambda e: e.memset(FILL.t[:], float(T)), writes=[FILL])
            S.dma("sp", lambda e: e.dma_start(out=ROWTOK.rearrange("(p r) c -> p r c", p=128), in_=FILL.t[:]), reads=[FILL], writes=[])
            FILL0 = t2([128, 128, 16])
            S.op("pool", lambda e: e.memset(FILL0.t[:], 0.0), writes=[FILL0])
            S.dma("sp", lambda e: e.dma_start(out=ROWW.rearrange("(p r) c -> p r c", p=128), in_=FILL0.t[:]), reads=[FILL0], writes=[])
            S.flush()
            for tt in range(32):
                for k in range(2):
                    S.dma("pool", lambda e, tt=tt, k=k: e.indirect_dma_start(
                        out=ROWTOK, out_offset=IOA(ap=DESTI[k].t[:, tt:tt + 1], axis=0), in_=TOK16.t[:, tt, :], in_offset=None),
                        reads=[DESTI[k], TOK16], writes=[])
                    S.dma("pool", lambda e, tt=tt, k=k: e.indirect_dma_start(
                        out=ROWW, out_offset=IOA(ap=DESTI[k].t[:, tt:tt + 1], axis=0), in_=W16[k].t[:, tt, :], in_offset=None),
                        reads=[DESTI[k], W16[k]], writes=[])
            BE = t2([128, 1]); cmp2 = t2([128, 64]); DG = t2([128, 128]); BEROW = t2([128, 128]); BASE = t2([128, 128])
            S.op("dve", lambda e: e.tensor_scalar(out=cmp2.t[:], in0=PEND.t[:], scalar1=P128, scalar2=None, op0=ALU.is_le), reads=[PEND, CONST], writes=[cmp2])
            S.op("dve", lambda e: e.tensor_reduce(out=BE.t[:], in_=cmp2.t[:], axis=AX.X, op=ALU.add), reads=[cmp2], writes=[BE])
            S.op("dve", lambda e: e.tensor_scalar(out=BE.t[:], in0=BE.t[:], scalar1=63.0, scalar2=None, op0=ALU.min), reads=[BE], writes=[BE])
            EMP = t2([128, 1])
            S.op("dve", lambda e: e.tensor_tensor(out=EMP.t[:], in0=P128, in1=PEND.t[:, 63:64], op=ALU.is_ge), reads=[PEND, CONST], writes=[EMP])
            S.op("dve", lambda e: e.scalar_tensor_tensor(out=BE.t[:], in0=EMP.t[:], scalar=1000.0, in1=BE.t[:], op0=ALU.mult, op1=ALU.add),
                 reads=[EMP, BE], writes=[BE])
            S.op("dve", lambda e: e.tensor_scalar(out=DG.t[:], in0=ident, scalar1=BE.t[:, 0:1], scalar2=None, op0=ALU.mult), reads=[BE, CONST], writes=[DG])
            pb = S.ps()
            S.op("pe", lambda e: e.matmul(pb.t[:, 0:128], lhsT=ONES, rhs=DG.t[:], start=True, stop=True), reads=[DG, CONST], writes=[pb])
            S.op("dve", lambda e: e.tensor_copy(out=BEROW.t[:], in_=pb.t[:, 0:128]), reads=[pb], writes=[BEROW])
            S.op("dve", lambda e: e.tensor_scalar(out=BASE.t[:], in0=BEROW.t[:], scalar1=4096.0, scalar2=PID, op0=ALU.mult, op1=ALU.add),
                 reads=[BEROW, CONST], writes=[BASE])
            S.op("dve", lambda e: e.tensor_copy(out=IDXG.t[:], in_=BASE.t[:]), reads=[BASE], writes=[IDXG])
            S.op("dve", lambda e: e.tensor_scalar(out=BASE.t[:], in0=BEROW.t[:], scalar1=512.0, scalar2=PID, op0=ALU.mult, op1=ALU.add),
                 reads=[BEROW, CONST], writes=[BASE])
            S.op("dve", lambda e: e.tensor_scalar(out=BASE.t[:], in0=BASE.t[:], scalar1=2.0, scalar2=None, op0=ALU.mult), reads=[BASE], writes=[BASE])
            S.op("dve", lambda e: e.tensor_copy(out=IDXD.t[:], in_=BASE.t[:]), reads=[BASE], writes=[IDXD])
            S.flush()
        io["_route_stack"].close()

        WG = [tl([128, 8, 512], BF16) for _ in range(3)]
        WU = [tl([128, 8, 512], BF16) for _ in range(3)]
        WDS = [[tl([128, D], BF16) for _ in range(4)] for _ in range(2)]
        WGv = [[Tl(w.t) for _ in range(8)] for w in WG]
        WUv = [[Tl(w.t) for _ in range(8)] for w in WU]
        WDv = [[[Tl(w.t) for _ in range(2)] for w in ws] for ws in WDS]
        xbs = [tl([128, D], BF16) for _ in range(2)]
        xbT = tl([128, 32, 128], BF16)
        hs = tl([128, 512]); hb = tl([128, 512], BF16); hT = tl([128, 4, 128], BF16)
        yb = tl([128, D])
        TIF = [tl([128, 1]) for _ in range(3)]; RW = [tl([128, 1]) for _ in range(3)]
        TI = [tl([128, 1], I32) for _ in range(3)]; TI2 = [tl([128, 2], I32) for _ in range(3)]; TF2 = [tl([128, 2]) for _ in range(3)]
        yacc_dep = tl([1, 1])
        S.op("pool", lambda e: e.memset(yb.t[:], 0.0), writes=[yb])
        for tt in range(T // 128):
            S.dma("sp", lambda e, tt=tt: e.dma_start(out=YACC[tt * 128:(tt + 1) * 128, :], in_=yb.t[:]), reads=[yb], writes=[yacc_dep])
        S.dma("sp", lambda e: e.dma_start(out=YACC[T:T + 1, :], in_=yb.t[0:1, :]), reads=[yb], writes=[yacc_dep])
        S.flush()
        REG_G = nc.gpsimd.alloc_register("bc_gate")
        nc.gpsimd.reg_mov(REG_G, 64 * D)
        REG_D = nc.gpsimd.alloc_register("bc_down")
        nc.gpsimd.reg_mov(REG_D, 70000)
        EWG = io["ew_gate"]; EWU = io["ew_up"]
        EWD = io["ew_down"].rearrange("r (two c) -> (r two) c", two=2)
        YACC2 = YACC.rearrange("r (two c) -> (r two) c", two=2)
        nw = 0

        def emit_idx(i):
            b = i % 3
            S.dma("sp", lambda e: e.dma_start(out=TIF[b].t[:], in_=ROWTOK[i * 128:(i + 1) * 128, 0:1], allow_slow_non_contiguous=True), writes=[TIF[b]])
            S.dma("sp", lambda e: e.dma_start(out=RW[b].t[:], in_=ROWW[i * 128:(i + 1) * 128, 0:1], allow_slow_non_contiguous=True), writes=[RW[b]])
            S.op("dve", lambda e: e.tensor_copy(out=TI[b].t[:], in_=TIF[b].t[:]), reads=[TIF[b]], writes=[TI[b]])
            S.op("dve", lambda e: e.tensor_scalar(out=TF2[b].t[:, 0:1], in0=TIF[b].t[:], scalar1=2.0, scalar2=None, op0=ALU.mult), reads=[TIF[b]], writes=[TF2[b]])
            S.op("dve", lambda e: e.tensor_scalar(out=TF2[b].t[:, 1:2], in0=TIF[b].t[:], scalar1=2.0, scalar2=1.0, op0=ALU.mult, op1=ALU.add),
                 reads=[TIF[b]], writes=[TF2[b]])
            S.op("dve", lambda e: e.tensor_copy(out=TI2[b].t[:], in_=TF2[b].t[:]), reads=[TF2[b]], writes=[TI2[b]])
            xb = xbs[i % 2]
            S.dma("pool", lambda e: e.indirect_dma_start(out=xb.t[:], out_offset=None, in_=U2, in_offset=IOA(ap=TI[b].t[:, 0:1], axis=0)),
                  reads=[TI[b]], writes=[xb])

        def emit_wd(i):
            wd = WDS[i % 2]
            for fc in range(4):
                for hf in range(2):
                    S.dma("pool", lambda e, fc=fc, hf=hf: e.indirect_dma_start(
                        out=wd[fc].t[:, hf * 2048:(hf + 1) * 2048], out_offset=None, in_=EWD, in_offset=IOA(ap=IDXD.t[:, i:i + 1], axis=0),
                        element_offset=(fc * 256 + hf) * 2048, bounds_check=REG_D, oob_is_err=False), reads=[IDXD], writes=[WDv[i % 2][fc][hf]])

        def emit_scatter(i):
            b = i % 3
            for hf in range(2):
                S.dma("pool", lambda e, hf=hf: e.indirect_dma_start(
                    out=YACC2, out_offset=IOA(ap=TI2[b].t[:, hf:hf + 1], axis=0), in_=yb.t[:, hf * 2048:(hf + 1) * 2048], in_offset=None,
                    compute_op=ALU.add), reads=[yb, TI2[b]], writes=[yacc_dep])

        emit_idx(0)
        emit_wd(0)
        for i in range(NB):
            b = i % 3
            if i + 1 < NB:
                emit_idx(i + 1)
                emit_wd(i + 1)
            xb = xbs[i % 2]
            wd = WDS[i % 2]
            for kg in range(8):
                pt = S.ps()
                ptb = pt.t[:, :].bitcast(BF16)
                for k4 in range(4):
                    kc = kg * 4 + k4
                    S.op("pe", lambda e, ptb=ptb, xb=xb, kc=kc, k4=k4: e.transpose(ptb[:, k4 * 128:(k4 + 1) * 128], xb.t[:, kc * 128:(kc + 1) * 128],
                                                                                  CONST_BF.t[:, :]), reads=[xb, CONST_BF], writes=[pt])
                if kg % 2 == 0:
                    S.op("dve", lambda e, ptb=ptb, kg=kg: e.tensor_copy(out=xbT.t[:, kg * 4:(kg + 1) * 4, :],
                                                                        in_=ptb[:, 0:512].rearrange("p (k t) -> p k t", k=4)), reads=[pt], writes=[xbT])
                else:
                    S.op("act", lambda e, ptb=ptb, kg=kg: e.activation(out=xbT.t[:, kg * 4:(kg + 1) * 4, :],
                                                                       in_=ptb[:, 0:512].rearrange("p (k t) -> p k t", k=4), func=AF.Copy), reads=[pt], writes=[xbT])
            pg = S.ps(hold=True)
            pu = S.ps(hold=True)
            for grp in range(4):
                wg = WG[nw % 3]
                wu = WU[nw % 3]
                wgv = WGv[nw % 3]
                wuv = WUv[nw % 3]
                nw += 1
                for k8 in range(8):
                    kc = grp * 8 + k8
                    S.dma("pool", lambda e, wg=wg, i=i, kc=kc, k8=k8: e.indirect_dma_start(
                        out=wg.t[:, k8, :], out_offset=None, in_=EWG, in_offset=IOA(ap=IDXG.t[:, i:i + 1], axis=0), element_offset=kc * 128 * 512,
                        bounds_check=REG_G, oob_is_err=False), reads=[IDXG], writes=[wgv[k8]])
                    S.dma("pool", lambda e, wu=wu, i=i, kc=kc, k8=k8: e.indirect_dma_start(
                        out=wu.t[:, k8, :], out_offset=None, in_=EWU, in_offset=IOA(ap=IDXG.t[:, i:i + 1], axis=0), element_offset=kc * 128 * 512,
                        bounds_check=REG_G, oob_is_err=False), reads=[IDXG], writes=[wuv[k8]])
                for k8 in range(8):
                    kc = grp * 8 + k8
                    S.op("pe", lambda e, pg=pg, wg=wg, kc=kc, k8=k8: e.matmul(pg.t[:, :], lhsT=xbT.t[:, kc, :], rhs=wg.t[:, k8, :], start=(kc == 0), stop=(kc == 31)),
                         reads=[xbT, wgv[k8]], writes=[pg])
                    S.op("pe", lambda e, pu=pu, wu=wu, kc=kc, k8=k8: e.matmul(pu.t[:, :], lhsT=xbT.t[:, kc, :], rhs=wu.t[:, k8, :], start=(kc == 0), stop=(kc == 31)),
                         reads=[xbT, wuv[k8]], writes=[pu])
                if grp == 1 and i > 0:
                    emit_scatter(i - 1)
            S.ps_release(pg)
            S.ps_release(pu)
            S.op("act", lambda e, pg=pg: e.activation(out=hs.t[:], in_=pg.t[:, :], func=AF.Silu), reads=[pg], writes=[hs])
            S.op("dve", lambda e, pu=pu, b=b: e.scalar_tensor_tensor(out=hb.t[:], in0=pu.t[:, :], scalar=RW[b].t[:, 0:1], in1=hs.t[:], op0=ALU.mult, op1=ALU.mult),
                 reads=[pu, RW[b], hs], writes=[hb])
            pt = S.ps()
            ptb = pt.t[:, :].bitcast(BF16)
            for fc in range(4):
                S.op("pe", lambda e, ptb=ptb, fc=fc: e.transpose(ptb[:, fc * 128:(fc + 1) * 128], hb.t[:, fc * 128:(fc + 1) * 128], CONST_BF.t[:, :]),
                     reads=[hb, CONST_BF], writes=[pt])
            S.op("dve", lambda e, ptb=ptb: e.tensor_copy(out=hT.t[:], in_=ptb[:, 0:512].rearrange("p (k t) -> p k t", k=4)), reads=[pt], writes=[hT])
            for cg in range(8):
                py = S.ps()
                for fc in range(4):
                    S.op("pe", lambda e, py=py, fc=fc, cg=cg, wd=wd: e.matmul(py.t[:, :], lhsT=hT.t[:, fc, :], rhs=wd[fc].t[:, cg * 512:(cg + 1) * 512],
                                                                      start=(fc == 0), stop=(fc == 3)), reads=[hT, WDv[i % 2][fc][cg // 4]], writes=[py])
                if cg % 2 == 0:
                    S.op("act", lambda e, py=py, cg=cg: e.activation(out=yb.t[:, cg * 512:(cg + 1) * 512], in_=py.t[:, :], func=AF.Copy), reads=[py], writes=[yb])
                else:
                    S.op("dve", lambda e, py=py, cg=cg: e.tensor_copy(out=yb.t[:, cg * 512:(cg + 1) * 512], in_=py.t[:, :]), reads=[py], writes=[yb])
        emit_scatter(NB - 1)
        S.flush()
    io["_moe_stack"].close()


def phase_final(nc, S, io, out):
    X1 = io["X1"]; YACC = io["YACC"]; MOD = io["MODROW"]
    with contextlib.ExitStack() as ph:
        def tl(shape, dt=F32):
            return S.tile(ph, shape, dt)
        G2 = tl([128, D]); NF = tl([128, D])
        S.dma("sp", lambda e: e.dma_start(out=G2.t[:], in_=MOD[0, 5 * D:6 * D].partition_broadcast(128)), writes=[G2])
        S.dma("sp", lambda e: e.dma_start(out=NF.t[:], in_=io["norm_f_g"][0].partition_broadcast(128)), writes=[NF])
        xts = [tl([128, D]) for _ in range(2)]
        yas = [tl([128, D]) for _ in range(2)]
        scr = tl([128, D], BF16)
        ssq = tl([128, 1]); rstd = tl([128, 1])
        outdep = tl([1, 1])
        for tt in range(T // 128):
            xt = xts[tt % 2]
            ya = yas[tt % 2]
            r0 = tt * 128
            S.dma("sp", lambda e, xt=xt, r0=r0: e.dma_start(out=xt.t[:], in_=X1[r0:r0 + 128, :]), writes=[xt])
            S.dma("sp", lambda e, ya=ya, r0=r0: e.dma_start(out=ya.t[:], in_=YACC[r0:r0 + 128, :]), writes=[ya])
            S.op("dve", lambda e, ya=ya: e.tensor_tensor(out=ya.t[:], in0=ya.t[:], in1=G2.t[:], op=ALU.mult), reads=[ya, G2], writes=[ya])
            S.op("pool", lambda e, ya=ya, xt=xt: e.tensor_tensor(out=xt.t[:], in0=xt.t[:], in1=ya.t[:], op=ALU.add), reads=[xt, ya], writes=[xt])
            io["_norm_tile"](xt, scr, ssq, rstd)
            S.op("dve", lambda e, xt=xt: e.tensor_tensor(out=xt.t[:], in0=xt.t[:], in1=NF.t[:], op=ALU.mult), reads=[xt, NF], writes=[xt])
            S.dma("sp", lambda e, xt=xt, r0=r0: e.dma_start(out=out[r0:r0 + 128, :], in_=xt.t[:]), reads=[xt], writes=[outdep])
        S.flush()


def _in_maps(inputs, cores, with_experts=True):
    f = lambda a: np.ascontiguousarray(a, dtype=np.float32)
    shared = {
        "w_cond": f(inputs["w_cond"][0]), "b_cond": f(inputs["b_cond"][0]).reshape(1, -1),
        "norm1_g": f(inputs["norm1_g"][0]).reshape(1, -1), "w_in": f(inputs["w_in"][0]),
        "rwkv_mu": f(inputs["rwkv_mu"][0]).reshape(1, -1), "rwkv_w0": f(inputs["rwkv_w0"][0]).reshape(1, -1),
        "rwkv_w_up": f(inputs["rwkv_w_up"][0]), "rwkv_a0": f(inputs["rwkv_a0"][0]).reshape(1, -1),
        "rwkv_a_up": f(inputs["rwkv_a_up"][0]), "rwkv_g_up": f(inputs["rwkv_g_up"][0]),
        "rwkv_k_k": f(inputs["rwkv_k_k"][0]).reshape(1, -1), "rwkv_k_a": f(inputs["rwkv_k_a"][0]).reshape(1, -1),
        "rwkv_r_k": f(inputs["rwkv_r_k"][0]).reshape(1, -1), "rwkv_lnx_w": f(inputs["rwkv_lnx_w"][0]).reshape(1, -1),
        "rwkv_lnx_b": f(inputs["rwkv_lnx_b"][0]).reshape(1, -1), "attn_sinks": f(inputs["attn_sinks"][0]).reshape(1, -1),
        "attn_out_g": f(inputs["attn_out_g"][0]).reshape(1, -1), "w_out": f(inputs["w_out"][0]),
        "norm2_g": f(inputs["norm2_g"][0]).reshape(1, -1),
        "router": f(np.concatenate([inputs["router_group"][0], inputs["router_expert"][0]], axis=1)),
        "router_bias": f(np.concatenate([inputs["router_group_bias"][0], inputs["router_expert_bias"][0]])).reshape(1, -1),
        "ew_gate": f(inputs["expert_w_gate"][0]).reshape(64 * D, 512),
        "ew_up": f(inputs["expert_w_up"][0]).reshape(64 * D, 512),
        "ew_down": f(inputs["expert_w_down"][0]).reshape(64 * 512, D),
        "norm_f_g": f(inputs["norm_f_g"]).reshape(1, -1),
        "consts": make_consts(), "attn_bias": make_attn_bias(),
    }
    if not with_experts:
        for k in ("ew_gate", "ew_up", "ew_down"):
            del shared[k]
    maps = []
    for b in cores:
        m = dict(shared)
        m["x"] = f(inputs["x"][b])
        m["c"] = f(inputs["c"][b]).reshape(1, -1)
        maps.append(m)
    return maps


def kernel(**inputs):
    nc = build()
    res = run_bass_kernel_spmd(nc, _in_maps(inputs, range(4)), core_ids=list(range(4)))
    return np.stack([np.asarray(r["out"], dtype=np.float32) for r in res.results], axis=0)
```
